# Optimizing a Trainium2 kernel written in Bass

```python
import jax, jax.numpy as jnp
from jax import lax
import numpy as np

D_MODEL = 1024
BATCH = 2
SEQ = 8192
DEPTH = 1

GRID_W = 64
CTX_LEN = 256
CONV_WIDTH = D_MODEL // 2
RET_HEADS = 4
RET_HEAD_DIM = (D_MODEL - CONV_WIDTH) // RET_HEADS
RET_WIDTH = RET_HEADS * RET_HEAD_DIM
MIX_WIDTH = CONV_WIDTH + RET_WIDTH
RET_CHUNK = 128
ROPE_BASE = 10000.0
N_EXPERTS = 16
EC_CAPACITY_FACTOR = 2
D_EXPERT = 2 * D_MODEL
RMS_EPS = 1e-6
GN_EPS = 1e-6
SPLITS = (CONV_WIDTH, CONV_WIDTH, CONV_WIDTH, RET_WIDTH, RET_WIDTH, RET_WIDTH, RET_WIDTH)
IN_COLS = sum(SPLITS)
SPLIT_IDX = tuple(int(s) for s in np.cumsum(SPLITS)[:-1])

kernel_name = "hybrid_conv_retention_ec_moe_dit"


def _rms_norm(x, g):
    xf = x.astype(jnp.float32)
    y = xf * lax.rsqrt(jnp.mean(xf * xf, axis=-1, keepdims=True) + RMS_EPS)
    return (y * g.astype(jnp.float32)).astype(x.dtype)


def _modulation(cond, w_ada, b_ada):
    m = jax.nn.silu(cond) @ w_ada + b_ada
    return jnp.split(m[:, None, :], 6, axis=-1)


def _rotary(t, pos):
    half = t.shape[-1] // 2
    inv = 1.0 / (ROPE_BASE ** (jnp.arange(half, dtype=jnp.float32) / half))
    ang = pos[:, None] * inv[None, :]
    cos = jnp.cos(ang)[None, :, None, :]
    sin = jnp.sin(ang)[None, :, None, :]
    t1, t2 = t[..., :half], t[..., half:]
    return jnp.concatenate([t1 * cos - t2 * sin, t1 * sin + t2 * cos], axis=-1)


def _ret_qkv(q, k, v, pos):
    b, n, _ = q.shape
    shp = (b, n, RET_HEADS, RET_HEAD_DIM)
    q = _rotary(q.reshape(shp).astype(jnp.float32), pos)
    k = _rotary(k.reshape(shp).astype(jnp.float32), pos) * (RET_HEAD_DIM ** -0.5)
    v = v.reshape(shp).astype(jnp.float32)
    return q, k, v


def _retention_scan(q, k, v, log_g, s0, include_self):
    b, n, h, d = q.shape
    nc = n // RET_CHUNK
    qc = q.reshape(b, nc, RET_CHUNK, h, d)
    kc = k.reshape(b, nc, RET_CHUNK, h, d)
    vc = v.reshape(b, nc, RET_CHUNK, h, d)
    idx = jnp.arange(RET_CHUNK, dtype=jnp.float32)
    rel = idx[:, None] - idx[None, :]
    mask = (rel >= 0) if include_self else (rel > 0)
    decay = jnp.where(mask[None], jnp.exp(log_g[:, None, None] * jnp.where(mask, rel, 0.0)[None]), 0.0)
    scores = jnp.einsum('bcihd,bcjhd->bchij', qc, kc) * decay[None, None]
    intra = jnp.einsum('bchij,bcjhd->bcihd', scores, vc)
    zeta = jnp.exp(log_g[None, :] * (RET_CHUNK - 1.0 - idx)[:, None])
    u = jnp.einsum('bcjhd,bcjhe->bchde', kc * zeta[None, None, :, :, None], vc)
    g_chunk = jnp.exp(log_g * RET_CHUNK)[None, :, None, None]

    def step(s, u_c):
        return g_chunk * s + u_c, s

    s_final, s_prev = lax.scan(step, s0, jnp.moveaxis(u, 1, 0))
    s_prev = jnp.moveaxis(s_prev, 0, 1)
    xi = jnp.exp(log_g[None, :] * (idx + 1.0)[:, None])
    inter = jnp.einsum('bcihd,bchde->bcihe', qc, s_prev) * xi[None, None, :, :, None]
    return (intra + inter).reshape(b, n, h, d), s_final


def _bidir_retention(q, k, v, log_gf, log_gb, s0_f, s0_b):
    y_f, s_f = _retention_scan(q, k, v, log_gf, s0_f, True)
    flip = lambda t: jnp.flip(t, axis=1)
    y_b, s_b = _retention_scan(flip(q), flip(k), flip(v), log_gb, s0_b, False)
    return y_f + flip(y_b), s_f, s_b


def _context_states(k, v, log_gf, log_gb):
    n = k.shape[1]
    pos = jnp.arange(n, dtype=jnp.float32)
    wf = jnp.exp(log_gf[None, :] * (n - 1.0 - pos)[:, None])
    wb = jnp.exp(log_gb[None, :] * pos[:, None])
    s_f = jnp.einsum('bnhd,nh,bnhe->bhde', k, wf, v)
    s_b = jnp.einsum('bnhd,nh,bnhe->bhde', k, wb, v)
    return s_f, s_b


def _ret_out(y, g, norm_g):
    b, n, _, _ = y.shape
    mu = jnp.mean(y, axis=-1, keepdims=True)
    var = jnp.mean(jnp.square(y - mu), axis=-1, keepdims=True)
    yn = ((y - mu) * lax.rsqrt(var + GN_EPS)).reshape(b, n, RET_WIDTH) * norm_g.astype(jnp.float32)
    return (jax.nn.silu(g.astype(jnp.float32)) * yn).astype(g.dtype)


def _conv_branch(gate_b, gate_c, u_in, w, bias, rows, width):
    bsz, n, ch = u_in.shape
    u = (gate_c * u_in).reshape(bsz, rows, width, ch)
    up = jnp.pad(u, ((0, 0), (0, 0), (1, 1), (0, 0)))
    y = up[:, :, :-2] * w[0] + up[:, :, 1:-1] * w[1] + up[:, :, 2:] * w[2] + bias
    return gate_b * y.reshape(bsz, n, ch)


def _expert_choice_ffn(h, w_router, b_router, w_gate, w_up, w_down):
    b, n, d = h.shape
    cap = max(1, EC_CAPACITY_FACTOR * n // N_EXPERTS)
    logits = h.astype(jnp.float32) @ w_router.astype(jnp.float32) + b_router.astype(jnp.float32)
    aff = jax.nn.softmax(logits, axis=-1)
    gate, idx = lax.top_k(jnp.swapaxes(aff, 1, 2), cap)
    xs = jax.vmap(lambda hb, ib: hb[ib])(h, idx)
    a = jnp.einsum('becd,edf->becf', xs, w_gate)
    u = jnp.einsum('becd,edf->becf', xs, w_up)
    y = jnp.einsum('becf,efd->becd', jax.nn.silu(a) * u, w_down) * gate[..., None].astype(h.dtype)
    return jax.vmap(lambda yb, ib: jnp.zeros((n, d), yb.dtype).at[ib.reshape(-1)].add(yb.reshape(-1, d)))(y, idx)


def setup_inputs(seed: int = 0) -> dict:
    key = jax.random.key(seed)
    ks = jax.random.split(key, 22)
    f32 = jnp.float32

    def nrm(k, shape, scale):
        return jax.random.normal(k, shape, f32) * scale

    decay_base = jnp.log(2.0 ** (5.0 + jnp.arange(RET_HEADS, dtype=f32)) - 1.0)
    return {
        "x": nrm(ks[0], (BATCH, SEQ, D_MODEL), 1.0),
        "c": nrm(ks[1], (BATCH, D_MODEL), 1.0),
        "ctx": nrm(ks[2], (BATCH, CTX_LEN, D_MODEL), 1.0),
        "c_ctx": nrm(ks[3], (D_MODEL,), 1.0),
        "w_ada": nrm(ks[4], (DEPTH, D_MODEL, 6 * D_MODEL), 0.5 * D_MODEL ** -0.5),
        "b_ada": nrm(ks[5], (DEPTH, 6 * D_MODEL), 0.02),
        "pre_mix_g": 1.0 + nrm(ks[6], (DEPTH, D_MODEL), 0.05),
        "post_mix_g": 1.0 + nrm(ks[7], (DEPTH, D_MODEL), 0.05),
        "pre_ffn_g": 1.0 + nrm(ks[8], (DEPTH, D_MODEL), 0.05),
        "post_ffn_g": 1.0 + nrm(ks[9], (DEPTH, D_MODEL), 0.05),
        "w_in": nrm(ks[10], (DEPTH, D_MODEL, IN_COLS), D_MODEL ** -0.5),
        "conv_w": nrm(ks[11], (DEPTH, 3, CONV_WIDTH), 3.0 ** -0.5),
        "conv_b": nrm(ks[12], (DEPTH, CONV_WIDTH), 0.02),
        "ret_decay_logit": decay_base[None, None, :] + nrm(ks[13], (DEPTH, 2, RET_HEADS), 0.1),
        "ret_norm_g": 1.0 + nrm(ks[14], (DEPTH, RET_WIDTH), 0.05),
        "w_out": nrm(ks[15], (DEPTH, MIX_WIDTH, D_MODEL), MIX_WIDTH ** -0.5),
        "w_router": nrm(ks[16], (DEPTH, D_MODEL, N_EXPERTS), D_MODEL ** -0.5),
        "b_router": nrm(ks[17], (DEPTH, N_EXPERTS), 0.01),
        "w_gate": nrm(ks[18], (DEPTH, N_EXPERTS, D_MODEL, D_EXPERT), D_MODEL ** -0.5),
        "w_up": nrm(ks[19], (DEPTH, N_EXPERTS, D_MODEL, D_EXPERT), D_MODEL ** -0.5),
        "w_down": nrm(ks[20], (DEPTH, N_EXPERTS, D_EXPERT, D_MODEL), D_EXPERT ** -0.5),
    }


def reference(x, c, ctx, c_ctx, w_ada, b_ada, pre_mix_g, post_mix_g, pre_ffn_g, post_ffn_g,
              w_in, conv_w, conv_b, ret_decay_logit, ret_norm_g, w_out,
              w_router, b_router, w_gate, w_up, w_down):
    n_lat = x.shape[1]
    ctx_len = ctx.shape[1]
    rows = n_lat // GRID_W
    pos_ctx = jnp.arange(ctx_len, dtype=jnp.float32)
    pos_lat = ctx_len + jnp.arange(n_lat, dtype=jnp.float32)
    bsz = x.shape[0]
    zero_state = jnp.zeros((bsz, RET_HEADS, RET_HEAD_DIM, RET_HEAD_DIM), jnp.float32)

    for i in range(DEPTH):
        last = i == DEPTH - 1
        sh1, sc1, g1, sh2, sc2, g2 = _modulation(c, w_ada[i], b_ada[i])
        csh1, csc1, cg1, csh2, csc2, cg2 = _modulation(c_ctx[None, :], w_ada[i], b_ada[i])
        log_gf = jax.nn.log_sigmoid(ret_decay_logit[i, 0].astype(jnp.float32))
        log_gb = jax.nn.log_sigmoid(ret_decay_logit[i, 1].astype(jnp.float32))

        hc = _rms_norm(ctx, pre_mix_g[i]) * (1.0 + csc1) + csh1
        c_b, c_c, c_x, c_q, c_k, c_v, c_g = jnp.split(hc @ w_in[i], SPLIT_IDX, axis=-1)
        qc, kc, vc = _ret_qkv(c_q, c_k, c_v, pos_ctx)
        if last:
            s_f, s_b = _context_states(kc, vc, log_gf, log_gb)
            ctx_next = ctx
        else:
            yc, s_f, s_b = _bidir_retention(qc, kc, vc, log_gf, log_gb, zero_state, zero_state)
            mix_c = jnp.concatenate([
                _conv_branch(c_b, c_c, c_x, conv_w[i], conv_b[i], 1, ctx_len),
                _ret_out(yc, c_g, ret_norm_g[i])], axis=-1) @ w_out[i]
            ctx_mid = ctx + cg1 * _rms_norm(mix_c, post_mix_g[i])
            hc2 = _rms_norm(ctx_mid, pre_ffn_g[i]) * (1.0 + csc2) + csh2
            ffn_c = _expert_choice_ffn(hc2, w_router[i], b_router[i], w_gate[i], w_up[i], w_down[i])
            ctx_next = ctx_mid + cg2 * _rms_norm(ffn_c, post_ffn_g[i])

        hx = _rms_norm(x, pre_mix_g[i]) * (1.0 + sc1) + sh1
        x_b, x_c, x_x, x_q, x_k, x_v, x_g = jnp.split(hx @ w_in[i], SPLIT_IDX, axis=-1)
        qx, kx, vx = _ret_qkv(x_q, x_k, x_v, pos_lat)
        yx, _, _ = _bidir_retention(qx, kx, vx, log_gf, log_gb, s_f, s_b)
        mix_x = jnp.concatenate([
            _conv_branch(x_b, x_c, x_x, conv_w[i], conv_b[i], rows, GRID_W),
            _ret_out(yx, x_g, ret_norm_g[i])], axis=-1) @ w_out[i]
        x = x + g1 * _rms_norm(mix_x, post_mix_g[i])
        hx2 = _rms_norm(x, pre_ffn_g[i]) * (1.0 + sc2) + sh2
        ffn_x = _expert_choice_ffn(hx2, w_router[i], b_router[i], w_gate[i], w_up[i], w_down[i])
        x = x + g2 * _rms_norm(ffn_x, post_ffn_g[i])
        ctx = ctx_next
    return x
```

```python
import numpy as np
import ml_dtypes
import concourse.bass as bass
import concourse.mybir as mybir
from concourse.bass_utils import run_bass_kernel_spmd
from contextlib import ExitStack

F32 = mybir.dt.float32
BF16 = mybir.dt.bfloat16
ALU = mybir.AluOpType
AF = mybir.ActivationFunctionType
AX = mybir.AxisListType

ENG = ('pe', 'act', 'dve', 'pool', 'sp')
D = 1024
TPC = 2048
NCH = 16
NE = 16
CAPG = 192
NG = 2
NS = NG * CAPG
KCAP = 1024.0
NBIS = 26
DBG = {}


class Buf:
    __slots__ = ('name', 'w', 'r', 'dsem', 'dcnt', 'const', 'psum')

    def __init__(self, name, psum=False):
        self.name = name
        self.psum = psum
        self.w = None
        self.r = {}
        self.dsem = None
        self.dcnt = 0
        self.const = False


class Prog:
    def __init__(self, nc, es):
        self.nc = nc
        self.es = es
        self.sem = {e: es.enter_context(nc.semaphore('s_' + e)) for e in ENG}
        self.cnt = {e: 0 for e in ENG}
        self.stream = {e: [] for e in ENG}
        self.known = {e: {} for e in ENG}
        self.dbufs = []
        self.frozen = False

    def _waits(self, eng, reads, writes):
        need = {}

        def add(tok):
            if tok is None:
                return
            sem, val, teng = tok
            if teng == 'pe' and eng == 'pe':
                return
            key = id(sem)
            if key not in need or need[key][1] < val:
                need[key] = (sem, val)

        for b in reads:
            add(b.w)
            if b.psum:
                for t in b.r.values():
                    if t[2] != eng:
                        add(t)
        for b in writes:
            add(b.w)
            for t in b.r.values():
                add(t)
        out = []
        kn = self.known[eng]
        for key, (sem, val) in need.items():
            if kn.get(key, 0) >= val:
                continue
            kn[key] = val
            out.append((sem, val))
        return out

    def _record(self, tok, reads, writes):
        key = id(tok[0])
        for b in reads:
            if not b.const:
                b.r[key] = tok
        for b in writes:
            b.w = tok
            b.r = {}

    def op(self, eng, fn, reads=(), writes=()):
        if self.frozen:
            return None
        waits = self._waits(eng, reads, writes)
        self.cnt[eng] += 1
        tok = (self.sem[eng], self.cnt[eng], eng)
        self._record(tok, reads, writes)
        self.stream[eng].append((waits, fn, self.sem[eng], 1))
        return tok

    def dma(self, q, fn, reads=(), writes=(), sembuf=None, inc=16):
        if self.frozen:
            return None
        waits = self._waits(q, reads, writes)
        sb = sembuf or (writes[0] if writes else reads[0])
        if sb.dsem is None:
            sb.dsem = self.es.enter_context(self.nc.semaphore('d_' + sb.name))
            self.dbufs.append(sb)
        sb.dcnt += inc
        tok = (sb.dsem, sb.dcnt, 'dma')
        self._record(tok, reads, writes)
        self.stream[q].append((waits, fn, sb.dsem, inc))
        return tok

    def wait_all(self, eng, toks):
        waits = []
        kn = self.known[eng]
        for (sem, val, _) in toks:
            if kn.get(id(sem), 0) >= val:
                continue
            kn[id(sem)] = val
            waits.append((sem, val))
        self.stream[eng].append((waits, None, None, 0))

    def barrier(self):
        if self.frozen:
            return
        toks = [(self.sem[e], self.cnt[e], e) for e in ENG if self.cnt[e] > 0]
        toks += [(b.dsem, b.dcnt, 'dma') for b in self.dbufs]
        for e in ENG:
            self.wait_all(e, toks)

    def emit(self, block):
        streams = self.stream

        def run(e, name):
            for waits, fn, sem, inc in streams[name]:
                for s, v in waits:
                    e.wait_ge(s, v)
                if fn is not None:
                    ins = fn(e)
                    ins.then_inc(sem, inc)

        @block.tensor
        def _(e):
            run(e, 'pe')

        @block.scalar
        def _(e):
            run(e, 'act')

        @block.vector
        def _(e):
            run(e, 'dve')

        @block.gpsimd
        def _(e):
            run(e, 'pool')

        @block.sync
        def _(e):
            run(e, 'sp')


class Arena:
    def __init__(self, big):
        self.big = big

    def at(self, off_kib, free_shape, dt, parts=128):
        n = int(np.prod(free_shape))
        nb = n * (2 if dt == BF16 else 4)
        n32 = (nb + 3) // 4
        off = int(off_kib * 256)
        ap = self.big[0:parts, off:off + n32]
        if dt != F32:
            ap = ap.bitcast(dt)
        if len(free_shape) == 2:
            ap = ap.rearrange("p (a b) -> p a b", a=free_shape[0], b=free_shape[1])
        elif len(free_shape) == 3:
            ap = ap.rearrange("p (a b c) -> p a b c", a=free_shape[0], b=free_shape[1], c=free_shape[2])
        return ap


class _Stop(Exception):
    pass


def build(dbg=(), stop=99):
    nc = bass.Bass("TRN2", target_bir_lowering=False)

    def din(name, shape, dt=F32):
        return nc.dram_tensor(name, list(shape), dt, kind="ExternalInput").ap()

    x_d = din("x", [TPC, D])
    ctx_d = din("ctx", [256, D])
    cvec_d = din("cvec", [128, 8, 2])
    wada_d = din("w_ada", [D, 6 * D])
    bada_d = din("b_ada2", [2, 6 * D])
    pmg_d = din("pmg", [128, 8])
    postmix_d = din("postmix_row", [128, D])
    preffn_d = din("preffn_row", [128, D])
    postffn_d = din("postffn_row", [128, D])
    win_d = din("w_in", [D, 3584])
    convw_d = din("convw", [128, 4, 3])
    convb_d = din("convb", [128, 4])
    dec_d = din("decay", [128, 8])
    retg_d = din("retg_row", [128, 512])
    wout_d = din("w_out", [D, D])
    wr_d = din("w_router", [128, 8, 16])
    br_d = din("brouter_row", [128, 16])
    ned = NE if stop > 8 else 1
    wg_d = din("w_gate", [ned, D, 2 * D])
    wu_d = din("w_up", [ned, D, 2 * D])
    wd_d = din("w_down", [ned, 2 * D, D])
    rotq_d = din("rotq", [128, 2, NCH, 64])
    rotk_d = din("rotk", [128, 2, NCH, 64])
    rotc_d = din("rotc", [128, 2, 2, 64])
    cst_d = din("cst", [128, 1600])
    identb_d = din("identb", [128, 128], BF16)
    selb_d = din("selb", [16, NE, 128], BF16)
    out_d = nc.dram_tensor("out", [TPC, D], F32, kind="ExternalOutput").ap()
    taps = {}
    for name, shape in dbg:
        taps[name] = nc.dram_tensor("dbg_" + name, list(shape), F32, kind="ExternalOutput").ap()

    xm_scr = nc.dram_tensor("xm_scr", [TPC, D], F32)
    hx2_scr = nc.dram_tensor("hx2_scr", [TPC, D], BF16)
    ag1_in = nc.dram_tensor("ag1_in", [128, 1024], F32)
    ag1_out = nc.dram_tensor("ag1_out", [512, 1024], F32)
    ag2_in = nc.dram_tensor("ag2_in", [16, TPC], F32)
    ag2_out = nc.dram_tensor("ag2_out", [64, TPC], F32)

    with ExitStack() as es:
        P = Prog(nc, es)
        big = es.enter_context(nc.sbuf_tensor("big", [128, 204 * 256], F32))
        A = Arena(big)
        psb = [es.enter_context(nc.psum_tensor(f"psb{i}", [128, 1024], F32)) for i in range(4)]
        BPS = [Buf(f"ps{i}", psum=True) for i in range(8)]

        def bank(i):
            return psb[i // 2][:, (i % 2) * 512:(i % 2) * 512 + 512]

        def bank_bf(i):
            return bank(i).bitcast(BF16)

        out_toks = []

        def ckpt(n):
            if stop == n:
                P.frozen = True


        def tap(name, src_ap, buf, q='sp', orr=None, **kw):
            if name in taps:
                dst = taps[name] if orr is None else taps[name].rearrange(orr, **kw)
                out_toks.append(P.dma(q, lambda e: e.dma_start(out=dst, in_=src_ap), reads=[buf], writes=[Buf("tap_" + name)], sembuf=Buf("tsem_" + name)))

        CST = A.at(0, [1600], F32)
        Bcst = Buf("cst"); Bcst.const = True
        c_iota = CST[:, 0:192]
        c_relF = CST[:, 192:320]
        c_mskF = CST[:, 320:448]
        c_relB = CST[:, 448:576]
        c_mskB = CST[:, 576:704]
        c_i1 = CST[:, 704:832]
        c_i128 = CST[:, 832:960]
        c_identf = CST[:, 960:1088]
        c_p127 = CST[:, 1088:1089]
        c_p = CST[:, 1089:1090]
        c_ctxf = CST[:, 1090:1092]
        c_ctxb = CST[:, 1092:1094]
        c_scv = CST[:, 1094:1098]
        c_grp = CST[:, 1100:1228]
        c_sel = CST[:, 1228:1244]
        c_c128f = CST[:, 1244:1260]
        c_c128b = CST[:, 1260:1276]
        c_xexp = CST[:, 1276:1292]
        c_ones = CST[:, 1300:1428]
        c_e01 = CST[:, 1428:1430]
        c_eps = CST[:, 1430:1431]
        c_sel0 = CST[0:2, 1442:1570]
        IDB = A.at(6.25, [128], BF16)
        Bidb = Buf("idb"); Bidb.const = True
        SELB = A.at(6.5, [NE, 128], BF16, parts=16)
        Bselb = Buf("selb"); Bselb.const = True
        SMALL = A.at(10.5, [384], F32)
        Bsm = Buf("small")
        LG = SMALL[:, 0:8]
        A1 = SMALL[:, 8:16]; SH1 = SMALL[:, 16:24]; A1C = SMALL[:, 24:32]; SH1C = SMALL[:, 32:40]
        ZF = SMALL[:, 40:44]; ZB = SMALL[:, 44:48]
        CZF = SMALL[:, 48:56]; CZB = SMALL[:, 56:64]
        CF = SMALL[:, 64:128]; CB = SMALL[:, 128:192]
        G128 = SMALL[:, 192:200]
        XCO = SMALL[:, 200:240]
        PMG = SMALL[:, 240:248]
        CW = SMALL[:, 248:260]
        CBI = SMALL[:, 260:264]
        BRR = SMALL[:, 264:280]
        THR = SMALL[:, 280:281]; MID = SMALL[:, 281:282]; CNT = SMALL[:, 282:283]; TT = SMALL[:, 283:284]
        THREM = SMALL[:, 284:285]
        STAT = SMALL[:, 288:320]
        DECL = SMALL[:, 320:328]
        AFF = A.at(12, [NCH, NE], F32)
        POST = A.at(13, [NCH, NE], F32)
        GA = A.at(14, [NCH, NE], F32)
        Baff, Bpost, Bga = Buf("aff"), Buf("post"), Buf("ga")
        POSM = A.at(15, [TPC], BF16, parts=16)
        Bposm = Buf("posm")
        LF = A.at(19, [4, 128], F32)
        LB = A.at(21, [4, 128], F32)
        Blf, Blb = Buf("lf"), Buf("lb")
        KT = A.at(24, [4, TPC], BF16)
        V = A.at(40, [NCH, 512], BF16)
        UF = A.at(56, [NCH, 512], BF16)
        UB = A.at(72, [NCH, 512], BF16)
        CONVT = A.at(88, [4, TPC], BF16)
        Bkt = [Buf(f"kt{c}") for c in range(NCH)]
        Bv = [Buf(f"v{c}") for c in range(NCH)]
        Buf_ = [Buf(f"uf{c}") for c in range(NCH)]
        Bub = [Buf(f"ub{c}") for c in range(NCH)]
        Bconvt = [Buf(f"convt{g}") for g in range(4)]
        WA = A.at(104, [8, 512], BF16)
        WB_ = A.at(112, [8, 512], BF16)
        WC = A.at(120, [8, 512], BF16)
        WD = A.at(128, [8, 512], BF16)
        WE = A.at(136, [8, 512], BF16)
        Bw = [Buf(f"w{i}") for i in range(5)]
        WSL = [WA, WB_, WC, WD, WE]
        ROT = A.at(144, [2, NCH, 64], F32)
        Brot = Buf("rot")
        ROTC = A.at(152, [2, 2, 64], F32)
        Brotc = Buf("rotc")
        DT = A.at(153, [4, 128], F32)
        XF = A.at(155, [4, 128], F32)
        XB = A.at(157, [4, 128], F32)
        Btab = Buf("tab")
        ROWS = A.at(159, [3, D], F32)
        Brows = Buf("rows")
        HXTG = A.at(159, [8, 512], BF16)
        Bhxtg = Buf("hxtg")
        NGR = A.at(171, [512], F32)
        Bngr = Buf("ngr")
        T0 = 173
        XC = [A.at(T0, [D], F32), A.at(T0 + 4, [D], F32)]
        Bxc = [Buf("xc0"), Buf("xc1")]
        XS = A.at(T0 + 8, [D], BF16)
        Bxs = Buf("xs")
        HXTC = A.at(T0 + 10, [8, 128], BF16)
        Bhxtc = Buf("hxtc")
        TMPA = A.at(T0 + 12, [4, 64], F32)
        TMPB = A.at(T0 + 13, [4, 64], F32)
        Btmp = Buf("tmpab")
        KR = A.at(T0 + 14, [512], BF16)
        Bkr = Buf("kr")
        KFs = A.at(T0 + 15, [512], BF16)
        KBs = A.at(T0 + 16, [512], BF16)
        Bkf, Bkb = Buf("kf"), Buf("kb")
        S1 = A.at(T0 + 17, [512], F32)
        S2 = A.at(T0 + 19, [512], F32)
        S3 = A.at(T0 + 21, [512], F32)
        Bs1, Bs2, Bs3 = Buf("s1"), Buf("s2"), Buf("s3")
        S4 = A.at(T0 + 23, [D], F32)
        Bs4 = Buf("s4")
        S5 = A.at(T0 + 27, [D], F32)
        Bs5 = Buf("s5")

        P.dma('sp', lambda e: e.dma_start(out=CST, in_=cst_d), writes=[Bcst])
        P.dma('sp', lambda e: e.dma_start(out=IDB, in_=identb_d), writes=[Bidb])
        P.dma('sp', lambda e: e.dma_start(out=SELB, in_=selb_d), writes=[Bselb])
        P.dma('sp', lambda e: e.dma_start(out=DECL, in_=dec_d), writes=[Bsm])
        P.dma('sp', lambda e: e.dma_start(out=PMG, in_=pmg_d), writes=[Bsm])
        P.dma('sp', lambda e: e.dma_start(out=CW, in_=convw_d.rearrange("p a b -> p (a b)")), writes=[Bsm])
        P.dma('sp', lambda e: e.dma_start(out=CBI, in_=convb_d), writes=[Bsm])
        P.dma('sp', lambda e: e.dma_start(out=BRR, in_=br_d), writes=[Bsm])
        P.dma('sp', lambda e: e.dma_start(out=ROT, in_=rotk_d), writes=[Brot])
        P.dma('sp', lambda e: e.dma_start(out=ROTC, in_=rotc_d), writes=[Brotc])
        P.dma('sp', lambda e: e.dma_start(out=NGR, in_=retg_d), writes=[Bngr])

        P.op('act', lambda e: e.activation(out=LG, in_=DECL, func=AF.Exp, scale=-1.0), reads=[Bsm], writes=[Bsm])
        P.op('dve', lambda e: e.tensor_scalar(out=LG, in0=LG, scalar1=1.0, scalar2=None, op0=ALU.add), reads=[Bsm], writes=[Bsm])
        P.op('act', lambda e: e.activation(out=LG, in_=LG, func=AF.Ln), reads=[Bsm], writes=[Bsm])
        P.op('dve', lambda e: e.tensor_scalar(out=LG, in0=LG, scalar1=-1.0, scalar2=None, op0=ALU.mult), reads=[Bsm], writes=[Bsm])
        for h in range(4):
            lf = LG[:, h:h + 1]
            lb = LG[:, 4 + h:5 + h]
            P.op('act', lambda e, h=h, lf=lf: e.activation(out=DT[:, h, :], in_=c_relF, func=AF.Exp, scale=lf), reads=[Bsm, Bcst], writes=[Btab])
            P.op('act', lambda e, h=h, lb=lb: e.activation(out=XF[:, h, :], in_=c_relB, func=AF.Exp, scale=lb), reads=[Bsm, Bcst], writes=[Btab])
            P.op('dve', lambda e, h=h: e.tensor_tensor(out=DT[:, h, :], in0=DT[:, h, :], in1=c_mskF, op=ALU.mult), reads=[Btab, Bcst], writes=[Btab])
            P.op('dve', lambda e, h=h: e.tensor_tensor(out=XF[:, h, :], in0=XF[:, h, :], in1=c_mskB, op=ALU.mult), reads=[Btab, Bcst], writes=[Btab])
            P.op('dve', lambda e, h=h: e.tensor_tensor(out=DT[:, h, :], in0=DT[:, h, :], in1=XF[:, h, :], op=ALU.add), reads=[Btab], writes=[Btab])
        for h in range(4):
            lf = LG[:, h:h + 1]
            lb = LG[:, 4 + h:5 + h]
            P.op('act', lambda e, h=h, lf=lf: e.activation(out=XF[:, h, :], in_=c_i1, func=AF.Exp, scale=lf), reads=[Bsm, Bcst, Btab], writes=[Btab])
            P.op('act', lambda e, h=h, lb=lb: e.activation(out=XB[:, h, :], in_=c_i128, func=AF.Exp, scale=lb), reads=[Bsm, Bcst], writes=[Btab])
            P.op('act', lambda e, h=h, lf=lf: e.activation(out=ZF[:, h:h + 1], in_=c_p127, func=AF.Exp, scale=lf), reads=[Bsm, Bcst], writes=[Bsm])
            P.op('act', lambda e, h=h, lb=lb: e.activation(out=ZB[:, h:h + 1], in_=c_p, func=AF.Exp, scale=lb), reads=[Bsm, Bcst], writes=[Bsm])
            for c in range(2):
                P.op('act', lambda e, h=h, c=c, lf=lf: e.activation(out=CZF[:, c * 4 + h:c * 4 + h + 1], in_=c_ctxf[:, c:c + 1], func=AF.Exp, scale=lf), reads=[Bsm, Bcst], writes=[Bsm])
                P.op('act', lambda e, h=h, c=c, lb=lb: e.activation(out=CZB[:, c * 4 + h:c * 4 + h + 1], in_=c_ctxb[:, c:c + 1], func=AF.Exp, scale=lb), reads=[Bsm, Bcst], writes=[Bsm])
            cfv = CF.rearrange("p (c h) -> p c h", h=4)
            cbv = CB.rearrange("p (c h) -> p c h", h=4)
            P.op('act', lambda e, h=h, lf=lf, cfv=cfv: e.activation(out=cfv[:, :, h], in_=c_c128f, func=AF.Exp, scale=lf), reads=[Bsm, Bcst], writes=[Bsm])
            P.op('act', lambda e, h=h, lb=lb, cbv=cbv: e.activation(out=cbv[:, :, h], in_=c_c128b, func=AF.Exp, scale=lb), reads=[Bsm, Bcst], writes=[Bsm])
            P.op('act', lambda e, h=h, lf=lf: e.activation(out=G128[:, h:h + 1], in_=c_ones[:, 0:1], func=AF.Exp, scale=lf), reads=[Bsm, Bcst], writes=[Bsm])
            P.op('act', lambda e, h=h, lb=lb: e.activation(out=G128[:, 4 + h:5 + h], in_=c_ones[:, 0:1], func=AF.Exp, scale=lb), reads=[Bsm, Bcst], writes=[Bsm])
            xco = XCO.rearrange("p (s h) -> p s h", h=4)
            P.op('act', lambda e, h=h, lf=lf, xco=xco: e.activation(out=xco[:, 0:4, h], in_=c_xexp[:, 0:4], func=AF.Exp, scale=lf), reads=[Bsm, Bcst], writes=[Bsm])
            P.op('act', lambda e, h=h, lb=lb, xco=xco: e.activation(out=xco[:, 4:8, h], in_=c_xexp[:, 4:8], func=AF.Exp, scale=lb), reads=[Bsm, Bcst], writes=[Bsm])
            P.op('act', lambda e, h=h, lf=lf, xco=xco: e.activation(out=xco[:, 8:9, h], in_=c_xexp[:, 8:9], func=AF.Exp, scale=lf), reads=[Bsm, Bcst], writes=[Bsm])
            P.op('act', lambda e, h=h, lb=lb, xco=xco: e.activation(out=xco[:, 9:10, h], in_=c_xexp[:, 9:10], func=AF.Exp, scale=lb), reads=[Bsm, Bcst], writes=[Bsm])
        for _ in range(7):
            P.op('dve', lambda e: e.tensor_tensor(out=G128, in0=G128, in1=G128, op=ALU.mult), reads=[Bsm], writes=[Bsm])
        xco = XCO.rearrange("p (s h) -> p s h", h=4)
        c_xmsk = CST[:, 1432:1442]
        P.op('dve', lambda e: e.tensor_tensor(out=xco, in0=xco, in1=c_xmsk.unsqueeze(2).to_broadcast([128, 10, 4]), op=ALU.mult), reads=[Bsm, Bcst], writes=[Bsm])

        ckpt(1)
        wslot = [0]

        def load_piece(src_ap, slot):
            P.dma('pool', lambda e: e.dma_start(out=WSL[slot], in_=src_ap.rearrange("(c p) f -> p c f", p=128)), writes=[Bw[slot]])

        CV = A.at(23, [8, 2], F32)
        ST_ = A.at(23.25, [8, 2], BF16)
        Bcv = Buf('cv')
        MROW = A.at(T0 + 27, [512], F32, parts=2)
        P.dma('sp', lambda e: e.dma_start(out=CV, in_=cvec_d), writes=[Bcv])
        P.op('act', lambda e: e.activation(out=ST_, in_=CV, func=AF.Silu), reads=[Bcv], writes=[Bcv])
        BADA = A.at(T0 + 17, [512], F32, parts=2)

        def mod_block(n, slot):
            load_piece(wada_d[:, n * 512:(n + 1) * 512], slot)
            P.dma('sp', lambda e: e.dma_start(out=BADA, in_=bada_d[:, n * 512:(n + 1) * 512]), writes=[Bs1])
            for dc in range(8):
                P.op('pe', lambda e, dc=dc: e.matmul(bank(0)[0:2, :], lhsT=ST_[:, dc, :], rhs=WSL[slot][:, dc, :], start=(dc == 0), stop=(dc == 7)),
                     reads=[Bcv, Bw[slot]], writes=[BPS[0]])
            P.op('dve', lambda e: e.tensor_tensor(out=MROW, in0=bank(0)[0:2, :], in1=BADA, op=ALU.add), reads=[BPS[0], Bs1], writes=[Bs5])

        def row_bcast(dst_ap, dst_buf, gain_src, plus_one):
            P.op('pe', lambda e: e.matmul(bank(1), lhsT=c_sel0, rhs=MROW, start=True, stop=True),
                 reads=[Bs5, Bcst], writes=[BPS[1]])
            if gain_src is None:
                P.op('act', lambda e: e.activation(out=dst_ap, in_=bank(1), func=AF.Copy), reads=[BPS[1]], writes=[dst_buf])
            elif plus_one:
                P.op('dve', lambda e: e.scalar_tensor_tensor(out=dst_ap, in0=bank(1), scalar=1.0, in1=gain_src, op0=ALU.add, op1=ALU.mult),
                     reads=[BPS[1], Bs2], writes=[dst_buf])
            else:
                P.op('dve', lambda e: e.tensor_tensor(out=dst_ap, in0=bank(1), in1=gain_src, op=ALU.mult), reads=[BPS[1], Bs2], writes=[dst_buf])

        def col_extract(n, dst_lat, dst_ctx, half):
            for cc in range(4):
                for r, dst in ((0, dst_lat), (1, dst_ctx)):
                    P.op('pe', lambda e, cc=cc, r=r: e.matmul(bank(2)[:, (r * 4 + cc):(r * 4 + cc) + 1], lhsT=MROW[0:2, cc * 128:(cc + 1) * 128],
                                                               rhs=c_e01[0:2, r:r + 1], start=True, stop=True),
                         reads=[Bs5, Bcst], writes=[BPS[2]])
            P.op('dve', lambda e: e.tensor_copy(out=dst_lat[:, half * 4:half * 4 + 4], in_=bank(2)[:, 0:4]), reads=[BPS[2]], writes=[Bsm])
            P.op('dve', lambda e: e.tensor_copy(out=dst_ctx[:, half * 4:half * 4 + 4], in_=bank(2)[:, 4:8]), reads=[BPS[2]], writes=[Bsm])

        for n in range(4):
            mod_block(n, n % 5)
            if n < 2:
                col_extract(n, SH1, SH1C, n)
            else:
                col_extract(n, A1, A1C, n - 2)
        for dst in (A1, A1C):
            P.op('dve', lambda e, dst=dst: e.scalar_tensor_tensor(out=dst, in0=dst, scalar=1.0, in1=PMG, op0=ALU.add, op1=ALU.mult), reads=[Bsm], writes=[Bsm])

        ckpt(2)
        P.barrier()
        load_piece(win_d[:, 1536 + 1 * 512:1536 + 2 * 512], 0)
        load_piece(win_d[:, 1536 + 2 * 512:1536 + 3 * 512], 1)
        load_piece(win_d[:, 512:1024], 2)
        load_piece(win_d[:, 1024:1536], 3)
        load_piece(win_d[:, 0:512], 4)

        STN = SMALL[:, 316:318]
        Bstn = Buf("stn")

        def load_x(src_d, row0, xbuf_i):
            xc, bxc = XC[xbuf_i], Bxc[xbuf_i]
            P.dma('sp', lambda e: e.dma_start(out=xc, in_=src_d[row0:row0 + 128, :]), writes=[bxc])

        def norm_a(xbuf_i):
            xc, bxc = XC[xbuf_i], Bxc[xbuf_i]
            P.op('act', lambda e: e.activation(out=XS, in_=xc, func=AF.Square, accum_out=STN[:, 0:1]), reads=[bxc], writes=[Bxs, Bstn])
            P.op('act', lambda e: e.activation(out=STN[:, 1:2], in_=STN[:, 0:1], func=AF.Sqrt, scale=1.0 / D, bias=c_eps), reads=[Bstn, Bcst], writes=[Bstn])

        def norm_b(xbuf_i):
            xc, bxc = XC[xbuf_i], Bxc[xbuf_i]
            P.op('dve', lambda e: e.reciprocal(out=STN[:, 1:2], in_=STN[:, 1:2]), reads=[Bstn], writes=[Bstn])
            P.op('act', lambda e: e.activation(out=XS, in_=xc, func=AF.Copy, scale=STN[:, 1:2]), reads=[bxc, Bstn], writes=[Bxs])
            tb = bank_bf(0).rearrange("p (c t) -> p c t", c=8)
            for dc in range(8):
                P.op('pe', lambda e, dc=dc: e.transpose(tb[:, dc, :], XS[:, dc * 128:(dc + 1) * 128], IDB), reads=[Bxs, Bidb], writes=[BPS[0]])

        def norm_compute(a_col, sh_col, xbuf_i, dst_ap, dst_buf):
            norm_a(xbuf_i)
            norm_b(xbuf_i)
            norm_c(a_col, sh_col, dst_ap, dst_buf)

        def norm_c(a_col, sh_col, dst_ap, dst_buf):
            tb = bank_bf(0).rearrange("p (c t) -> p c t", c=8)
            for dc in range(8):
                P.op('dve', lambda e, dc=dc: e.tensor_scalar(out=dst_ap[:, dc, :], in0=tb[:, dc, :], scalar1=a_col[:, dc:dc + 1], scalar2=sh_col[:, dc:dc + 1], op0=ALU.mult, op1=ALU.add),
                     reads=[BPS[0], Bsm], writes=[dst_buf])

        def norm_transpose(src_d, row0, a_col, sh_col, xbuf_i, dst_ap, dst_buf, keep_x=False):
            load_x(src_d, row0, xbuf_i)
            norm_compute(a_col, sh_col, xbuf_i, dst_ap, dst_buf)

        def rotary(ps_ap, cos_ap, sin_ap, dst_ap, dst_buf, ps_buf, tab_buf):
            pv = ps_ap.rearrange("p (h t d) -> p h t d", h=4, t=2)
            dv = dst_ap.rearrange("p (h t d) -> p h t d", h=4, t=2)
            cb = cos_ap.unsqueeze(1).to_broadcast([128, 4, 64])
            sb = sin_ap.unsqueeze(1).to_broadcast([128, 4, 64])
            P.op('dve', lambda e: e.tensor_tensor(out=TMPA, in0=pv[:, :, 0, :], in1=cb, op=ALU.mult), reads=[ps_buf, tab_buf], writes=[Btmp])
            P.op('dve', lambda e: e.tensor_tensor(out=TMPB, in0=pv[:, :, 1, :], in1=sb, op=ALU.mult), reads=[ps_buf, tab_buf], writes=[Btmp])
            P.op('dve', lambda e: e.tensor_tensor(out=dv[:, :, 0, :], in0=TMPA, in1=TMPB, op=ALU.subtract), reads=[Btmp], writes=[dst_buf])
            P.op('dve', lambda e: e.tensor_tensor(out=TMPA, in0=pv[:, :, 0, :], in1=sb, op=ALU.mult), reads=[ps_buf, tab_buf], writes=[Btmp])
            P.op('dve', lambda e: e.tensor_tensor(out=TMPB, in0=pv[:, :, 1, :], in1=cb, op=ALU.mult), reads=[ps_buf, tab_buf], writes=[Btmp])
            P.op('dve', lambda e: e.tensor_tensor(out=dv[:, :, 1, :], in0=TMPA, in1=TMPB, op=ALU.add), reads=[Btmp], writes=[dst_buf])

        def kv_chunk(hx_ap, hx_buf, cos_ap, sin_ap, tab_buf, zf_ap, zb_ap, v_dst, v_buf, ck=False, mid=None):
            for dc in range(8):
                P.op('pe', lambda e, dc=dc: e.matmul(bank(1), lhsT=hx_ap[:, dc, :], rhs=WSL[0][:, dc, :], start=(dc == 0), stop=(dc == 7)),
                     reads=[hx_buf, Bw[0]], writes=[BPS[1]])
            for dc in range(8):
                P.op('pe', lambda e, dc=dc: e.matmul(bank(2), lhsT=hx_ap[:, dc, :], rhs=WSL[1][:, dc, :], start=(dc == 0), stop=(dc == 7)),
                     reads=[hx_buf, Bw[1]], writes=[BPS[2]])
            if mid is not None:
                mid()
            P.op('act', lambda e: e.activation(out=v_dst, in_=bank(2), func=AF.Copy), reads=[BPS[2]], writes=[v_buf])
            if ck:
                ckpt(22)
            rotary(bank(1), cos_ap, sin_ap, KR, Bkr, BPS[1], tab_buf)
            if ck:
                ckpt(23)
            for h in range(4):
                hs = slice(h * 128, (h + 1) * 128)
                P.op('dve', lambda e, h=h, hs=hs: e.tensor_scalar(out=KFs[:, hs], in0=KR[:, hs], scalar1=zf_ap[:, h:h + 1], scalar2=None, op0=ALU.mult), reads=[Bkr, Bsm], writes=[Bkf])
                P.op('dve', lambda e, h=h, hs=hs: e.tensor_scalar(out=KBs[:, hs], in0=KR[:, hs], scalar1=zb_ap[:, h:h + 1], scalar2=None, op0=ALU.mult), reads=[Bkr, Bsm], writes=[Bkb])

        VC = S3.bitcast(BF16)[:, 0:512]
        SCF = A.at(T0 + 27, [4, 128], F32)
        SCB = A.at(T0 + 29, [4, 128], F32)
        xco = XCO.rearrange("p (s h) -> p s h", h=4)
        s1v_ = S1.rearrange("p (h d) -> p h d", h=4)
        for c in range(2):
            norm_transpose(ctx_d, c * 128, A1C, SH1C, c % 2, HXTC, Bhxtc)
            if c == 0:
                ckpt(21)
            kv_chunk(HXTC, Bhxtc, ROTC[:, 0, c, :], ROTC[:, 1, c, :], Brotc, CZF[:, c * 4:c * 4 + 4], CZB[:, c * 4:c * 4 + 4], VC, Bs3, ck=(c == 0))
            for h in range(4):
                hs = slice(h * 128, (h + 1) * 128)
                P.op('pe', lambda e, hs=hs: e.matmul(bank(4)[:, hs], lhsT=KFs[:, hs], rhs=VC[:, hs], start=True, stop=True),
                     reads=[Bkf, Bs3], writes=[BPS[4]])
            for h in range(4):
                hs = slice(h * 128, (h + 1) * 128)
                P.op('pe', lambda e, hs=hs: e.matmul(bank(5)[:, hs], lhsT=KBs[:, hs], rhs=VC[:, hs], start=True, stop=True),
                     reads=[Bkb, Bs3], writes=[BPS[5]])
            if c == 0:
                ckpt(25)
            for (dstv, bi, col) in ((SCF, 4, 8), (SCB, 5, 9)):
                for h in range(4):
                    hs = slice(h * 128, (h + 1) * 128)
                    if c == 0:
                        P.op('dve', lambda e, dstv=dstv, bi=bi, col=col, h=h, hs=hs: e.tensor_scalar(out=dstv[:, h, :], in0=bank(bi)[:, hs], scalar1=xco[:, col, h:h + 1], scalar2=None, op0=ALU.mult),
                             reads=[BPS[bi], Bsm], writes=[Bs5])
                    else:
                        P.op('dve', lambda e, dstv=dstv, bi=bi, col=col, h=h, hs=hs: e.scalar_tensor_tensor(out=dstv[:, h, :], in0=bank(bi)[:, hs], scalar=xco[:, col, h:h + 1], in1=dstv[:, h, :], op0=ALU.mult, op1=ALU.add),
                             reads=[BPS[bi], Bsm, Bs5], writes=[Bs5])
        tap("scf", SCF.rearrange("p h d -> p (h d)"), Bs5)

        ckpt(3)
        P.op('dve', lambda e: e.memset(LF, 0.0), writes=[Blf])
        P.op('dve', lambda e: e.memset(LB, 0.0), writes=[Blb])
        cfv = CF.rearrange("p (c h) -> p c h", h=4)
        cbv = CB.rearrange("p (c h) -> p c h", h=4)
        def pre_norm(c):
            norm_compute(A1, SH1, c % 2, HXTG[:, :, (c % 4) * 128:(c % 4) * 128 + 128], Bhxtg)

        load_x(x_d, 0, 0)
        pre_norm(0)
        for c in range(NCH):
            g = c // 4
            if c + 1 < NCH:
                load_x(x_d, (c + 1) * 128, (c + 1) % 2)
            hx = HXTG[:, :, (c % 4) * 128:(c % 4) * 128 + 128]
            kv_chunk(hx, Bhxtg, ROT[:, 0, c, :], ROT[:, 1, c, :], Brot, ZF, ZB, V[:, c, :], Bv[c],
                     mid=(lambda c=c: pre_norm(c + 1)) if (c + 1 < NCH and c % 4 != 3) else None)
            if c == 0:
                ckpt(32)
            tb = bank_bf(3).rearrange("p (h t) -> p h t", h=8)
            for h in range(4):
                P.op('pe', lambda e, h=h: e.transpose(tb[:, h, :], KR[:, h * 128:(h + 1) * 128], IDB), reads=[Bkr, Bidb], writes=[BPS[3]])
            P.op('act', lambda e, c=c: e.activation(out=KT[:, :, c * 128:(c + 1) * 128], in_=tb[:, 0:4, :], func=AF.Copy), reads=[BPS[3]], writes=[Bkt[c]])
            if c == 0:
                ckpt(33)
            for h in range(4):
                hs = slice(h * 128, (h + 1) * 128)
                P.op('pe', lambda e, hs=hs, c=c: e.matmul(bank(4)[:, hs], lhsT=KFs[:, hs], rhs=V[:, c, hs], start=True, stop=True),
                     reads=[Bkf, Bv[c]], writes=[BPS[4]])
            for h in range(4):
                hs = slice(h * 128, (h + 1) * 128)
                P.op('pe', lambda e, hs=hs, c=c: e.matmul(bank(5)[:, hs], lhsT=KBs[:, hs], rhs=V[:, c, hs], start=True, stop=True),
                     reads=[Bkb, Bv[c]], writes=[BPS[5]])
            P.op('act', lambda e, c=c: e.activation(out=UF[:, c, :], in_=bank(4), func=AF.Copy), reads=[BPS[4]], writes=[Buf_[c]])
            P.op('act', lambda e, c=c: e.activation(out=UB[:, c, :], in_=bank(5), func=AF.Copy), reads=[BPS[5]], writes=[Bub[c]])
            if c == 0:
                ckpt(34)
            for h in range(4):
                hs = slice(h * 128, (h + 1) * 128)
                P.op('dve', lambda e, c=c, h=h, hs=hs: e.scalar_tensor_tensor(out=LF[:, h, :], in0=bank(4)[:, hs], scalar=cfv[:, c, h:h + 1], in1=LF[:, h, :], op0=ALU.mult, op1=ALU.add),
                     reads=[BPS[4], Bsm, Blf], writes=[Blf])
                P.op('dve', lambda e, c=c, h=h, hs=hs: e.scalar_tensor_tensor(out=LB[:, h, :], in0=bank(5)[:, hs], scalar=cbv[:, c, h:h + 1], in1=LB[:, h, :], op0=ALU.mult, op1=ALU.add),
                     reads=[BPS[5], Bsm, Blb], writes=[Blb])
            if c == 0:
                ckpt(341)
            if c == 1:
                ckpt(342)
            if c == 2:
                ckpt(35)
            if c % 4 == 3:
                ckpt(36) if c == 3 else None
                for j in range(4):
                    js = slice(j * 128, (j + 1) * 128)
                    for bi, slot in ((6, 2), (7, 3), (3, 4)):
                        for dc in range(8):
                            P.op('pe', lambda e, dc=dc, bi=bi, slot=slot, js=js: e.matmul(bank(bi), lhsT=WSL[slot][:, dc, js], rhs=HXTG[:, dc, :], start=(dc == 0), stop=(dc == 7)),
                                 reads=[Bhxtg, Bw[slot]], writes=[BPS[bi]])
                    P.op('act', lambda e: e.activation(out=S2, in_=bank(6), func=AF.Copy), reads=[BPS[6]], writes=[Bs2])
                    P.op('dve', lambda e: e.tensor_tensor(out=S2, in0=S2, in1=bank(7), op=ALU.mult), reads=[BPS[7], Bs2], writes=[Bs2])
                    P.op('dve', lambda e, j=j: e.tensor_scalar(out=S3, in0=S2, scalar1=CW[:, j * 3 + 1:j * 3 + 2], scalar2=CBI[:, j:j + 1], op0=ALU.mult, op1=ALU.add),
                         reads=[Bs2, Bsm], writes=[Bs3])
                    u3 = S2.rearrange("p (r w) -> p r w", w=64)
                    y3 = S3.rearrange("p (r w) -> p r w", w=64)
                    P.op('dve', lambda e, j=j: e.scalar_tensor_tensor(out=y3[:, :, 1:64], in0=u3[:, :, 0:63], scalar=CW[:, j * 3:j * 3 + 1], in1=y3[:, :, 1:64], op0=ALU.mult, op1=ALU.add),
                         reads=[Bs2, Bs3, Bsm], writes=[Bs3])
                    P.op('dve', lambda e, j=j: e.scalar_tensor_tensor(out=y3[:, :, 0:63], in0=u3[:, :, 1:64], scalar=CW[:, j * 3 + 2:j * 3 + 3], in1=y3[:, :, 0:63], op0=ALU.mult, op1=ALU.add),
                         reads=[Bs2, Bs3, Bsm], writes=[Bs3])
                    P.op('dve', lambda e, j=j, g=g: e.tensor_tensor(out=CONVT[:, j, g * 512:(g + 1) * 512], in0=S3, in1=bank(3), op=ALU.mult),
                         reads=[Bs3, BPS[3]], writes=[Bconvt[g]])
                if c + 1 < NCH:
                    pre_norm(c + 1)

        ckpt(4)
        Bag1i, Bag1o = Buf("ag1i"), Buf("ag1o")
        P.dma('sp', lambda e: e.dma_start(out=ag1_in.ap()[:, 0:512], in_=LF.rearrange("p h d -> p (h d)")), reads=[Blf], writes=[Bag1i])
        P.dma('sp', lambda e: e.dma_start(out=ag1_in.ap()[:, 512:1024], in_=LB.rearrange("p h d -> p (h d)")), reads=[Blb], writes=[Bag1i])
        P.dma('pool', lambda e: e.collective_compute("AllGather", ALU.bypass, replica_groups=[[0, 1, 2, 3], [4, 5, 6, 7]],
                                                     ins=[ag1_in.ap().opt()], outs=[ag1_out.ap().opt()]),
              reads=[Bag1i], writes=[Bag1o], inc=1)
        G1 = ROWS[:, 0, :]; A2 = ROWS[:, 1, :]; SH2 = ROWS[:, 2, :]
        W5 = A.at(T0, [8, 512], BF16)
        BADA_b = A.at(T0 + 10, [512], F32, parts=2)
        MROW_b = A.at(T0 + 12, [512], F32, parts=2)
        GAIN_b = A.at(T0 + 14, [512], F32)
        Bw5s = Buf("w5s")
        mods = []
        for (blk0, dst, gsrc, p1) in ((4, G1, postmix_d, False), (8, A2, preffn_d, True), (6, SH2, None, False)):
            for hf in range(2):
                mods.append((blk0 + hf, dst, gsrc, p1, hf))

        def emit_mod(i):
            n, dst, gsrc, p1, hf = mods[i]
            if i % 2 == 0:
                wsl, bw = WSL[4], [Bw[4]]
            else:
                wsl, bw = W5, [Bxc[0], Bxc[1]]
            P.dma('pool', lambda e: e.dma_start(out=wsl, in_=wada_d[:, n * 512:(n + 1) * 512].rearrange("(c p) f -> p c f", p=128)), writes=bw,
                  sembuf=(Bw[4] if i % 2 == 0 else Bw5s))
            P.dma('sp', lambda e: e.dma_start(out=BADA_b, in_=bada_d[:, n * 512:(n + 1) * 512]), writes=[Bhxtc])
            for dc in range(8):
                P.op('pe', lambda e, dc=dc: e.matmul(bank(0)[0:2, :], lhsT=ST_[:, dc, :], rhs=wsl[:, dc, :], start=(dc == 0), stop=(dc == 7)),
                     reads=[Bcv] + bw[:1], writes=[BPS[0]])
            P.op('dve', lambda e: e.tensor_tensor(out=MROW_b, in0=bank(0)[0:2, :], in1=BADA_b, op=ALU.add), reads=[BPS[0], Bhxtc], writes=[Btmp])
            if gsrc is not None:
                P.dma('sp', lambda e: e.dma_start(out=GAIN_b, in_=gsrc[:, hf * 512:(hf + 1) * 512]), writes=[Bkr, Bkf])
            P.op('pe', lambda e: e.matmul(bank(1), lhsT=c_sel0, rhs=MROW_b, start=True, stop=True), reads=[Btmp, Bcst], writes=[BPS[1]])
            d = dst[:, hf * 512:(hf + 1) * 512]
            if gsrc is None:
                P.op('act', lambda e: e.activation(out=d, in_=bank(1), func=AF.Copy), reads=[BPS[1]], writes=[Brows, Bhxtg])
            elif p1:
                P.op('dve', lambda e: e.scalar_tensor_tensor(out=d, in0=bank(1), scalar=1.0, in1=GAIN_b, op0=ALU.add, op1=ALU.mult),
                     reads=[BPS[1], Bkr, Bkf], writes=[Brows, Bhxtg])
            else:
                P.op('dve', lambda e: e.tensor_tensor(out=d, in0=bank(1), in1=GAIN_b, op=ALU.mult), reads=[BPS[1], Bkr, Bkf], writes=[Brows, Bhxtg])

        emit_mod(0)
        emit_mod(1)
        load_piece(win_d[:, 1536:2048], 0)
        load_piece(win_d[:, 1536 + 3 * 512:3584], 1)
        load_piece(wout_d[:, 0:512], 2)
        load_piece(wout_d[:, 512:1024], 3)
        P.dma('sp', lambda e: e.dma_start(out=ROT, in_=rotq_d), writes=[Brot])
        SINF = S1.rearrange("p (h d) -> p h d", h=4)
        SINB = S2.rearrange("p (h d) -> p h d", h=4)
        P.op('dve', lambda e: e.tensor_copy(out=SINF, in_=SCF), reads=[Bs5], writes=[Bs1])
        P.op('dve', lambda e: e.tensor_copy(out=SINB, in_=SCB), reads=[Bs5], writes=[Bs2])
        for s in range(4):
            P.dma('sp', lambda e, s=s: e.dma_start(out=S4, in_=ag1_out.ap()[s * 128:(s + 1) * 128, :]), reads=[Bag1o], writes=[Bs4])
            s4f = S4[:, 0:512].rearrange("p (h d) -> p h d", h=4)
            s4b = S4[:, 512:1024].rearrange("p (h d) -> p h d", h=4)
            for h in range(4):
                P.op('dve', lambda e, s=s, h=h, s4f=s4f: e.scalar_tensor_tensor(out=SINF[:, h, :], in0=s4f[:, h, :], scalar=xco[:, s, h:h + 1], in1=SINF[:, h, :], op0=ALU.mult, op1=ALU.add),
                     reads=[Bs4, Bsm, Bs1], writes=[Bs1])
                P.op('dve', lambda e, s=s, h=h, s4b=s4b: e.scalar_tensor_tensor(out=SINB[:, h, :], in0=s4b[:, h, :], scalar=xco[:, 4 + s, h:h + 1], in1=SINB[:, h, :], op0=ALU.mult, op1=ALU.add),
                     reads=[Bs4, Bsm, Bs2], writes=[Bs2])
        tap("sinf", S1, Bs1)
        tap("sinb", S2, Bs2)
        def v4(ap):
            return ap.rearrange("p (h d) -> p h d", h=4)
        Bs4a, Bs4b, Bs5a = Buf("s4a"), Buf("s4b"), Buf("s5a")
        FB = [(SINF, Bs1), (v4(S3), Bs3), (v4(S4[:, 0:512]), Bs4a)]
        BB = [(SINB, Bs2), (v4(S4[:, 512:1024]), Bs4b), (v4(S5[:, 0:512]), Bs5a)]
        first_extra = {id(Bs4a): [Bs4], id(Bs4b): [Bs4], id(Bs5a): [Bs5]}

        def scan_step(k, c, bufs, U, BU, goff):
            (cur, bcur), (nxt, bnxt) = bufs[k % 3], bufs[(k + 1) % 3]
            uc = v4(U[:, c, :])
            extra = first_extra.pop(id(bnxt), [])
            for h in range(4):
                P.op('dve', lambda e, h=h: e.scalar_tensor_tensor(out=nxt[:, h, :], in0=cur[:, h, :], scalar=G128[:, goff + h:goff + h + 1], in1=uc[:, h, :], op0=ALU.mult, op1=ALU.add),
                     reads=[bcur, Bsm, BU[c]], writes=[bnxt] + (extra if h == 0 else []))
            P.op('act', lambda e: e.activation(out=uc, in_=cur, func=AF.Copy), reads=[bcur], writes=[BU[c]])

        for k in range(NCH):
            scan_step(k, k, FB, UF, Buf_, 0)
            scan_step(k, NCH - 1 - k, BB, UB, Bub, 4)
            if k in (1, 5, 9, 13):
                emit_mod(2 + (k - 1) // 4)
        tap("rows", ROWS.rearrange("p a b -> p (a b)"), Brows)
        P.barrier()

        ckpt(6)
        Bxm, Bhx2 = Buf("xm_scr"), Buf("hx2_scr")
        WR = A.at(152, [8, 16], F32)
        Bwr = Buf("wr")
        P.dma('sp', lambda e: e.dma_start(out=WR, in_=wr_d), reads=[Brotc], writes=[Bwr])
        QT = A.at(T0 + 15, [4, 128], BF16)
        QFT = A.at(T0 + 16, [4, 128], BF16)
        QBT = A.at(T0 + 12, [4, 128], BF16)
        XSJ = A.at(136, [D], BF16)
        XSH = A.at(138, [D], BF16)
        Bxsj, Bxsh = Buf("xsj"), Buf("xsh")
        load_x(x_d, 0, 0)
        norm_compute(A1, SH1, 0, HXTC, Bhxtc)
        Bst = Buf("stat")
        DUM = STAT[:, 30:32]
        Bdum = Buf("dum")
        P.op('dve', lambda e: e.memset(DUM, 1.0), writes=[Bdum])

        def qg_proj():
            for dc in range(8):
                P.op('pe', lambda e, dc=dc: e.matmul(bank(4), lhsT=HXTC[:, dc, :], rhs=WSL[0][:, dc, :], start=(dc == 0), stop=(dc == 7)),
                     reads=[Bhxtc, Bw[0]], writes=[BPS[4]])
            for dc in range(8):
                P.op('pe', lambda e, dc=dc: e.matmul(bank(5), lhsT=HXTC[:, dc, :], rhs=WSL[1][:, dc, :], start=(dc == 0), stop=(dc == 7)),
                     reads=[Bhxtc, Bw[1]], writes=[BPS[5]])

        qg_proj()
        for c in range(NCH):
            cs = slice(c * 128, (c + 1) * 128)
            xi = c % 2
            w4 = [Bw[4]] if c == 0 else []
            if c + 1 < NCH:
                load_x(x_d, (c + 1) * 128, (c + 1) % 2)
            rotary(bank(4), ROT[:, 0, c, :], ROT[:, 1, c, :], KR, Bkr, BPS[4], Brot)
            P.op('act', lambda e: e.activation(out=S1, in_=bank(5), func=AF.Silu), reads=[BPS[5]], writes=[Bs1])
            tb = bank_bf(3).rearrange("p (h t) -> p h t", h=8)
            for h in range(4):
                P.op('pe', lambda e, h=h: e.transpose(tb[:, h, :], KR[:, h * 128:(h + 1) * 128], IDB), reads=[Bkr, Bidb], writes=[BPS[3]])
            P.op('act', lambda e: e.activation(out=QT, in_=tb[:, 0:4, :], func=AF.Copy), reads=[BPS[3]], writes=[Bkf])
            P.op('act', lambda e: e.activation(out=DUM[:, 1:2], in_=DUM[:, 0:1], func=AF.Sqrt), reads=[Bdum], writes=[Bdum])
            P.op('dve', lambda e: e.tensor_tensor(out=QFT, in0=tb[:, 0:4, :], in1=XF, op=ALU.mult), reads=[BPS[3], Btab], writes=[Bkb])
            P.op('dve', lambda e: e.tensor_tensor(out=QBT, in0=tb[:, 0:4, :], in1=XB, op=ALU.mult), reads=[BPS[3], Btab], writes=[Btmp])
            for h in range(4):
                P.op('pe', lambda e, h=h, cs=cs: e.matmul(bank(4)[:, h * 128:(h + 1) * 128], lhsT=KT[:, h, cs], rhs=QT[:, h, :], start=True, stop=True),
                     reads=[Bkt[c], Bkf], writes=[BPS[4]])
            STb = S2.bitcast(BF16)[:, 0:512]
            P.op('dve', lambda e: e.tensor_tensor(out=STb.rearrange("p (h i) -> p h i", h=4), in0=bank(4).rearrange("p (h i) -> p h i", h=4), in1=DT, op=ALU.mult),
                 reads=[BPS[4], Btab], writes=[Bs2])
            for h in range(4):
                hs = slice(h * 128, (h + 1) * 128)
                P.op('pe', lambda e, hs=hs, c=c: e.matmul(bank(5)[:, hs], lhsT=STb[:, hs], rhs=V[:, c, hs], start=True, stop=False), reads=[Bs2, Bv[c]], writes=[BPS[5]])
                P.op('pe', lambda e, hs=hs, h=h, c=c: e.matmul(bank(5)[:, hs], lhsT=QFT[:, h, :], rhs=UF[:, c, hs], start=False, stop=False), reads=[Bkb, Buf_[c]], writes=[BPS[5]])
                P.op('pe', lambda e, hs=hs, h=h, c=c: e.matmul(bank(5)[:, hs], lhsT=QBT[:, h, :], rhs=UB[:, c, hs], start=False, stop=True), reads=[Btmp, Bub[c]], writes=[BPS[5]])
            if c == 0:
                P.op('act', lambda e: e.activation(out=S3, in_=bank(5), func=AF.Copy), reads=[BPS[5]], writes=[Bs3])
                tap("y0", S3, Bs3)
            yv = bank(5).rearrange("p (h d) -> p h d", h=4)
            P.op('act', lambda e: e.activation(out=S3, in_=bank(5), func=AF.Square), reads=[BPS[5]], writes=[Bs3])
            P.op('dve', lambda e: e.tensor_reduce(out=STAT[:, 4:8], in_=yv, axis=AX.X, op=ALU.add), reads=[BPS[5]], writes=[Bst])
            P.op('dve', lambda e: e.tensor_reduce(out=STAT[:, 8:12], in_=S3.rearrange("p (h d) -> p h d", h=4), axis=AX.X, op=ALU.add), reads=[Bs3], writes=[Bst])
            P.op('dve', lambda e: e.tensor_scalar(out=STAT[:, 4:8], in0=STAT[:, 4:8], scalar1=1.0 / 128, scalar2=None, op0=ALU.mult), reads=[Bst], writes=[Bst])
            P.op('dve', lambda e: e.tensor_tensor(out=STAT[:, 12:16], in0=STAT[:, 4:8], in1=STAT[:, 4:8], op=ALU.mult), reads=[Bst], writes=[Bst])
            P.op('dve', lambda e: e.scalar_tensor_tensor(out=STAT[:, 8:12], in0=STAT[:, 8:12], scalar=1.0 / 128, in1=STAT[:, 12:16], op0=ALU.mult, op1=ALU.subtract), reads=[Bst], writes=[Bst])
            P.op('act', lambda e: e.activation(out=STAT[:, 8:12], in_=STAT[:, 8:12], func=AF.Sqrt, bias=c_eps), reads=[Bst, Bcst], writes=[Bst])
            if c + 1 < NCH:
                norm_a((c + 1) % 2)
            P.op('dve', lambda e: e.reciprocal(out=STAT[:, 8:12], in_=STAT[:, 8:12]), reads=[Bst], writes=[Bst])
            if c + 1 < NCH:
                norm_b((c + 1) % 2)
            for h in range(4):
                hs = slice(h * 128, (h + 1) * 128)
                P.op('dve', lambda e, h=h, hs=hs: e.tensor_scalar(out=S3[:, hs], in0=bank(5)[:, hs], scalar1=STAT[:, 4 + h:5 + h], scalar2=STAT[:, 8 + h:9 + h], op0=ALU.subtract, op1=ALU.mult),
                     reads=[BPS[5], Bst], writes=[Bs3])
            P.op('dve', lambda e: e.tensor_tensor(out=S3, in0=S3, in1=NGR, op=ALU.mult), reads=[Bs3, Bngr], writes=[Bs3])
            P.op('dve', lambda e: e.tensor_tensor(out=KR, in0=S3, in1=S1, op=ALU.mult), reads=[Bs3, Bs1], writes=[Bkr])
            if c + 1 < NCH:
                norm_c(A1, SH1, HXTC, Bhxtc)
            for h in range(4):
                P.op('pe', lambda e, h=h: e.transpose(tb[:, 4 + h, :], KR[:, h * 128:(h + 1) * 128], IDB), reads=[Bkr, Bidb], writes=[BPS[3]])
            ROTt = S2.bitcast(BF16)[:, 512:1024].rearrange("p (h t) -> p h t", h=4)
            P.op('act', lambda e: e.activation(out=ROTt, in_=tb[:, 4:8, :], func=AF.Copy), reads=[BPS[3]], writes=[Bs2])
            for hf in range(2):
                for kc in range(8):
                    if kc < 4:
                        lhs = CONVT[:, kc, cs]
                        rd = [Bconvt[c // 4]]
                    else:
                        lhs = ROTt[:, kc - 4, :]
                        rd = [Bs2]
                    P.op('pe', lambda e, hf=hf, kc=kc, lhs=lhs: e.matmul(bank(6 + hf), lhsT=lhs, rhs=WSL[2 + hf][:, kc, :], start=(kc == 0), stop=(kc == 7)),
                         reads=rd + [Bw[2 + hf]], writes=[BPS[6 + hf]])
            if c + 1 < NCH:
                qg_proj()
            M = psb[3][:, :]
            P.op('act', lambda e: e.activation(out=XSJ, in_=M, func=AF.Square, accum_out=STAT[:, 16:17]), reads=[BPS[6], BPS[7]], writes=[Bxsj, Bst] + w4)
            P.op('act', lambda e: e.activation(out=STAT[:, 17:18], in_=STAT[:, 16:17], func=AF.Sqrt, scale=1.0 / D, bias=c_eps), reads=[Bst, Bcst], writes=[Bst])
            P.op('dve', lambda e: e.reciprocal(out=STAT[:, 17:18], in_=STAT[:, 17:18]), reads=[Bst], writes=[Bst])
            P.op('dve', lambda e: e.scalar_tensor_tensor(out=S4, in0=M, scalar=STAT[:, 17:18], in1=G1, op0=ALU.mult, op1=ALU.mult), reads=[BPS[6], BPS[7], Bst, Brows], writes=[Bs4])
            P.op('dve', lambda e, xi=xi: e.tensor_tensor(out=S4, in0=S4, in1=XC[xi], op=ALU.add), reads=[Bs4, Bxc[xi]], writes=[Bs4])
            P.dma('sp', lambda e, c=c: e.dma_start(out=xm_scr.ap()[c * 128:(c + 1) * 128, :], in_=S4), reads=[Bs4], writes=[Bxm])
            if c == 0:
                tap("xm0", S4, Bs4)
            P.op('act', lambda e: e.activation(out=XSJ, in_=S4, func=AF.Square, accum_out=STAT[:, 18:19]), reads=[Bs4], writes=[Bxsj, Bst])
            P.op('act', lambda e: e.activation(out=STAT[:, 19:20], in_=STAT[:, 18:19], func=AF.Sqrt, scale=1.0 / D, bias=c_eps), reads=[Bst, Bcst], writes=[Bst])
            P.op('dve', lambda e: e.reciprocal(out=STAT[:, 19:20], in_=STAT[:, 19:20]), reads=[Bst], writes=[Bst])
            P.op('dve', lambda e: e.scalar_tensor_tensor(out=S5, in0=S4, scalar=STAT[:, 19:20], in1=A2, op0=ALU.mult, op1=ALU.mult), reads=[Bs4, Bst, Brows], writes=[Bs5])
            P.op('dve', lambda e: e.tensor_tensor(out=S5, in0=S5, in1=SH2, op=ALU.add), reads=[Bs5, Brows], writes=[Bs5])
            tf = psb[0][:, :].rearrange("p (c t) -> p c t", c=8)
            for dc in range(8):
                P.op('pe', lambda e, dc=dc: e.transpose(tf[:, dc, :], S5[:, dc * 128:(dc + 1) * 128], c_identf), reads=[Bs5, Bcst], writes=[BPS[0], BPS[1]])
            HX2T = S4.rearrange("p (c t) -> p c t", c=8)
            P.op('act', lambda e: e.activation(out=HX2T, in_=tf, func=AF.Copy), reads=[BPS[0], BPS[1]], writes=[Bs4])
            P.op('act', lambda e: e.activation(out=XSH, in_=S5, func=AF.Copy), reads=[Bs5], writes=[Bxsh] + w4)
            P.dma('sp', lambda e, c=c: e.dma_start(out=hx2_scr.ap()[c * 128:(c + 1) * 128, :], in_=XSH), reads=[Bxsh], writes=[Bhx2])
            for dc in range(8):
                P.op('pe', lambda e, dc=dc: e.matmul(bank(2)[:, 0:16], lhsT=HX2T[:, dc, :], rhs=WR[:, dc, :], start=(dc == 0), stop=(dc == 7)), reads=[Bs4, Bwr], writes=[BPS[2]])
            P.op('dve', lambda e: e.tensor_tensor(out=STAT[:, 0:16], in0=bank(2)[:, 0:16], in1=BRR, op=ALU.add), reads=[BPS[2], Bsm], writes=[Bst])
            P.op('dve', lambda e: e.tensor_reduce(out=STAT[:, 20:21], in_=STAT[:, 0:16], axis=AX.X, op=ALU.max), reads=[Bst], writes=[Bst])
            P.op('dve', lambda e: e.tensor_scalar(out=STAT[:, 20:21], in0=STAT[:, 20:21], scalar1=-1.0, scalar2=None, op0=ALU.mult), reads=[Bst], writes=[Bst])
            P.op('act', lambda e: e.activation(out=STAT[:, 0:16], in_=STAT[:, 0:16], func=AF.Exp, bias=STAT[:, 20:21], accum_out=STAT[:, 21:22]), reads=[Bst], writes=[Bst])
            P.op('dve', lambda e: e.reciprocal(out=STAT[:, 21:22], in_=STAT[:, 21:22]), reads=[Bst], writes=[Bst])
            P.op('dve', lambda e, c=c: e.tensor_scalar(out=AFF[:, c, :], in0=STAT[:, 0:16], scalar1=STAT[:, 21:22], scalar2=None, op0=ALU.mult), reads=[Bst], writes=[Baff])
        tap("aff", AFF.rearrange("p c e -> p (c e)"), Baff)

        ckpt(7)
        P.barrier()
        ACC = A.at(24, [NCH, D], F32)
        HX2 = A.at(88, [NCH, D], BF16)
        Bacc = [Buf(f"acc{c}") for c in range(NCH)]
        Bhx = Buf("hx2")
        RING = [A.at(120 + 8 * i, [8, 512], BF16) for i in range(5)]
        Bring = [Buf(f"ring{i}") for i in range(5)]
        XST = A.at(160, [8, NS], BF16)
        HT = A.at(166, [16, NS], BF16)
        YG = A.at(178, [3, D], BF16)
        PT_ = A.at(184, [NCH, CAPG], BF16)
        PTT = A.at(190, [4, D], BF16)
        Bxst, Bht, Byg, Bpt, Bptt = Buf("xst"), Buf("ht"), Buf("yg"), Buf("pt"), Buf("ptt")
        SA = A.at(198, [NS], F32)
        Bsa = Buf("sa")
        AFFEM = A.at(160, [TPC], F32, parts=16)
        MASK = A.at(168, [TPC], F32, parts=16)
        CUM = A.at(176, [TPC], F32, parts=16)
        AFFALL = A.at(184, [1024], F32)
        Baffem, Bmask, Bcum, Baffall = Buf("affem"), Buf("mask"), Buf("cum"), Buf("affall")
        CMPS = A.at(188, [1024], F32)
        Bcmp = Buf('cmp')
        ONES16 = A.at(192, [1024], F32, parts=16)
        Bones16 = Buf('ones16')
        P.op('dve', lambda e: e.memset(ONES16, 1.0), writes=[Bones16])
        P.dma('sp', lambda e: e.dma_start(out=HX2, in_=hx2_scr.ap().rearrange("(c p) d -> p c d", p=128)), reads=[Bhx2], writes=[Bhx])
        G2 = A.at(199.5, [D], F32)
        Bg2 = Buf("g2")
        MROW2 = A.at(128, [D], F32, parts=2)
        CV2 = A.at(132, [8, 2], F32)
        ST2 = A.at(132.5, [8, 2], BF16)
        WT = [A.at(136, [8, 512], BF16), A.at(152, [8, 512], BF16)]
        BADA2 = A.at(144, [D], F32, parts=2)
        GAIN2 = A.at(148, [D], F32)
        Bfin, Bbada2, Bgain2, Bmrow2 = Buf("fin"), Buf("bada2"), Buf("gain2"), Buf("mrow2")
        Bwt = [Buf("wt0"), Buf("wt1")]
        P.dma('sp', lambda e: e.dma_start(out=CV2, in_=cvec_d), writes=[Bfin])
        P.op('act', lambda e: e.activation(out=ST2, in_=CV2, func=AF.Silu), reads=[Bfin], writes=[Bfin])
        for hf in range(2):
            P.dma('pool', lambda e, hf=hf: e.dma_start(out=WT[hf], in_=wada_d[:, (10 + hf) * 512:(11 + hf) * 512].rearrange("(c p) f -> p c f", p=128)), writes=[Bwt[hf]])
        P.dma('sp', lambda e: e.dma_start(out=BADA2, in_=bada_d[:, 10 * 512:12 * 512]), writes=[Bbada2])
        P.dma('sp', lambda e: e.dma_start(out=GAIN2, in_=postffn_d), writes=[Bgain2])
        for c in range(NCH):
            P.op('dve', lambda e, c=c: e.memset(ACC[:, c, :], 0.0), writes=[Bacc[c]])
        for c in range(NCH):
            P.op('pe', lambda e, c=c: e.transpose(bank(c // 4)[0:16, (c % 4) * 128:(c % 4) * 128 + 128], AFF[:, c, :], c_identf), reads=[Baff, Bcst], writes=[BPS[c // 4]])
        for q in range(4):
            P.op('act', lambda e, q=q: e.activation(out=AFFEM[:, q * 512:(q + 1) * 512], in_=bank(q)[0:16, :], func=AF.Copy), reads=[BPS[q]], writes=[Baffem])
        Bag2i, Bag2o = Buf("ag2i"), Buf("ag2o")
        P.dma('sp', lambda e: e.dma_start(out=ag2_in.ap(), in_=AFFEM), reads=[Baffem], writes=[Bag2i])
        P.dma('pool', lambda e: e.collective_compute("AllGather", ALU.bypass, replica_groups=[[0, 1, 2, 3], [4, 5, 6, 7]],
                                                     ins=[ag2_in.ap().opt()], outs=[ag2_out.ap().opt()]),
              reads=[Bag2i], writes=[Bag2o], inc=1)
        P.dma('sp', lambda e: e.dma_start(out=AFFALL, in_=ag2_out.ap().rearrange("r (h t) -> (r h) t", h=2)), reads=[Bag2o], writes=[Baffall])
        P.op('dve', lambda e: e.memset(MID, 0.5), writes=[Bsm])
        for k in range(1, NBIS + 1):
            hk = 2.0 ** -(k)
            hn = 2.0 ** -(k + 1)
            P.op('dve', lambda e: e.tensor_scalar(out=CMPS, in0=AFFALL, scalar1=MID, scalar2=None, op0=ALU.is_ge, op1=ALU.add, accum_out=CNT),
                 reads=[Baffall, Bsm], writes=[Bcmp, Bsm])
            P.op('pe', lambda e: e.matmul(bank(0)[:, 0:1], lhsT=c_grp, rhs=CNT, start=True, stop=True), reads=[Bsm, Bcst], writes=[BPS[0]])
            P.op('dve', lambda e, hk=hk: e.tensor_scalar(out=TT, in0=bank(0)[:, 0:1], scalar1=KCAP, scalar2=hk, op0=ALU.is_ge, op1=ALU.mult), reads=[BPS[0]], writes=[Bsm])
            P.op('dve', lambda e, hn=hn: e.scalar_tensor_tensor(out=MID, in0=MID, scalar=-hn, in1=TT, op0=ALU.add, op1=ALU.add), reads=[Bsm], writes=[Bsm])
        P.op('dve', lambda e: e.tensor_scalar(out=THR, in0=MID, scalar1=-(2.0 ** -(NBIS + 1)), scalar2=None, op0=ALU.add), reads=[Bsm], writes=[Bsm])
        P.op('pe', lambda e: e.matmul(bank(1)[0:16, 0:1], lhsT=c_sel, rhs=THR, start=True, stop=True), reads=[Bsm, Bcst], writes=[BPS[1]])
        P.op('dve', lambda e: e.tensor_copy(out=THREM[0:16, :], in_=bank(1)[0:16, 0:1]), reads=[BPS[1]], writes=[Bsm])
        tap("thr", THREM[0:16, :], Bsm)
        P.op('dve', lambda e: e.tensor_scalar(out=MASK, in0=AFFEM, scalar1=THREM[0:16, :], scalar2=None, op0=ALU.is_ge), reads=[Baffem, Bsm], writes=[Bmask])
        for g in range(NG):
            gs = slice(g * 1024, (g + 1) * 1024)
            P.op('dve', lambda e, gs=gs: e.tensor_tensor_scan(out=CUM[:, gs], data0=ONES16, data1=MASK[:, gs], initial=0.0, op0=ALU.mult, op1=ALU.add),
                 reads=[Bmask, Bones16], writes=[Bcum])
        P.op('dve', lambda e: e.tensor_tensor(out=CUM, in0=CUM, in1=MASK, op=ALU.mult), reads=[Bmask, Bcum], writes=[Bcum])
        P.op('dve', lambda e: e.tensor_scalar(out=CUM, in0=CUM, scalar1=-1.0, scalar2=None, op0=ALU.add), reads=[Bcum], writes=[Bcum])
        P.op('act', lambda e: e.activation(out=POSM, in_=CUM, func=AF.Copy), reads=[Bcum], writes=[Bposm])
        for c in range(NCH):
            P.op('pe', lambda e, c=c: e.transpose(bank(2)[:, c * 16:(c + 1) * 16], CUM[:, c * 128:(c + 1) * 128], c_identf[0:16, 0:16]), reads=[Bcum, Bcst], writes=[BPS[2]])
        P.op('dve', lambda e: e.tensor_copy(out=POST.rearrange("p c e -> p (c e)"), in_=bank(2)[:, 0:256]), reads=[BPS[2]], writes=[Bpost])
        P.op('dve', lambda e: e.scalar_tensor_tensor(out=GA.rearrange("p c e -> p (c e)"), in0=POST.rearrange("p c e -> p (c e)"), scalar=0.0, in1=AFF.rearrange("p c e -> p (c e)"), op0=ALU.is_ge, op1=ALU.mult),
             reads=[Bpost, Baff], writes=[Bga])
        tap("post", POST.rearrange("p c e -> p (c e)"), Bpost)
        for hf in range(2):
            hsl = slice(hf * 512, (hf + 1) * 512)
            for dc in range(8):
                P.op('pe', lambda e, dc=dc, hf=hf: e.matmul(bank(0)[0:2, :], lhsT=ST2[:, dc, :], rhs=WT[hf][:, dc, :], start=(dc == 0), stop=(dc == 7)), reads=[Bfin, Bwt[hf]], writes=[BPS[0]])
            P.op('dve', lambda e, hsl=hsl: e.tensor_tensor(out=MROW2[:, hsl], in0=bank(0)[0:2, :], in1=BADA2[:, hsl], op=ALU.add), reads=[BPS[0], Bbada2], writes=[Bmrow2])
            P.op('pe', lambda e, hsl=hsl: e.matmul(bank(1), lhsT=c_sel0, rhs=MROW2[:, hsl], start=True, stop=True), reads=[Bmrow2, Bcst], writes=[BPS[1]])
            P.op('dve', lambda e, hsl=hsl: e.tensor_tensor(out=G2[:, hsl], in0=bank(1), in1=GAIN2[:, hsl], op=ALU.mult), reads=[BPS[1], Bgain2], writes=[Bg2])
        P.barrier()

        ckpt(8)
        ring_i = [0]

        def ring_load(src_ap):
            i = ring_i[0] % 5
            ring_i[0] += 1
            P.dma('pool', lambda e: e.dma_start(out=RING[i], in_=src_ap.rearrange("(c p) f -> p c f", p=128)), writes=[Bring[i]])
            return i

        def expert_pieces(ex):
            lst = []
            for j in range(4):
                lst.append(wg_d[ex, :, j * 512:(j + 1) * 512])
                lst.append(wu_d[ex, :, j * 512:(j + 1) * 512])
            for hf in range(2):
                for a in range(2):
                    lst.append(wd_d[ex, a * 1024:(a + 1) * 1024, hf * 512:(hf + 1) * 512])
            return lst

        all_pieces = []
        for ex in range(ned):
            all_pieces += expert_pieces(ex)
        piece_slot = {}
        next_load = [0]

        def ensure_loaded(upto):
            while next_load[0] <= upto and next_load[0] < len(all_pieces):
                piece_slot[next_load[0]] = ring_load(all_pieces[next_load[0]])
                next_load[0] += 1

        SCOL = [c_scv[:, 0:1], c_scv[:, 1:2], c_scv[:, 2:3], c_scv[:, 3:4]]
        for ex in range(ned):
            base = ex * 12
            ensure_loaded(base + 3)
            for c in range(NCH):
                P.op('dve', lambda e, c=c, ex=ex: e.tensor_scalar(out=PT_[:, c, :], in0=c_iota, scalar1=POST[:, c, ex:ex + 1], scalar2=None, op0=ALU.is_equal),
                     reads=[Bpost, Bcst], writes=[Bpt])
            for q in range(4):
                P.op('pe', lambda e, q=q, ex=ex: e.matmul(bank(4 + (q % 2)), lhsT=SELB[:, ex, :], rhs=POSM[:, q * 512:(q + 1) * 512], start=True, stop=True),
                     reads=[Bselb, Bposm], writes=[BPS[4 + (q % 2)]])
                g = q // 2
                for k in range(2):
                    idx = g * 2 + k
                    P.op('dve', lambda e, q=q, idx=idx: e.tensor_scalar(out=PTT[:, idx, (q % 2) * 512:(q % 2) * 512 + 512], in0=bank(4 + (q % 2)), scalar1=SCOL[idx], scalar2=None, op0=ALU.is_equal),
                         reads=[BPS[4 + (q % 2)], Bcst], writes=[Bptt])
            for g in range(NG):
                for dp in range(4):
                    for k in range(2):
                        dc = dp * 2 + k
                        for cc in range(8):
                            c = g * 8 + cc
                            P.op('pe', lambda e, dp=dp, k=k, dc=dc, c=c, cc=cc: e.matmul(bank(dp)[:, k * CAPG:(k + 1) * CAPG], lhsT=HX2[:, c, dc * 128:(dc + 1) * 128], rhs=PT_[:, c, :], start=(cc == 0), stop=(cc == 7)),
                                 reads=[Bhx, Bpt], writes=[BPS[dp]])
                    P.op('act', lambda e, dp=dp, g=g: e.activation(out=XST[:, dp * 2:dp * 2 + 2, g * CAPG:(g + 1) * CAPG], in_=bank(dp)[:, 0:2 * CAPG].rearrange("p (k s) -> p k s", k=2), func=AF.Copy),
                         reads=[BPS[dp]], writes=[Bxst])
            for j in range(4):
                ensure_loaded(base + 2 * j + 4)
                sg_ = piece_slot[base + 2 * j]
                su_ = piece_slot[base + 2 * j + 1]
                for f in range(4):
                    fc = j * 4 + f
                    fs = slice(f * 128, (f + 1) * 128)
                    ba = 4 + (fc % 2) * 2
                    for dc in range(8):
                        P.op('pe', lambda e, dc=dc, fs=fs, ba=ba, sg_=sg_: e.matmul(bank(ba)[:, 0:NS], lhsT=RING[sg_][:, dc, fs], rhs=XST[:, dc, :], start=(dc == 0), stop=(dc == 7)),
                             reads=[Bring[sg_], Bxst], writes=[BPS[ba]])
                    for dc in range(8):
                        P.op('pe', lambda e, dc=dc, fs=fs, ba=ba, su_=su_: e.matmul(bank(ba + 1)[:, 0:NS], lhsT=RING[su_][:, dc, fs], rhs=XST[:, dc, :], start=(dc == 0), stop=(dc == 7)),
                             reads=[Bring[su_], Bxst], writes=[BPS[ba + 1]])
                    P.op('act', lambda e, ba=ba: e.activation(out=SA, in_=bank(ba)[:, 0:NS], func=AF.Silu), reads=[BPS[ba]], writes=[Bsa])
                    P.op('dve', lambda e, ba=ba, fc=fc: e.tensor_tensor(out=HT[:, fc, :], in0=SA, in1=bank(ba + 1)[:, 0:NS], op=ALU.mult), reads=[Bsa, BPS[ba + 1]], writes=[Bht])
            for hf in range(2):
                ensure_loaded(base + 8 + 2 * hf + 4)
                sd = [piece_slot[base + 8 + 2 * hf], piece_slot[base + 8 + 2 * hf + 1]]
                for sc in range(3):
                    bi = (hf * 3 + sc) % 4
                    for fc in range(16):
                        P.op('pe', lambda e, fc=fc, sc=sc, bi=bi, sd=sd: e.matmul(bank(bi), lhsT=HT[:, fc, sc * 128:(sc + 1) * 128], rhs=RING[sd[fc // 8]][:, fc % 8, :], start=(fc == 0), stop=(fc == 15)),
                             reads=[Bht, Bring[sd[fc // 8]]], writes=[BPS[bi]])
                    P.op('act', lambda e, sc=sc, hf=hf, bi=bi: e.activation(out=YG[:, sc, hf * 512:(hf + 1) * 512], in_=bank(bi), func=AF.Copy), reads=[BPS[bi]], writes=[Byg])
            for c in range(NCH):
                g = c // 8
                tl = (c % 8) * 128
                bo = 2 * (c % 2)
                for hf in range(2):
                    for k in range(2):
                        idx = g * 2 + k
                        sc = g + k
                        P.op('pe', lambda e, hf=hf, k=k, idx=idx, sc=sc, tl=tl, bo=bo: e.matmul(bank(4 + bo + hf), lhsT=PTT[:, idx, tl:tl + 128], rhs=YG[:, sc, hf * 512:(hf + 1) * 512], start=(k == 0), stop=(k == 1)),
                             reads=[Bptt, Byg], writes=[BPS[4 + bo + hf]])
                O = psb[2 + (c % 2)][:, :]
                P.op('dve', lambda e, c=c, ex=ex, O=O: e.scalar_tensor_tensor(out=ACC[:, c, :], in0=O, scalar=GA[:, c, ex:ex + 1], in1=ACC[:, c, :], op0=ALU.mult, op1=ALU.add),
                     reads=[BPS[4 + bo], BPS[5 + bo], Bga, Bacc[c]], writes=[Bacc[c]])
        tap("acc0", ACC[:, 0, :], Bacc[0])

        ckpt(9)
        P.barrier()
        NXM = 4
        XM = [A.at(120 + 4 * i, [D], F32) for i in range(NXM)]
        SQ = A.at(136, [D], F32)
        SQJ = A.at(140, [D], F32)
        Bxmm = [Buf(f"xmm{i}") for i in range(NXM)]
        Bsq, Bsqj = Buf("sq"), Buf("sqj")
        Bstf = [Buf("stf0"), Buf("stf1")]
        Bout = Buf("out")
        for c in range(NCH):
            i = c % NXM
            st = STAT[:, 24 + 2 * (c % 2):26 + 2 * (c % 2)]
            bst = Bstf[c % 2]
            P.dma('sp', lambda e, c=c, i=i: e.dma_start(out=XM[i], in_=xm_scr.ap()[c * 128:(c + 1) * 128, :]), reads=[Bxm], writes=[Bxmm[i]])
            P.op('act', lambda e, c=c, st=st: e.activation(out=SQJ, in_=ACC[:, c, :], func=AF.Square, accum_out=st[:, 0:1]), reads=[Bacc[c]], writes=[Bsqj, bst])
            P.op('act', lambda e, st=st: e.activation(out=st[:, 1:2], in_=st[:, 0:1], func=AF.Sqrt, scale=1.0 / D, bias=c_eps), reads=[bst, Bcst], writes=[bst])
            P.op('dve', lambda e, st=st: e.reciprocal(out=st[:, 1:2], in_=st[:, 1:2]), reads=[bst], writes=[bst])
            P.op('dve', lambda e, c=c, st=st: e.scalar_tensor_tensor(out=SQ, in0=ACC[:, c, :], scalar=st[:, 1:2], in1=G2, op0=ALU.mult, op1=ALU.mult), reads=[Bacc[c], bst, Bg2], writes=[Bsq])
            P.op('dve', lambda e, i=i: e.tensor_tensor(out=XM[i], in0=XM[i], in1=SQ, op=ALU.add), reads=[Bsq, Bxmm[i]], writes=[Bxmm[i]])
            out_toks.append(P.dma('sp', lambda e, c=c, i=i: e.dma_start(out=out_d[c * 128:(c + 1) * 128, :], in_=XM[i]), reads=[Bxmm[i]], writes=[Bout], sembuf=Bxmm[i]))
        P.wait_all('sp', [t for t in out_toks if t is not None])
        with nc.Block() as block:
            P.emit(block)
    return nc


def _host_consts(r):
    cst = np.zeros((128, 1600), np.float32)
    p = np.arange(128, dtype=np.float32)
    i = np.arange(128, dtype=np.float32)
    cst[:, 0:192] = np.arange(192, dtype=np.float32)[None, :]
    rel = i[None, :] - p[:, None]
    cst[:, 192:320] = np.maximum(rel, 0)
    cst[:, 320:448] = (rel >= 0)
    cst[:, 448:576] = np.maximum(-rel, 0)
    cst[:, 576:704] = (rel < 0)
    cst[:, 704:832] = (i + 1)[None, :]
    cst[:, 832:960] = (128 - i)[None, :]
    cst[:, 960:1088] = np.eye(128, dtype=np.float32)
    cst[:, 1088] = 127 - p
    cst[:, 1089] = p
    for c in range(2):
        cst[:, 1090 + c] = 255 - (c * 128 + p)
        cst[:, 1092 + c] = c * 128 + p
    for idx, (g, sc) in enumerate(((0, 0), (0, 1), (1, 1), (1, 2))):
        v = 128 * sc + p - g * CAPG
        cst[:, 1094 + idx] = np.where((v >= 0) & (v < CAPG), v, -5.0)
    pe = (np.arange(128) // 2) % 16
    cst[:, 1100:1228] = (pe[:, None] == pe[None, :])
    sel = np.zeros((128, 16), np.float32)
    for e in range(16):
        sel[2 * e, e] = 1
    cst[:, 1228:1244] = sel
    cst[:, 1244:1260] = (128 * (15 - np.arange(16)))[None, :]
    cst[:, 1260:1276] = (128 * np.arange(16))[None, :]
    xe = np.zeros(10, np.float32)
    xm = np.zeros(10, np.float32)
    for s in range(4):
        if s < r:
            xe[s] = 2048 * (r - 1 - s); xm[s] = 1
        if s > r:
            xe[4 + s] = 2048 * (s - r - 1); xm[4 + s] = 1
    xe[8] = 2048 * r; xm[8] = 1
    xe[9] = 2048 * (3 - r); xm[9] = 1
    cst[:, 1276:1286] = xe[None, :]
    cst[:, 1432:1442] = xm[None, :]
    cst[:, 1300:1428] = 1.0
    cst[0, 1428] = 1.0
    cst[1, 1429] = 1.0
    cst[:, 1430] = 1e-6
    cst[0, 1442:1570] = 1.0
    half = 64
    inv = (1.0 / (10000.0 ** (np.arange(half, dtype=np.float32) / half))).astype(np.float32)
    def tabs(pos, scale):
        ang = pos[:, None].astype(np.float32) * inv[None, :]
        return (np.cos(ang) * scale).astype(np.float32), (np.sin(ang) * scale).astype(np.float32)
    pos = (256 + r * 2048 + np.arange(2048)).astype(np.float32)
    cq, sq = tabs(pos, 1.0)
    ck, sk = tabs(pos, 128.0 ** -0.5)
    cc, sc_ = tabs(np.arange(256).astype(np.float32), 128.0 ** -0.5)
    def lay(t, n):
        return t.reshape(n, 128, 64).transpose(1, 0, 2)
    rotq = np.stack([lay(cq, 16), lay(sq, 16)], axis=1)
    rotk = np.stack([lay(ck, 16), lay(sk, 16)], axis=1)
    rotc = np.stack([lay(cc, 2), lay(sc_, 2)], axis=1)
    return cst, np.ascontiguousarray(rotq), np.ascontiguousarray(rotk), np.ascontiguousarray(rotc)


def make_in_maps(x, c, ctx, c_ctx, w_ada, b_ada, pre_mix_g, post_mix_g, pre_ffn_g, post_ffn_g,
                 w_in, conv_w, conv_b, ret_decay_logit, ret_norm_g, w_out,
                 w_router, b_router, w_gate, w_up, w_down):
    f = lambda a: np.ascontiguousarray(np.asarray(a, dtype=np.float32))
    x, c, ctx, c_ctx = f(x), f(c), f(ctx), f(c_ctx)
    rep = lambda v: np.ascontiguousarray(np.broadcast_to(f(v).reshape(1, -1), (128, f(v).size)))
    shared = {
        "w_ada": f(w_ada[0]), "b_ada2": np.ascontiguousarray(np.broadcast_to(f(b_ada[0])[None, :], (2, 6 * D))),
        "pmg": np.ascontiguousarray(f(pre_mix_g[0]).reshape(8, 128).T),
        "postmix_row": rep(post_mix_g[0]), "preffn_row": rep(pre_ffn_g[0]), "postffn_row": rep(post_ffn_g[0]),
        "w_in": f(w_in[0]),
        "convw": np.ascontiguousarray(f(conv_w[0]).reshape(3, 4, 128).transpose(2, 1, 0)),
        "convb": np.ascontiguousarray(f(conv_b[0]).reshape(4, 128).T),
        "decay": rep(f(ret_decay_logit[0]).reshape(-1)),
        "retg_row": rep(ret_norm_g[0]),
        "w_out": f(w_out[0]),
        "w_router": np.ascontiguousarray(f(w_router[0]).reshape(8, 128, 16).transpose(1, 0, 2)),
        "brouter_row": rep(b_router[0]),
        "w_gate": f(w_gate[0]), "w_up": f(w_up[0]), "w_down": f(w_down[0]),
        "identb": np.eye(128, dtype=np.float32).astype(ml_dtypes.bfloat16),
    }
    selb = np.zeros((16, NE, 128), np.float32)
    for e in range(NE):
        selb[e, e, :] = 1
    shared["selb"] = selb.astype(ml_dtypes.bfloat16)
    maps = []
    for j in range(8):
        b, r = j // 4, j % 4
        cst, rotq, rotk, rotc = _host_consts(r)
        cv = np.stack([c[b].reshape(8, 128).T, c_ctx.reshape(8, 128).T], axis=-1)
        m = dict(shared)
        m.update({"x": np.ascontiguousarray(x[b, r * TPC:(r + 1) * TPC]), "ctx": np.ascontiguousarray(ctx[b]),
                  "cvec": np.ascontiguousarray(cv.astype(np.float32)), "cst": cst, "rotq": rotq, "rotk": rotk, "rotc": rotc})
        maps.append(m)
    return maps


_NC_CACHE = {}


def kernel(**inputs):
    if "nc" not in _NC_CACHE:
        _NC_CACHE["nc"] = build()
    nc = _NC_CACHE["nc"]
    maps = make_in_maps(**inputs)
    res = run_bass_kernel_spmd(nc, maps, core_ids=list(range(8)))
    out = np.zeros((2, 8192, D), np.float32)
    for j in range(8):
        b, r = j // 4, j % 4
        out[b, r * TPC:(r + 1) * TPC] = res.results[j]["out"]
    return out
```

```python
import numpy as np
import ml_dtypes
import concourse.bass as bass
import concourse.mybir as mybir
from concourse.bass_utils import run_bass_kernel_spmd
from contextlib import ExitStack

F32 = mybir.dt.float32
BF16 = mybir.dt.bfloat16
ALU = mybir.AluOpType
AF = mybir.ActivationFunctionType
AX = mybir.AxisListType

ENG = ('pe', 'act', 'dve', 'pool', 'sp')
D = 1024
TPC = 2048
NCH = 16
NE = 16
CAPG = 192
NG = 2
NS = NG * CAPG
KCAP = 1024.0
NBIS = 26
DBG = {}


class Buf:
    __slots__ = ('name', 'w', 'r', 'dsem', 'dcnt', 'const', 'psum')

    def __init__(self, name, psum=False):
        self.name = name
        self.psum = psum
        self.w = None
        self.r = {}
        self.dsem = None
        self.dcnt = 0
        self.const = False


class Prog:
    def __init__(self, nc, es):
        self.nc = nc
        self.es = es
        self.sem = {e: es.enter_context(nc.semaphore('s_' + e)) for e in ENG}
        self.cnt = {e: 0 for e in ENG}
        self.stream = {e: [] for e in ENG}
        self.known = {e: {} for e in ENG}
        self.dbufs = []
        self.frozen = False

    def _waits(self, eng, reads, writes):
        need = {}

        def add(tok):
            if tok is None:
                return
            sem, val, teng = tok
            if teng == 'pe' and eng == 'pe':
                return
            key = id(sem)
            if key not in need or need[key][1] < val:
                need[key] = (sem, val)

        for b in reads:
            add(b.w)
            if b.psum:
                for t in b.r.values():
                    if t[2] != eng:
                        add(t)
        for b in writes:
            add(b.w)
            for t in b.r.values():
                add(t)
        out = []
        kn = self.known[eng]
        for key, (sem, val) in need.items():
            if kn.get(key, 0) >= val:
                continue
            kn[key] = val
            out.append((sem, val))
        return out

    def _record(self, tok, reads, writes):
        key = id(tok[0])
        for b in reads:
            if not b.const:
                b.r[key] = tok
        for b in writes:
            b.w = tok
            b.r = {}

    def op(self, eng, fn, reads=(), writes=()):
        if self.frozen:
            return None
        waits = self._waits(eng, reads, writes)
        self.cnt[eng] += 1
        tok = (self.sem[eng], self.cnt[eng], eng)
        self._record(tok, reads, writes)
        self.stream[eng].append((waits, fn, self.sem[eng], 1))
        return tok

    def dma(self, q, fn, reads=(), writes=(), sembuf=None, inc=16):
        if self.frozen:
            return None
        waits = self._waits(q, reads, writes)
        sb = sembuf or (writes[0] if writes else reads[0])
        if sb.dsem is None:
            sb.dsem = self.es.enter_context(self.nc.semaphore('d_' + sb.name))
            self.dbufs.append(sb)
        sb.dcnt += inc
        tok = (sb.dsem, sb.dcnt, 'dma')
        self._record(tok, reads, writes)
        self.stream[q].append((waits, fn, sb.dsem, inc))
        return tok

    def wait_all(self, eng, toks):
        waits = []
        kn = self.known[eng]
        for (sem, val, _) in toks:
            if kn.get(id(sem), 0) >= val:
                continue
            kn[id(sem)] = val
            waits.append((sem, val))
        self.stream[eng].append((waits, None, None, 0))

    def barrier(self):
        if self.frozen:
            return
        toks = [(self.sem[e], self.cnt[e], e) for e in ENG if self.cnt[e] > 0]
        toks += [(b.dsem, b.dcnt, 'dma') for b in self.dbufs]
        for e in ENG:
            self.wait_all(e, toks)

    def emit(self, block):
        streams = self.stream

        def run(e, name):
            for waits, fn, sem, inc in streams[name]:
                for s, v in waits:
                    e.wait_ge(s, v)
                if fn is not None:
                    ins = fn(e)
                    ins.then_inc(sem, inc)

        @block.tensor
        def _(e):
            run(e, 'pe')

        @block.scalar
        def _(e):
            run(e, 'act')

        @block.vector
        def _(e):
            run(e, 'dve')

        @block.gpsimd
        def _(e):
            run(e, 'pool')

        @block.sync
        def _(e):
            run(e, 'sp')


class Arena:
    def __init__(self, big):
        self.big = big

    def at(self, off_kib, free_shape, dt, parts=128):
        n = int(np.prod(free_shape))
        nb = n * (2 if dt == BF16 else 4)
        n32 = (nb + 3) // 4
        off = int(off_kib * 256)
        ap = self.big[0:parts, off:off + n32]
        if dt != F32:
            ap = ap.bitcast(dt)
        if len(free_shape) == 2:
            ap = ap.rearrange("p (a b) -> p a b", a=free_shape[0], b=free_shape[1])
        elif len(free_shape) == 3:
            ap = ap.rearrange("p (a b c) -> p a b c", a=free_shape[0], b=free_shape[1], c=free_shape[2])
        return ap


class _Stop(Exception):
    pass


def build(dbg=(), stop=99):
    nc = bass.Bass("TRN2", target_bir_lowering=False)

    def din(name, shape, dt=F32):
        return nc.dram_tensor(name, list(shape), dt, kind="ExternalInput").ap()

    x_d = din("x", [TPC, D])
    ctx_d = din("ctx", [256, D])
    cvec_d = din("cvec", [128, 8, 2])
    wada_d = din("w_ada", [D, 6 * D])
    bada_d = din("b_ada2", [2, 6 * D])
    pmg_d = din("pmg", [128, 8])
    postmix_d = din("postmix_row", [128, D])
    preffn_d = din("preffn_row", [128, D])
    postffn_d = din("postffn_row", [128, D])
    win_d = din("w_in", [D, 3584])
    convw_d = din("convw", [128, 4, 3])
    convb_d = din("convb", [128, 4])
    dec_d = din("decay", [128, 8])
    retg_d = din("retg_row", [128, 512])
    wout_d = din("w_out", [D, D])
    wr_d = din("w_router", [128, 8, 16])
    br_d = din("brouter_row", [128, 16])
    ned = NE if stop > 8 else 1
    wg_d = din("w_gate", [ned, D, 2 * D])
    wu_d = din("w_up", [ned, D, 2 * D])
    wd_d = din("w_down", [ned, 2 * D, D])
    rotq_d = din("rotq", [128, 2, NCH, 64])
    rotk_d = din("rotk", [128, 2, NCH, 64])
    rotc_d = din("rotc", [128, 2, 2, 64])
    cst_d = din("cst", [128, 1600])
    identb_d = din("identb", [128, 128], BF16)
    selb_d = din("selb", [16, NE, 128], BF16)
    out_d = nc.dram_tensor("out", [TPC, D], F32, kind="ExternalOutput").ap()
    taps = {}
    for name, shape in dbg:
        taps[name] = nc.dram_tensor("dbg_" + name, list(shape), F32, kind="ExternalOutput").ap()

    xm_scr = nc.dram_tensor("xm_scr", [TPC, D], F32)
    hx2_scr = nc.dram_tensor("hx2_scr", [TPC, D], BF16)
    ag1_in = nc.dram_tensor("ag1_in", [128, 1024], F32)
    ag1_out = nc.dram_tensor("ag1_out", [512, 1024], F32)
    ag2_in = nc.dram_tensor("ag2_in", [16, TPC], F32)
    ag2_out = nc.dram_tensor("ag2_out", [64, TPC], F32)

    with ExitStack() as es:
        P = Prog(nc, es)
        big = es.enter_context(nc.sbuf_tensor("big", [128, 204 * 256], F32))
        A = Arena(big)
        psb = [es.enter_context(nc.psum_tensor(f"psb{i}", [128, 1024], F32)) for i in range(4)]
        BPS = [Buf(f"ps{i}", psum=True) for i in range(8)]

        def bank(i):
            return psb[i // 2][:, (i % 2) * 512:(i % 2) * 512 + 512]

        def bank_bf(i):
            return bank(i).bitcast(BF16)

        out_toks = []

        def ckpt(n):
            if stop == n:
                P.frozen = True


        def tap(name, src_ap, buf, q='sp', orr=None, **kw):
            if name in taps:
                dst = taps[name] if orr is None else taps[name].rearrange(orr, **kw)
                out_toks.append(P.dma(q, lambda e: e.dma_start(out=dst, in_=src_ap), reads=[buf], writes=[Buf("tap_" + name)], sembuf=Buf("tsem_" + name)))

        CST = A.at(0, [1600], F32)
        Bcst = Buf("cst"); Bcst.const = True
        c_iota = CST[:, 0:192]
        c_relF = CST[:, 192:320]
        c_mskF = CST[:, 320:448]
        c_relB = CST[:, 448:576]
        c_mskB = CST[:, 576:704]
        c_i1 = CST[:, 704:832]
        c_i128 = CST[:, 832:960]
        c_identf = CST[:, 960:1088]
        c_p127 = CST[:, 1088:1089]
        c_p = CST[:, 1089:1090]
        c_ctxf = CST[:, 1090:1092]
        c_ctxb = CST[:, 1092:1094]
        c_scv = CST[:, 1094:1098]
        c_grp = CST[:, 1100:1228]
        c_sel = CST[:, 1228:1244]
        c_c128f = CST[:, 1244:1260]
        c_c128b = CST[:, 1260:1276]
        c_xexp = CST[:, 1276:1292]
        c_ones = CST[:, 1300:1428]
        c_e01 = CST[:, 1428:1430]
        c_eps = CST[:, 1430:1431]
        c_sel0 = CST[0:2, 1442:1570]
        IDB = A.at(6.25, [128], BF16)
        Bidb = Buf("idb"); Bidb.const = True
        SELB = A.at(6.5, [NE, 128], BF16, parts=16)
        Bselb = Buf("selb"); Bselb.const = True
        SMALL = A.at(10.5, [384], F32)
        Bsm = Buf("small")
        LG = SMALL[:, 0:8]
        A1 = SMALL[:, 8:16]; SH1 = SMALL[:, 16:24]; A1C = SMALL[:, 24:32]; SH1C = SMALL[:, 32:40]
        ZF = SMALL[:, 40:44]; ZB = SMALL[:, 44:48]
        CZF = SMALL[:, 48:56]; CZB = SMALL[:, 56:64]
        CF = SMALL[:, 64:128]; CB = SMALL[:, 128:192]
        G128 = SMALL[:, 192:200]
        XCO = SMALL[:, 200:240]
        PMG = SMALL[:, 240:248]
        CW = SMALL[:, 248:260]
        CBI = SMALL[:, 260:264]
        BRR = SMALL[:, 264:280]
        THR = SMALL[:, 280:281]; MID = SMALL[:, 281:282]; CNT = SMALL[:, 282:283]; TT = SMALL[:, 283:284]
        THREM = SMALL[:, 284:285]
        STAT = SMALL[:, 288:320]
        DECL = SMALL[:, 320:328]
        AFF = A.at(12, [NCH, NE], F32)
        POST = A.at(13, [NCH, NE], F32)
        GA = A.at(14, [NCH, NE], F32)
        Baff, Bpost, Bga = Buf("aff"), Buf("post"), Buf("ga")
        POSM = A.at(15, [TPC], BF16, parts=16)
        Bposm = Buf("posm")
        LF = A.at(19, [4, 128], F32)
        LB = A.at(21, [4, 128], F32)
        Blf, Blb = Buf("lf"), Buf("lb")
        KT = A.at(24, [4, TPC], BF16)
        V = A.at(40, [NCH, 512], BF16)
        UF = A.at(56, [NCH, 512], BF16)
        UB = A.at(72, [NCH, 512], BF16)
        CONVT = A.at(88, [4, TPC], BF16)
        Bkt = [Buf(f"kt{c}") for c in range(NCH)]
        Bv = [Buf(f"v{c}") for c in range(NCH)]
        Buf_ = [Buf(f"uf{c}") for c in range(NCH)]
        Bub = [Buf(f"ub{c}") for c in range(NCH)]
        Bconvt = [Buf(f"convt{g}") for g in range(4)]
        WA = A.at(104, [8, 512], BF16)
        WB_ = A.at(112, [8, 512], BF16)
        WC = A.at(120, [8, 512], BF16)
        WD = A.at(128, [8, 512], BF16)
        WE = A.at(136, [8, 512], BF16)
        Bw = [Buf(f"w{i}") for i in range(5)]
        WSL = [WA, WB_, WC, WD, WE]
        ROT = A.at(144, [2, NCH, 64], F32)
        Brot = Buf("rot")
        ROTC = A.at(152, [2, 2, 64], F32)
        Brotc = Buf("rotc")
        DT = A.at(153, [4, 128], F32)
        XF = A.at(155, [4, 128], F32)
        XB = A.at(157, [4, 128], F32)
        Btab = Buf("tab")
        ROWS = A.at(159, [3, D], F32)
        Brows = Buf("rows")
        HXTG = A.at(159, [8, 512], BF16)
        Bhxtg = Buf("hxtg")
        NGR = A.at(171, [512], F32)
        Bngr = Buf("ngr")
        T0 = 173
        XC = [A.at(T0, [D], F32), A.at(T0 + 4, [D], F32)]
        Bxc = [Buf("xc0"), Buf("xc1")]
        XS = A.at(T0 + 8, [D], BF16)
        Bxs = Buf("xs")
        HXTC = A.at(T0 + 10, [8, 128], BF16)
        Bhxtc = Buf("hxtc")
        TMPA = A.at(T0 + 12, [4, 64], F32)
        TMPB = A.at(T0 + 13, [4, 64], F32)
        Btmp = Buf("tmpab")
        KR = A.at(T0 + 14, [512], BF16)
        Bkr = Buf("kr")
        KFs = A.at(T0 + 15, [512], BF16)
        KBs = A.at(T0 + 16, [512], BF16)
        Bkf, Bkb = Buf("kf"), Buf("kb")
        S1 = A.at(T0 + 17, [512], F32)
        S2 = A.at(T0 + 19, [512], F32)
        S3 = A.at(T0 + 21, [512], F32)
        Bs1, Bs2, Bs3 = Buf("s1"), Buf("s2"), Buf("s3")
        S4 = A.at(T0 + 23, [D], F32)
        Bs4 = Buf("s4")
        S5 = A.at(T0 + 27, [D], F32)
        Bs5 = Buf("s5")

        P.dma('sp', lambda e: e.dma_start(out=CST, in_=cst_d), writes=[Bcst])
        P.dma('sp', lambda e: e.dma_start(out=IDB, in_=identb_d), writes=[Bidb])
        P.dma('sp', lambda e: e.dma_start(out=SELB, in_=selb_d), writes=[Bselb])
        P.dma('sp', lambda e: e.dma_start(out=DECL, in_=dec_d), writes=[Bsm])
        P.dma('sp', lambda e: e.dma_start(out=PMG, in_=pmg_d), writes=[Bsm])
        P.dma('sp', lambda e: e.dma_start(out=CW, in_=convw_d.rearrange("p a b -> p (a b)")), writes=[Bsm])
        P.dma('sp', lambda e: e.dma_start(out=CBI, in_=convb_d), writes=[Bsm])
        P.dma('sp', lambda e: e.dma_start(out=BRR, in_=br_d), writes=[Bsm])
        P.dma('sp', lambda e: e.dma_start(out=ROT, in_=rotk_d), writes=[Brot])
        P.dma('sp', lambda e: e.dma_start(out=ROTC, in_=rotc_d), writes=[Brotc])
        P.dma('sp', lambda e: e.dma_start(out=NGR, in_=retg_d), writes=[Bngr])

        P.op('act', lambda e: e.activation(out=LG, in_=DECL, func=AF.Exp, scale=-1.0), reads=[Bsm], writes=[Bsm])
        P.op('dve', lambda e: e.tensor_scalar(out=LG, in0=LG, scalar1=1.0, scalar2=None, op0=ALU.add), reads=[Bsm], writes=[Bsm])
        P.op('act', lambda e: e.activation(out=LG, in_=LG, func=AF.Ln), reads=[Bsm], writes=[Bsm])
        P.op('dve', lambda e: e.tensor_scalar(out=LG, in0=LG, scalar1=-1.0, scalar2=None, op0=ALU.mult), reads=[Bsm], writes=[Bsm])
        for h in range(4):
            lf = LG[:, h:h + 1]
            lb = LG[:, 4 + h:5 + h]
            P.op('act', lambda e, h=h, lf=lf: e.activation(out=DT[:, h, :], in_=c_relF, func=AF.Exp, scale=lf), reads=[Bsm, Bcst], writes=[Btab])
            P.op('act', lambda e, h=h, lb=lb: e.activation(out=XF[:, h, :], in_=c_relB, func=AF.Exp, scale=lb), reads=[Bsm, Bcst], writes=[Btab])
            P.op('dve', lambda e, h=h: e.tensor_tensor(out=DT[:, h, :], in0=DT[:, h, :], in1=c_mskF, op=ALU.mult), reads=[Btab, Bcst], writes=[Btab])
            P.op('dve', lambda e, h=h: e.tensor_tensor(out=XF[:, h, :], in0=XF[:, h, :], in1=c_mskB, op=ALU.mult), reads=[Btab, Bcst], writes=[Btab])
            P.op('dve', lambda e, h=h: e.tensor_tensor(out=DT[:, h, :], in0=DT[:, h, :], in1=XF[:, h, :], op=ALU.add), reads=[Btab], writes=[Btab])
        for h in range(4):
            lf = LG[:, h:h + 1]
            lb = LG[:, 4 + h:5 + h]
            P.op('act', lambda e, h=h, lf=lf: e.activation(out=XF[:, h, :], in_=c_i1, func=AF.Exp, scale=lf), reads=[Bsm, Bcst, Btab], writes=[Btab])
            P.op('act', lambda e, h=h, lb=lb: e.activation(out=XB[:, h, :], in_=c_i128, func=AF.Exp, scale=lb), reads=[Bsm, Bcst], writes=[Btab])
            P.op('act', lambda e, h=h, lf=lf: e.activation(out=ZF[:, h:h + 1], in_=c_p127, func=AF.Exp, scale=lf), reads=[Bsm, Bcst], writes=[Bsm])
            P.op('act', lambda e, h=h, lb=lb: e.activation(out=ZB[:, h:h + 1], in_=c_p, func=AF.Exp, scale=lb), reads=[Bsm, Bcst], writes=[Bsm])
            for c in range(2):
                P.op('act', lambda e, h=h, c=c, lf=lf: e.activation(out=CZF[:, c * 4 + h:c * 4 + h + 1], in_=c_ctxf[:, c:c + 1], func=AF.Exp, scale=lf), reads=[Bsm, Bcst], writes=[Bsm])
                P.op('act', lambda e, h=h, c=c, lb=lb: e.activation(out=CZB[:, c * 4 + h:c * 4 + h + 1], in_=c_ctxb[:, c:c + 1], func=AF.Exp, scale=lb), reads=[Bsm, Bcst], writes=[Bsm])
            cfv = CF.rearrange("p (c h) -> p c h", h=4)
            cbv = CB.rearrange("p (c h) -> p c h", h=4)
            P.op('act', lambda e, h=h, lf=lf, cfv=cfv: e.activation(out=cfv[:, :, h], in_=c_c128f, func=AF.Exp, scale=lf), reads=[Bsm, Bcst], writes=[Bsm])
            P.op('act', lambda e, h=h, lb=lb, cbv=cbv: e.activation(out=cbv[:, :, h], in_=c_c128b, func=AF.Exp, scale=lb), reads=[Bsm, Bcst], writes=[Bsm])
            P.op('act', lambda e, h=h, lf=lf: e.activation(out=G128[:, h:h + 1], in_=c_ones[:, 0:1], func=AF.Exp, scale=lf), reads=[Bsm, Bcst], writes=[Bsm])
            P.op('act', lambda e, h=h, lb=lb: e.activation(out=G128[:, 4 + h:5 + h], in_=c_ones[:, 0:1], func=AF.Exp, scale=lb), reads=[Bsm, Bcst], writes=[Bsm])
            xco = XCO.rearrange("p (s h) -> p s h", h=4)
            P.op('act', lambda e, h=h, lf=lf, xco=xco: e.activation(out=xco[:, 0:4, h], in_=c_xexp[:, 0:4], func=AF.Exp, scale=lf), reads=[Bsm, Bcst], writes=[Bsm])
            P.op('act', lambda e, h=h, lb=lb, xco=xco: e.activation(out=xco[:, 4:8, h], in_=c_xexp[:, 4:8], func=AF.Exp, scale=lb), reads=[Bsm, Bcst], writes=[Bsm])
            P.op('act', lambda e, h=h, lf=lf, xco=xco: e.activation(out=xco[:, 8:9, h], in_=c_xexp[:, 8:9], func=AF.Exp, scale=lf), reads=[Bsm, Bcst], writes=[Bsm])
            P.op('act', lambda e, h=h, lb=lb, xco=xco: e.activation(out=xco[:, 9:10, h], in_=c_xexp[:, 9:10], func=AF.Exp, scale=lb), reads=[Bsm, Bcst], writes=[Bsm])
        for _ in range(7):
            P.op('dve', lambda e: e.tensor_tensor(out=G128, in0=G128, in1=G128, op=ALU.mult), reads=[Bsm], writes=[Bsm])
        xco = XCO.rearrange("p (s h) -> p s h", h=4)
        c_xmsk = CST[:, 1432:1442]
        P.op('dve', lambda e: e.tensor_tensor(out=xco, in0=xco, in1=c_xmsk.unsqueeze(2).to_broadcast([128, 10, 4]), op=ALU.mult), reads=[Bsm, Bcst], writes=[Bsm])

        ckpt(1)
        wslot = [0]

        def load_piece(src_ap, slot):
            P.dma('pool', lambda e: e.dma_start(out=WSL[slot], in_=src_ap.rearrange("(c p) f -> p c f", p=128)), writes=[Bw[slot]])

        CV = A.at(23, [8, 2], F32)
        ST_ = A.at(23.25, [8, 2], BF16)
        Bcv = Buf('cv')
        MROW = A.at(T0 + 27, [512], F32, parts=2)
        P.dma('sp', lambda e: e.dma_start(out=CV, in_=cvec_d), writes=[Bcv])
        P.op('act', lambda e: e.activation(out=ST_, in_=CV, func=AF.Silu), reads=[Bcv], writes=[Bcv])
        BADA = A.at(T0 + 17, [512], F32, parts=2)

        def mod_block(n, slot):
            load_piece(wada_d[:, n * 512:(n + 1) * 512], slot)
            P.dma('sp', lambda e: e.dma_start(out=BADA, in_=bada_d[:, n * 512:(n + 1) * 512]), writes=[Bs1])
            for dc in range(8):
                P.op('pe', lambda e, dc=dc: e.matmul(bank(0)[0:2, :], lhsT=ST_[:, dc, :], rhs=WSL[slot][:, dc, :], start=(dc == 0), stop=(dc == 7)),
                     reads=[Bcv, Bw[slot]], writes=[BPS[0]])
            P.op('dve', lambda e: e.tensor_tensor(out=MROW, in0=bank(0)[0:2, :], in1=BADA, op=ALU.add), reads=[BPS[0], Bs1], writes=[Bs5])

        def row_bcast(dst_ap, dst_buf, gain_src, plus_one):
            P.op('pe', lambda e: e.matmul(bank(1), lhsT=c_sel0, rhs=MROW, start=True, stop=True),
                 reads=[Bs5, Bcst], writes=[BPS[1]])
            if gain_src is None:
                P.op('act', lambda e: e.activation(out=dst_ap, in_=bank(1), func=AF.Copy), reads=[BPS[1]], writes=[dst_buf])
            elif plus_one:
                P.op('dve', lambda e: e.scalar_tensor_tensor(out=dst_ap, in0=bank(1), scalar=1.0, in1=gain_src, op0=ALU.add, op1=ALU.mult),
                     reads=[BPS[1], Bs2], writes=[dst_buf])
            else:
                P.op('dve', lambda e: e.tensor_tensor(out=dst_ap, in0=bank(1), in1=gain_src, op=ALU.mult), reads=[BPS[1], Bs2], writes=[dst_buf])

        def col_extract(n, dst_lat, dst_ctx, half):
            for cc in range(4):
                for r, dst in ((0, dst_lat), (1, dst_ctx)):
                    P.op('pe', lambda e, cc=cc, r=r: e.matmul(bank(2)[:, (r * 4 + cc):(r * 4 + cc) + 1], lhsT=MROW[0:2, cc * 128:(cc + 1) * 128],
                                                               rhs=c_e01[0:2, r:r + 1], start=True, stop=True),
                         reads=[Bs5, Bcst], writes=[BPS[2]])
            P.op('dve', lambda e: e.tensor_copy(out=dst_lat[:, half * 4:half * 4 + 4], in_=bank(2)[:, 0:4]), reads=[BPS[2]], writes=[Bsm])
            P.op('dve', lambda e: e.tensor_copy(out=dst_ctx[:, half * 4:half * 4 + 4], in_=bank(2)[:, 4:8]), reads=[BPS[2]], writes=[Bsm])

        for n in range(4):
            mod_block(n, n % 5)
            if n < 2:
                col_extract(n, SH1, SH1C, n)
            else:
                col_extract(n, A1, A1C, n - 2)
        for dst in (A1, A1C):
            P.op('dve', lambda e, dst=dst: e.scalar_tensor_tensor(out=dst, in0=dst, scalar=1.0, in1=PMG, op0=ALU.add, op1=ALU.mult), reads=[Bsm], writes=[Bsm])

        ckpt(2)
        P.barrier()
        load_piece(win_d[:, 1536 + 1 * 512:1536 + 2 * 512], 0)
        load_piece(win_d[:, 1536 + 2 * 512:1536 + 3 * 512], 1)
        load_piece(win_d[:, 512:1024], 2)
        load_piece(win_d[:, 1024:1536], 3)
        load_piece(win_d[:, 0:512], 4)

        STN = SMALL[:, 316:318]
        Bstn = Buf("stn")

        def load_x(src_d, row0, xbuf_i):
            xc, bxc = XC[xbuf_i], Bxc[xbuf_i]
            P.dma('sp', lambda e: e.dma_start(out=xc, in_=src_d[row0:row0 + 128, :]), writes=[bxc])

        def norm_compute(a_col, sh_col, xbuf_i, dst_ap, dst_buf):
            xc, bxc = XC[xbuf_i], Bxc[xbuf_i]
            P.op('act', lambda e: e.activation(out=XS, in_=xc, func=AF.Square, accum_out=STN[:, 0:1]), reads=[bxc], writes=[Bxs, Bstn])
            P.op('act', lambda e: e.activation(out=STN[:, 1:2], in_=STN[:, 0:1], func=AF.Sqrt, scale=1.0 / D, bias=c_eps), reads=[Bstn, Bcst], writes=[Bstn])
            P.op('dve', lambda e: e.reciprocal(out=STN[:, 1:2], in_=STN[:, 1:2]), reads=[Bstn], writes=[Bstn])
            P.op('act', lambda e: e.activation(out=XS, in_=xc, func=AF.Copy, scale=STN[:, 1:2]), reads=[bxc, Bstn], writes=[Bxs])
            tb = bank_bf(0).rearrange("p (c t) -> p c t", c=8)
            for dc in range(8):
                P.op('pe', lambda e, dc=dc: e.transpose(tb[:, dc, :], XS[:, dc * 128:(dc + 1) * 128], IDB), reads=[Bxs, Bidb], writes=[BPS[0]])
            for dc in range(8):
                P.op('dve', lambda e, dc=dc: e.tensor_scalar(out=dst_ap[:, dc, :], in0=tb[:, dc, :], scalar1=a_col[:, dc:dc + 1], scalar2=sh_col[:, dc:dc + 1], op0=ALU.mult, op1=ALU.add),
                     reads=[BPS[0], Bsm], writes=[dst_buf])

        def norm_transpose(src_d, row0, a_col, sh_col, xbuf_i, dst_ap, dst_buf, keep_x=False):
            load_x(src_d, row0, xbuf_i)
            norm_compute(a_col, sh_col, xbuf_i, dst_ap, dst_buf)

        def rotary(ps_ap, cos_ap, sin_ap, dst_ap, dst_buf, ps_buf, tab_buf):
            pv = ps_ap.rearrange("p (h t d) -> p h t d", h=4, t=2)
            dv = dst_ap.rearrange("p (h t d) -> p h t d", h=4, t=2)
            cb = cos_ap.unsqueeze(1).to_broadcast([128, 4, 64])
            sb = sin_ap.unsqueeze(1).to_broadcast([128, 4, 64])
            P.op('dve', lambda e: e.tensor_tensor(out=TMPA, in0=pv[:, :, 0, :], in1=cb, op=ALU.mult), reads=[ps_buf, tab_buf], writes=[Btmp])
            P.op('dve', lambda e: e.tensor_tensor(out=TMPB, in0=pv[:, :, 1, :], in1=sb, op=ALU.mult), reads=[ps_buf, tab_buf], writes=[Btmp])
            P.op('dve', lambda e: e.tensor_tensor(out=dv[:, :, 0, :], in0=TMPA, in1=TMPB, op=ALU.subtract), reads=[Btmp], writes=[dst_buf])
            P.op('dve', lambda e: e.tensor_tensor(out=TMPA, in0=pv[:, :, 0, :], in1=sb, op=ALU.mult), reads=[ps_buf, tab_buf], writes=[Btmp])
            P.op('dve', lambda e: e.tensor_tensor(out=TMPB, in0=pv[:, :, 1, :], in1=cb, op=ALU.mult), reads=[ps_buf, tab_buf], writes=[Btmp])
            P.op('dve', lambda e: e.tensor_tensor(out=dv[:, :, 1, :], in0=TMPA, in1=TMPB, op=ALU.add), reads=[Btmp], writes=[dst_buf])

        def kv_chunk(hx_ap, hx_buf, cos_ap, sin_ap, tab_buf, zf_ap, zb_ap, v_dst, v_buf, ck=False, mid=None):
            for dc in range(8):
                P.op('pe', lambda e, dc=dc: e.matmul(bank(1), lhsT=hx_ap[:, dc, :], rhs=WSL[0][:, dc, :], start=(dc == 0), stop=(dc == 7)),
                     reads=[hx_buf, Bw[0]], writes=[BPS[1]])
            for dc in range(8):
                P.op('pe', lambda e, dc=dc: e.matmul(bank(2), lhsT=hx_ap[:, dc, :], rhs=WSL[1][:, dc, :], start=(dc == 0), stop=(dc == 7)),
                     reads=[hx_buf, Bw[1]], writes=[BPS[2]])
            if mid is not None:
                mid()
            P.op('act', lambda e: e.activation(out=v_dst, in_=bank(2), func=AF.Copy), reads=[BPS[2]], writes=[v_buf])
            if ck:
                ckpt(22)
            rotary(bank(1), cos_ap, sin_ap, KR, Bkr, BPS[1], tab_buf)
            if ck:
                ckpt(23)
            for h in range(4):
                hs = slice(h * 128, (h + 1) * 128)
                P.op('dve', lambda e, h=h, hs=hs: e.tensor_scalar(out=KFs[:, hs], in0=KR[:, hs], scalar1=zf_ap[:, h:h + 1], scalar2=None, op0=ALU.mult), reads=[Bkr, Bsm], writes=[Bkf])
                P.op('dve', lambda e, h=h, hs=hs: e.tensor_scalar(out=KBs[:, hs], in0=KR[:, hs], scalar1=zb_ap[:, h:h + 1], scalar2=None, op0=ALU.mult), reads=[Bkr, Bsm], writes=[Bkb])

        VC = S3.bitcast(BF16)[:, 0:512]
        SCF = A.at(T0 + 27, [4, 128], F32)
        SCB = A.at(T0 + 29, [4, 128], F32)
        xco = XCO.rearrange("p (s h) -> p s h", h=4)
        s1v_ = S1.rearrange("p (h d) -> p h d", h=4)
        for c in range(2):
            norm_transpose(ctx_d, c * 128, A1C, SH1C, c % 2, HXTC, Bhxtc)
            if c == 0:
                ckpt(21)
            kv_chunk(HXTC, Bhxtc, ROTC[:, 0, c, :], ROTC[:, 1, c, :], Brotc, CZF[:, c * 4:c * 4 + 4], CZB[:, c * 4:c * 4 + 4], VC, Bs3, ck=(c == 0))
            for h in range(4):
                hs = slice(h * 128, (h + 1) * 128)
                P.op('pe', lambda e, hs=hs: e.matmul(bank(4)[:, hs], lhsT=KFs[:, hs], rhs=VC[:, hs], start=True, stop=True),
                     reads=[Bkf, Bs3], writes=[BPS[4]])
            for h in range(4):
                hs = slice(h * 128, (h + 1) * 128)
                P.op('pe', lambda e, hs=hs: e.matmul(bank(5)[:, hs], lhsT=KBs[:, hs], rhs=VC[:, hs], start=True, stop=True),
                     reads=[Bkb, Bs3], writes=[BPS[5]])
            if c == 0:
                ckpt(25)
            for (dstv, bi, col) in ((SCF, 4, 8), (SCB, 5, 9)):
                for h in range(4):
                    hs = slice(h * 128, (h + 1) * 128)
                    if c == 0:
                        P.op('dve', lambda e, dstv=dstv, bi=bi, col=col, h=h, hs=hs: e.tensor_scalar(out=dstv[:, h, :], in0=bank(bi)[:, hs], scalar1=xco[:, col, h:h + 1], scalar2=None, op0=ALU.mult),
                             reads=[BPS[bi], Bsm], writes=[Bs5])
                    else:
                        P.op('dve', lambda e, dstv=dstv, bi=bi, col=col, h=h, hs=hs: e.scalar_tensor_tensor(out=dstv[:, h, :], in0=bank(bi)[:, hs], scalar=xco[:, col, h:h + 1], in1=dstv[:, h, :], op0=ALU.mult, op1=ALU.add),
                             reads=[BPS[bi], Bsm, Bs5], writes=[Bs5])
        tap("scf", SCF.rearrange("p h d -> p (h d)"), Bs5)

        ckpt(3)
        P.op('dve', lambda e: e.memset(LF, 0.0), writes=[Blf])
        P.op('dve', lambda e: e.memset(LB, 0.0), writes=[Blb])
        cfv = CF.rearrange("p (c h) -> p c h", h=4)
        cbv = CB.rearrange("p (c h) -> p c h", h=4)
        def pre_norm(c):
            norm_compute(A1, SH1, c % 2, HXTG[:, :, (c % 4) * 128:(c % 4) * 128 + 128], Bhxtg)

        load_x(x_d, 0, 0)
        pre_norm(0)
        for c in range(NCH):
            g = c // 4
            if c + 1 < NCH:
                load_x(x_d, (c + 1) * 128, (c + 1) % 2)
            hx = HXTG[:, :, (c % 4) * 128:(c % 4) * 128 + 128]
            kv_chunk(hx, Bhxtg, ROT[:, 0, c, :], ROT[:, 1, c, :], Brot, ZF, ZB, V[:, c, :], Bv[c],
                     mid=(lambda c=c: pre_norm(c + 1)) if (c + 1 < NCH and c % 4 != 3) else None)
            if c == 0:
                ckpt(32)
            tb = bank_bf(3).rearrange("p (h t) -> p h t", h=8)
            for h in range(4):
                P.op('pe', lambda e, h=h: e.transpose(tb[:, h, :], KR[:, h * 128:(h + 1) * 128], IDB), reads=[Bkr, Bidb], writes=[BPS[3]])
            P.op('act', lambda e, c=c: e.activation(out=KT[:, :, c * 128:(c + 1) * 128], in_=tb[:, 0:4, :], func=AF.Copy), reads=[BPS[3]], writes=[Bkt[c]])
            if c == 0:
                ckpt(33)
            for h in range(4):
                hs = slice(h * 128, (h + 1) * 128)
                P.op('pe', lambda e, hs=hs, c=c: e.matmul(bank(4)[:, hs], lhsT=KFs[:, hs], rhs=V[:, c, hs], start=True, stop=True),
                     reads=[Bkf, Bv[c]], writes=[BPS[4]])
            for h in range(4):
                hs = slice(h * 128, (h + 1) * 128)
                P.op('pe', lambda e, hs=hs, c=c: e.matmul(bank(5)[:, hs], lhsT=KBs[:, hs], rhs=V[:, c, hs], start=True, stop=True),
                     reads=[Bkb, Bv[c]], writes=[BPS[5]])
            P.op('act', lambda e, c=c: e.activation(out=UF[:, c, :], in_=bank(4), func=AF.Copy), reads=[BPS[4]], writes=[Buf_[c]])
            P.op('act', lambda e, c=c: e.activation(out=UB[:, c, :], in_=bank(5), func=AF.Copy), reads=[BPS[5]], writes=[Bub[c]])
            if c == 0:
                ckpt(34)
            for h in range(4):
                hs = slice(h * 128, (h + 1) * 128)
                P.op('dve', lambda e, c=c, h=h, hs=hs: e.scalar_tensor_tensor(out=LF[:, h, :], in0=bank(4)[:, hs], scalar=cfv[:, c, h:h + 1], in1=LF[:, h, :], op0=ALU.mult, op1=ALU.add),
                     reads=[BPS[4], Bsm, Blf], writes=[Blf])
                P.op('dve', lambda e, c=c, h=h, hs=hs: e.scalar_tensor_tensor(out=LB[:, h, :], in0=bank(5)[:, hs], scalar=cbv[:, c, h:h + 1], in1=LB[:, h, :], op0=ALU.mult, op1=ALU.add),
                     reads=[BPS[5], Bsm, Blb], writes=[Blb])
            if c == 0:
                ckpt(341)
            if c == 1:
                ckpt(342)
            if c == 2:
                ckpt(35)
            if c % 4 == 3:
                ckpt(36) if c == 3 else None
                for j in range(4):
                    js = slice(j * 128, (j + 1) * 128)
                    for bi, slot in ((6, 2), (7, 3), (3, 4)):
                        for dc in range(8):
                            P.op('pe', lambda e, dc=dc, bi=bi, slot=slot, js=js: e.matmul(bank(bi), lhsT=WSL[slot][:, dc, js], rhs=HXTG[:, dc, :], start=(dc == 0), stop=(dc == 7)),
                                 reads=[Bhxtg, Bw[slot]], writes=[BPS[bi]])
                    P.op('act', lambda e: e.activation(out=S2, in_=bank(6), func=AF.Copy), reads=[BPS[6]], writes=[Bs2])
                    P.op('dve', lambda e: e.tensor_tensor(out=S2, in0=S2, in1=bank(7), op=ALU.mult), reads=[BPS[7], Bs2], writes=[Bs2])
                    P.op('dve', lambda e, j=j: e.tensor_scalar(out=S3, in0=S2, scalar1=CW[:, j * 3 + 1:j * 3 + 2], scalar2=CBI[:, j:j + 1], op0=ALU.mult, op1=ALU.add),
                         reads=[Bs2, Bsm], writes=[Bs3])
                    u3 = S2.rearrange("p (r w) -> p r w", w=64)
                    y3 = S3.rearrange("p (r w) -> p r w", w=64)
                    P.op('dve', lambda e, j=j: e.scalar_tensor_tensor(out=y3[:, :, 1:64], in0=u3[:, :, 0:63], scalar=CW[:, j * 3:j * 3 + 1], in1=y3[:, :, 1:64], op0=ALU.mult, op1=ALU.add),
                         reads=[Bs2, Bs3, Bsm], writes=[Bs3])
                    P.op('dve', lambda e, j=j: e.scalar_tensor_tensor(out=y3[:, :, 0:63], in0=u3[:, :, 1:64], scalar=CW[:, j * 3 + 2:j * 3 + 3], in1=y3[:, :, 0:63], op0=ALU.mult, op1=ALU.add),
                         reads=[Bs2, Bs3, Bsm], writes=[Bs3])
                    P.op('dve', lambda e, j=j, g=g: e.tensor_tensor(out=CONVT[:, j, g * 512:(g + 1) * 512], in0=S3, in1=bank(3), op=ALU.mult),
                         reads=[Bs3, BPS[3]], writes=[Bconvt[g]])
                if c + 1 < NCH:
                    pre_norm(c + 1)

        ckpt(4)
        Bag1i, Bag1o = Buf("ag1i"), Buf("ag1o")
        P.dma('sp', lambda e: e.dma_start(out=ag1_in.ap()[:, 0:512], in_=LF.rearrange("p h d -> p (h d)")), reads=[Blf], writes=[Bag1i])
        P.dma('sp', lambda e: e.dma_start(out=ag1_in.ap()[:, 512:1024], in_=LB.rearrange("p h d -> p (h d)")), reads=[Blb], writes=[Bag1i])
        P.dma('pool', lambda e: e.collective_compute("AllGather", ALU.bypass, replica_groups=[[0, 1, 2, 3], [4, 5, 6, 7]],
                                                     ins=[ag1_in.ap().opt()], outs=[ag1_out.ap().opt()]),
              reads=[Bag1i], writes=[Bag1o], inc=1)
        G1 = ROWS[:, 0, :]; A2 = ROWS[:, 1, :]; SH2 = ROWS[:, 2, :]
        W5 = A.at(T0, [8, 512], BF16)
        BADA_b = A.at(T0 + 10, [512], F32, parts=2)
        MROW_b = A.at(T0 + 12, [512], F32, parts=2)
        GAIN_b = A.at(T0 + 14, [512], F32)
        Bw5s = Buf("w5s")
        mods = []
        for (blk0, dst, gsrc, p1) in ((4, G1, postmix_d, False), (8, A2, preffn_d, True), (6, SH2, None, False)):
            for hf in range(2):
                mods.append((blk0 + hf, dst, gsrc, p1, hf))

        def emit_mod(i):
            n, dst, gsrc, p1, hf = mods[i]
            if i % 2 == 0:
                wsl, bw = WSL[4], [Bw[4]]
            else:
                wsl, bw = W5, [Bxc[0], Bxc[1]]
            P.dma('pool', lambda e: e.dma_start(out=wsl, in_=wada_d[:, n * 512:(n + 1) * 512].rearrange("(c p) f -> p c f", p=128)), writes=bw,
                  sembuf=(Bw[4] if i % 2 == 0 else Bw5s))
            P.dma('sp', lambda e: e.dma_start(out=BADA_b, in_=bada_d[:, n * 512:(n + 1) * 512]), writes=[Bhxtc])
            for dc in range(8):
                P.op('pe', lambda e, dc=dc: e.matmul(bank(0)[0:2, :], lhsT=ST_[:, dc, :], rhs=wsl[:, dc, :], start=(dc == 0), stop=(dc == 7)),
                     reads=[Bcv] + bw[:1], writes=[BPS[0]])
            P.op('dve', lambda e: e.tensor_tensor(out=MROW_b, in0=bank(0)[0:2, :], in1=BADA_b, op=ALU.add), reads=[BPS[0], Bhxtc], writes=[Btmp])
            if gsrc is not None:
                P.dma('sp', lambda e: e.dma_start(out=GAIN_b, in_=gsrc[:, hf * 512:(hf + 1) * 512]), writes=[Bkr, Bkf])
            P.op('pe', lambda e: e.matmul(bank(1), lhsT=c_sel0, rhs=MROW_b, start=True, stop=True), reads=[Btmp, Bcst], writes=[BPS[1]])
            d = dst[:, hf * 512:(hf + 1) * 512]
            if gsrc is None:
                P.op('act', lambda e: e.activation(out=d, in_=bank(1), func=AF.Copy), reads=[BPS[1]], writes=[Brows, Bhxtg])
            elif p1:
                P.op('dve', lambda e: e.scalar_tensor_tensor(out=d, in0=bank(1), scalar=1.0, in1=GAIN_b, op0=ALU.add, op1=ALU.mult),
                     reads=[BPS[1], Bkr, Bkf], writes=[Brows, Bhxtg])
            else:
                P.op('dve', lambda e: e.tensor_tensor(out=d, in0=bank(1), in1=GAIN_b, op=ALU.mult), reads=[BPS[1], Bkr, Bkf], writes=[Brows, Bhxtg])

        emit_mod(0)
        emit_mod(1)
        load_piece(win_d[:, 1536:2048], 0)
        load_piece(win_d[:, 1536 + 3 * 512:3584], 1)
        load_piece(wout_d[:, 0:512], 2)
        load_piece(wout_d[:, 512:1024], 3)
        P.dma('sp', lambda e: e.dma_start(out=ROT, in_=rotq_d), writes=[Brot])
        SINF = S1.rearrange("p (h d) -> p h d", h=4)
        SINB = S2.rearrange("p (h d) -> p h d", h=4)
        P.op('dve', lambda e: e.tensor_copy(out=SINF, in_=SCF), reads=[Bs5], writes=[Bs1])
        P.op('dve', lambda e: e.tensor_copy(out=SINB, in_=SCB), reads=[Bs5], writes=[Bs2])
        for s in range(4):
            P.dma('sp', lambda e, s=s: e.dma_start(out=S4, in_=ag1_out.ap()[s * 128:(s + 1) * 128, :]), reads=[Bag1o], writes=[Bs4])
            s4f = S4[:, 0:512].rearrange("p (h d) -> p h d", h=4)
            s4b = S4[:, 512:1024].rearrange("p (h d) -> p h d", h=4)
            for h in range(4):
                P.op('dve', lambda e, s=s, h=h, s4f=s4f: e.scalar_tensor_tensor(out=SINF[:, h, :], in0=s4f[:, h, :], scalar=xco[:, s, h:h + 1], in1=SINF[:, h, :], op0=ALU.mult, op1=ALU.add),
                     reads=[Bs4, Bsm, Bs1], writes=[Bs1])
                P.op('dve', lambda e, s=s, h=h, s4b=s4b: e.scalar_tensor_tensor(out=SINB[:, h, :], in0=s4b[:, h, :], scalar=xco[:, 4 + s, h:h + 1], in1=SINB[:, h, :], op0=ALU.mult, op1=ALU.add),
                     reads=[Bs4, Bsm, Bs2], writes=[Bs2])
        tap("sinf", S1, Bs1)
        tap("sinb", S2, Bs2)
        def v4(ap):
            return ap.rearrange("p (h d) -> p h d", h=4)
        Bs4a, Bs4b, Bs5a = Buf("s4a"), Buf("s4b"), Buf("s5a")
        FB = [(SINF, Bs1), (v4(S3), Bs3), (v4(S4[:, 0:512]), Bs4a)]
        BB = [(SINB, Bs2), (v4(S4[:, 512:1024]), Bs4b), (v4(S5[:, 0:512]), Bs5a)]
        first_extra = {id(Bs4a): [Bs4], id(Bs4b): [Bs4], id(Bs5a): [Bs5]}

        def scan_step(k, c, bufs, U, BU, goff):
            (cur, bcur), (nxt, bnxt) = bufs[k % 3], bufs[(k + 1) % 3]
            uc = v4(U[:, c, :])
            extra = first_extra.pop(id(bnxt), [])
            for h in range(4):
                P.op('dve', lambda e, h=h: e.scalar_tensor_tensor(out=nxt[:, h, :], in0=cur[:, h, :], scalar=G128[:, goff + h:goff + h + 1], in1=uc[:, h, :], op0=ALU.mult, op1=ALU.add),
                     reads=[bcur, Bsm, BU[c]], writes=[bnxt] + (extra if h == 0 else []))
            P.op('act', lambda e: e.activation(out=uc, in_=cur, func=AF.Copy), reads=[bcur], writes=[BU[c]])

        for k in range(NCH):
            scan_step(k, k, FB, UF, Buf_, 0)
            scan_step(k, NCH - 1 - k, BB, UB, Bub, 4)
            if k in (1, 5, 9, 13):
                emit_mod(2 + (k - 1) // 4)
        tap("rows", ROWS.rearrange("p a b -> p (a b)"), Brows)
        P.barrier()

        ckpt(6)
        Bxm, Bhx2 = Buf("xm_scr"), Buf("hx2_scr")
        WR = A.at(152, [8, 16], F32)
        Bwr = Buf("wr")
        P.dma('sp', lambda e: e.dma_start(out=WR, in_=wr_d), reads=[Brotc], writes=[Bwr])
        QT = A.at(T0 + 15, [4, 128], BF16)
        QFT = A.at(T0 + 16, [4, 128], BF16)
        QBT = A.at(T0 + 12, [4, 128], BF16)
        XSJ = A.at(136, [D], BF16)
        XSH = A.at(138, [D], BF16)
        Bxsj, Bxsh = Buf("xsj"), Buf("xsh")
        load_x(x_d, 0, 0)
        norm_compute(A1, SH1, 0, HXTC, Bhxtc)
        Bst = Buf("stat")

        def qg_proj():
            for dc in range(8):
                P.op('pe', lambda e, dc=dc: e.matmul(bank(4), lhsT=HXTC[:, dc, :], rhs=WSL[0][:, dc, :], start=(dc == 0), stop=(dc == 7)),
                     reads=[Bhxtc, Bw[0]], writes=[BPS[4]])
            for dc in range(8):
                P.op('pe', lambda e, dc=dc: e.matmul(bank(5), lhsT=HXTC[:, dc, :], rhs=WSL[1][:, dc, :], start=(dc == 0), stop=(dc == 7)),
                     reads=[Bhxtc, Bw[1]], writes=[BPS[5]])

        qg_proj()
        for c in range(NCH):
            cs = slice(c * 128, (c + 1) * 128)
            xi = c % 2
            w4 = [Bw[4]] if c == 0 else []
            if c + 1 < NCH:
                load_x(x_d, (c + 1) * 128, (c + 1) % 2)
            rotary(bank(4), ROT[:, 0, c, :], ROT[:, 1, c, :], KR, Bkr, BPS[4], Brot)
            P.op('act', lambda e: e.activation(out=S1, in_=bank(5), func=AF.Silu), reads=[BPS[5]], writes=[Bs1])
            tb = bank_bf(3).rearrange("p (h t) -> p h t", h=8)
            for h in range(4):
                P.op('pe', lambda e, h=h: e.transpose(tb[:, h, :], KR[:, h * 128:(h + 1) * 128], IDB), reads=[Bkr, Bidb], writes=[BPS[3]])
            P.op('act', lambda e: e.activation(out=QT, in_=tb[:, 0:4, :], func=AF.Copy), reads=[BPS[3]], writes=[Bkf])
            P.op('dve', lambda e: e.tensor_tensor(out=QFT, in0=tb[:, 0:4, :], in1=XF, op=ALU.mult), reads=[BPS[3], Btab], writes=[Bkb])
            P.op('dve', lambda e: e.tensor_tensor(out=QBT, in0=tb[:, 0:4, :], in1=XB, op=ALU.mult), reads=[BPS[3], Btab], writes=[Btmp])
            for h in range(4):
                P.op('pe', lambda e, h=h, cs=cs: e.matmul(bank(4)[:, h * 128:(h + 1) * 128], lhsT=KT[:, h, cs], rhs=QT[:, h, :], start=True, stop=True),
                     reads=[Bkt[c], Bkf], writes=[BPS[4]])
            STb = S2.bitcast(BF16)[:, 0:512]
            P.op('dve', lambda e: e.tensor_tensor(out=STb.rearrange("p (h i) -> p h i", h=4), in0=bank(4).rearrange("p (h i) -> p h i", h=4), in1=DT, op=ALU.mult),
                 reads=[BPS[4], Btab], writes=[Bs2])
            for h in range(4):
                hs = slice(h * 128, (h + 1) * 128)
                P.op('pe', lambda e, hs=hs, c=c: e.matmul(bank(5)[:, hs], lhsT=STb[:, hs], rhs=V[:, c, hs], start=True, stop=False), reads=[Bs2, Bv[c]], writes=[BPS[5]])
                P.op('pe', lambda e, hs=hs, h=h, c=c: e.matmul(bank(5)[:, hs], lhsT=QFT[:, h, :], rhs=UF[:, c, hs], start=False, stop=False), reads=[Bkb, Buf_[c]], writes=[BPS[5]])
                P.op('pe', lambda e, hs=hs, h=h, c=c: e.matmul(bank(5)[:, hs], lhsT=QBT[:, h, :], rhs=UB[:, c, hs], start=False, stop=True), reads=[Btmp, Bub[c]], writes=[BPS[5]])
            if c == 0:
                P.op('act', lambda e: e.activation(out=S3, in_=bank(5), func=AF.Copy), reads=[BPS[5]], writes=[Bs3])
                tap("y0", S3, Bs3)
            yv = bank(5).rearrange("p (h d) -> p h d", h=4)
            P.op('act', lambda e: e.activation(out=S3, in_=bank(5), func=AF.Square), reads=[BPS[5]], writes=[Bs3])
            P.op('dve', lambda e: e.tensor_reduce(out=STAT[:, 4:8], in_=yv, axis=AX.X, op=ALU.add), reads=[BPS[5]], writes=[Bst])
            P.op('dve', lambda e: e.tensor_reduce(out=STAT[:, 8:12], in_=S3.rearrange("p (h d) -> p h d", h=4), axis=AX.X, op=ALU.add), reads=[Bs3], writes=[Bst])
            P.op('dve', lambda e: e.tensor_scalar(out=STAT[:, 4:8], in0=STAT[:, 4:8], scalar1=1.0 / 128, scalar2=None, op0=ALU.mult), reads=[Bst], writes=[Bst])
            P.op('dve', lambda e: e.tensor_tensor(out=STAT[:, 12:16], in0=STAT[:, 4:8], in1=STAT[:, 4:8], op=ALU.mult), reads=[Bst], writes=[Bst])
            P.op('dve', lambda e: e.scalar_tensor_tensor(out=STAT[:, 8:12], in0=STAT[:, 8:12], scalar=1.0 / 128, in1=STAT[:, 12:16], op0=ALU.mult, op1=ALU.subtract), reads=[Bst], writes=[Bst])
            P.op('act', lambda e: e.activation(out=STAT[:, 8:12], in_=STAT[:, 8:12], func=AF.Sqrt, bias=c_eps), reads=[Bst, Bcst], writes=[Bst])
            P.op('dve', lambda e: e.reciprocal(out=STAT[:, 8:12], in_=STAT[:, 8:12]), reads=[Bst], writes=[Bst])
            for h in range(4):
                hs = slice(h * 128, (h + 1) * 128)
                P.op('dve', lambda e, h=h, hs=hs: e.tensor_scalar(out=S3[:, hs], in0=bank(5)[:, hs], scalar1=STAT[:, 4 + h:5 + h], scalar2=STAT[:, 8 + h:9 + h], op0=ALU.subtract, op1=ALU.mult),
                     reads=[BPS[5], Bst], writes=[Bs3])
            P.op('dve', lambda e: e.tensor_tensor(out=S3, in0=S3, in1=NGR, op=ALU.mult), reads=[Bs3, Bngr], writes=[Bs3])
            P.op('dve', lambda e: e.tensor_tensor(out=KR, in0=S3, in1=S1, op=ALU.mult), reads=[Bs3, Bs1], writes=[Bkr])
            for h in range(4):
                P.op('pe', lambda e, h=h: e.transpose(tb[:, 4 + h, :], KR[:, h * 128:(h + 1) * 128], IDB), reads=[Bkr, Bidb], writes=[BPS[3]])
            ROTt = S2.bitcast(BF16)[:, 512:1024].rearrange("p (h t) -> p h t", h=4)
            P.op('act', lambda e: e.activation(out=ROTt, in_=tb[:, 4:8, :], func=AF.Copy), reads=[BPS[3]], writes=[Bs2])
            for hf in range(2):
                for kc in range(8):
                    if kc < 4:
                        lhs = CONVT[:, kc, cs]
                        rd = [Bconvt[c // 4]]
                    else:
                        lhs = ROTt[:, kc - 4, :]
                        rd = [Bs2]
                    P.op('pe', lambda e, hf=hf, kc=kc, lhs=lhs: e.matmul(bank(6 + hf), lhsT=lhs, rhs=WSL[2 + hf][:, kc, :], start=(kc == 0), stop=(kc == 7)),
                         reads=rd + [Bw[2 + hf]], writes=[BPS[6 + hf]])
            if c + 1 < NCH:
                norm_compute(A1, SH1, (c + 1) % 2, HXTC, Bhxtc)
                qg_proj()
            M = psb[3][:, :]
            P.op('act', lambda e: e.activation(out=XSJ, in_=M, func=AF.Square, accum_out=STAT[:, 16:17]), reads=[BPS[6], BPS[7]], writes=[Bxsj, Bst] + w4)
            P.op('act', lambda e: e.activation(out=STAT[:, 17:18], in_=STAT[:, 16:17], func=AF.Sqrt, scale=1.0 / D, bias=c_eps), reads=[Bst, Bcst], writes=[Bst])
            P.op('dve', lambda e: e.reciprocal(out=STAT[:, 17:18], in_=STAT[:, 17:18]), reads=[Bst], writes=[Bst])
            P.op('dve', lambda e: e.scalar_tensor_tensor(out=S4, in0=M, scalar=STAT[:, 17:18], in1=G1, op0=ALU.mult, op1=ALU.mult), reads=[BPS[6], BPS[7], Bst, Brows], writes=[Bs4])
            P.op('dve', lambda e, xi=xi: e.tensor_tensor(out=S4, in0=S4, in1=XC[xi], op=ALU.add), reads=[Bs4, Bxc[xi]], writes=[Bs4])
            P.dma('sp', lambda e, c=c: e.dma_start(out=xm_scr.ap()[c * 128:(c + 1) * 128, :], in_=S4), reads=[Bs4], writes=[Bxm])
            if c == 0:
                tap("xm0", S4, Bs4)
            P.op('act', lambda e: e.activation(out=XSJ, in_=S4, func=AF.Square, accum_out=STAT[:, 18:19]), reads=[Bs4], writes=[Bxsj, Bst])
            P.op('act', lambda e: e.activation(out=STAT[:, 19:20], in_=STAT[:, 18:19], func=AF.Sqrt, scale=1.0 / D, bias=c_eps), reads=[Bst, Bcst], writes=[Bst])
            P.op('dve', lambda e: e.reciprocal(out=STAT[:, 19:20], in_=STAT[:, 19:20]), reads=[Bst], writes=[Bst])
            P.op('dve', lambda e: e.scalar_tensor_tensor(out=S5, in0=S4, scalar=STAT[:, 19:20], in1=A2, op0=ALU.mult, op1=ALU.mult), reads=[Bs4, Bst, Brows], writes=[Bs5])
            P.op('dve', lambda e: e.tensor_tensor(out=S5, in0=S5, in1=SH2, op=ALU.add), reads=[Bs5, Brows], writes=[Bs5])
            P.op('act', lambda e: e.activation(out=XSH, in_=S5, func=AF.Copy), reads=[Bs5], writes=[Bxsh] + w4)
            P.dma('sp', lambda e, c=c: e.dma_start(out=hx2_scr.ap()[c * 128:(c + 1) * 128, :], in_=XSH), reads=[Bxsh], writes=[Bhx2])
            tf = psb[0][:, :].rearrange("p (c t) -> p c t", c=8)
            for dc in range(8):
                P.op('pe', lambda e, dc=dc: e.transpose(tf[:, dc, :], S5[:, dc * 128:(dc + 1) * 128], c_identf), reads=[Bs5, Bcst], writes=[BPS[0], BPS[1]])
            HX2T = S4.rearrange("p (c t) -> p c t", c=8)
            P.op('act', lambda e: e.activation(out=HX2T, in_=tf, func=AF.Copy), reads=[BPS[0], BPS[1]], writes=[Bs4])
            for dc in range(8):
                P.op('pe', lambda e, dc=dc: e.matmul(bank(2)[:, 0:16], lhsT=HX2T[:, dc, :], rhs=WR[:, dc, :], start=(dc == 0), stop=(dc == 7)), reads=[Bs4, Bwr], writes=[BPS[2]])
            P.op('dve', lambda e: e.tensor_tensor(out=STAT[:, 0:16], in0=bank(2)[:, 0:16], in1=BRR, op=ALU.add), reads=[BPS[2], Bsm], writes=[Bst])
            P.op('dve', lambda e: e.tensor_reduce(out=STAT[:, 20:21], in_=STAT[:, 0:16], axis=AX.X, op=ALU.max), reads=[Bst], writes=[Bst])
            P.op('dve', lambda e: e.tensor_scalar(out=STAT[:, 20:21], in0=STAT[:, 20:21], scalar1=-1.0, scalar2=None, op0=ALU.mult), reads=[Bst], writes=[Bst])
            P.op('act', lambda e: e.activation(out=STAT[:, 0:16], in_=STAT[:, 0:16], func=AF.Exp, bias=STAT[:, 20:21], accum_out=STAT[:, 21:22]), reads=[Bst], writes=[Bst])
            P.op('dve', lambda e: e.reciprocal(out=STAT[:, 21:22], in_=STAT[:, 21:22]), reads=[Bst], writes=[Bst])
            P.op('dve', lambda e, c=c: e.tensor_scalar(out=AFF[:, c, :], in0=STAT[:, 0:16], scalar1=STAT[:, 21:22], scalar2=None, op0=ALU.mult), reads=[Bst], writes=[Baff])
        tap("aff", AFF.rearrange("p c e -> p (c e)"), Baff)

        ckpt(7)
        P.barrier()
        ACC = A.at(24, [NCH, D], F32)
        HX2 = A.at(88, [NCH, D], BF16)
        Bacc = [Buf(f"acc{c}") for c in range(NCH)]
        Bhx = Buf("hx2")
        RING = [A.at(120 + 8 * i, [8, 512], BF16) for i in range(5)]
        Bring = [Buf(f"ring{i}") for i in range(5)]
        XST = A.at(160, [8, NS], BF16)
        HT = A.at(166, [16, NS], BF16)
        YG = A.at(178, [3, D], BF16)
        PT_ = A.at(184, [NCH, CAPG], BF16)
        PTT = A.at(190, [4, D], BF16)
        Bxst, Bht, Byg, Bpt, Bptt = Buf("xst"), Buf("ht"), Buf("yg"), Buf("pt"), Buf("ptt")
        SA = A.at(198, [NS], F32)
        Bsa = Buf("sa")
        AFFEM = A.at(160, [TPC], F32, parts=16)
        MASK = A.at(168, [TPC], F32, parts=16)
        CUM = A.at(176, [TPC], F32, parts=16)
        AFFALL = A.at(184, [1024], F32)
        Baffem, Bmask, Bcum, Baffall = Buf("affem"), Buf("mask"), Buf("cum"), Buf("affall")
        CMPS = A.at(188, [1024], F32)
        Bcmp = Buf('cmp')
        ONES16 = A.at(192, [1024], F32, parts=16)
        Bones16 = Buf('ones16')
        P.op('dve', lambda e: e.memset(ONES16, 1.0), writes=[Bones16])
        P.dma('sp', lambda e: e.dma_start(out=HX2, in_=hx2_scr.ap().rearrange("(c p) d -> p c d", p=128)), reads=[Bhx2], writes=[Bhx])
        G2 = A.at(199.5, [D], F32)
        Bg2 = Buf("g2")
        MROW2 = A.at(128, [D], F32, parts=2)
        CV2 = A.at(132, [8, 2], F32)
        ST2 = A.at(132.5, [8, 2], BF16)
        WT = [A.at(136, [8, 512], BF16), A.at(152, [8, 512], BF16)]
        BADA2 = A.at(144, [D], F32, parts=2)
        GAIN2 = A.at(148, [D], F32)
        Bfin, Bbada2, Bgain2, Bmrow2 = Buf("fin"), Buf("bada2"), Buf("gain2"), Buf("mrow2")
        Bwt = [Buf("wt0"), Buf("wt1")]
        P.dma('sp', lambda e: e.dma_start(out=CV2, in_=cvec_d), writes=[Bfin])
        P.op('act', lambda e: e.activation(out=ST2, in_=CV2, func=AF.Silu), reads=[Bfin], writes=[Bfin])
        for hf in range(2):
            P.dma('pool', lambda e, hf=hf: e.dma_start(out=WT[hf], in_=wada_d[:, (10 + hf) * 512:(11 + hf) * 512].rearrange("(c p) f -> p c f", p=128)), writes=[Bwt[hf]])
        P.dma('sp', lambda e: e.dma_start(out=BADA2, in_=bada_d[:, 10 * 512:12 * 512]), writes=[Bbada2])
        P.dma('sp', lambda e: e.dma_start(out=GAIN2, in_=postffn_d), writes=[Bgain2])
        for c in range(NCH):
            P.op('dve', lambda e, c=c: e.memset(ACC[:, c, :], 0.0), writes=[Bacc[c]])
        for c in range(NCH):
            P.op('pe', lambda e, c=c: e.transpose(bank(c // 4)[0:16, (c % 4) * 128:(c % 4) * 128 + 128], AFF[:, c, :], c_identf), reads=[Baff, Bcst], writes=[BPS[c // 4]])
        for q in range(4):
            P.op('act', lambda e, q=q: e.activation(out=AFFEM[:, q * 512:(q + 1) * 512], in_=bank(q)[0:16, :], func=AF.Copy), reads=[BPS[q]], writes=[Baffem])
        Bag2i, Bag2o = Buf("ag2i"), Buf("ag2o")
        P.dma('sp', lambda e: e.dma_start(out=ag2_in.ap(), in_=AFFEM), reads=[Baffem], writes=[Bag2i])
        P.dma('pool', lambda e: e.collective_compute("AllGather", ALU.bypass, replica_groups=[[0, 1, 2, 3], [4, 5, 6, 7]],
                                                     ins=[ag2_in.ap().opt()], outs=[ag2_out.ap().opt()]),
              reads=[Bag2i], writes=[Bag2o], inc=1)
        P.dma('sp', lambda e: e.dma_start(out=AFFALL, in_=ag2_out.ap().rearrange("r (h t) -> (r h) t", h=2)), reads=[Bag2o], writes=[Baffall])
        P.op('dve', lambda e: e.memset(MID, 0.5), writes=[Bsm])
        for k in range(1, NBIS + 1):
            hk = 2.0 ** -(k)
            hn = 2.0 ** -(k + 1)
            P.op('dve', lambda e: e.tensor_scalar(out=CMPS, in0=AFFALL, scalar1=MID, scalar2=None, op0=ALU.is_ge, op1=ALU.add, accum_out=CNT),
                 reads=[Baffall, Bsm], writes=[Bcmp, Bsm])
            P.op('pe', lambda e: e.matmul(bank(0)[:, 0:1], lhsT=c_grp, rhs=CNT, start=True, stop=True), reads=[Bsm, Bcst], writes=[BPS[0]])
            P.op('dve', lambda e, hk=hk: e.tensor_scalar(out=TT, in0=bank(0)[:, 0:1], scalar1=KCAP, scalar2=hk, op0=ALU.is_ge, op1=ALU.mult), reads=[BPS[0]], writes=[Bsm])
            P.op('dve', lambda e, hn=hn: e.scalar_tensor_tensor(out=MID, in0=MID, scalar=-hn, in1=TT, op0=ALU.add, op1=ALU.add), reads=[Bsm], writes=[Bsm])
        P.op('dve', lambda e: e.tensor_scalar(out=THR, in0=MID, scalar1=-(2.0 ** -(NBIS + 1)), scalar2=None, op0=ALU.add), reads=[Bsm], writes=[Bsm])
        P.op('pe', lambda e: e.matmul(bank(1)[0:16, 0:1], lhsT=c_sel, rhs=THR, start=True, stop=True), reads=[Bsm, Bcst], writes=[BPS[1]])
        P.op('dve', lambda e: e.tensor_copy(out=THREM[0:16, :], in_=bank(1)[0:16, 0:1]), reads=[BPS[1]], writes=[Bsm])
        tap("thr", THREM[0:16, :], Bsm)
        P.op('dve', lambda e: e.tensor_scalar(out=MASK, in0=AFFEM, scalar1=THREM[0:16, :], scalar2=None, op0=ALU.is_ge), reads=[Baffem, Bsm], writes=[Bmask])
        for g in range(NG):
            gs = slice(g * 1024, (g + 1) * 1024)
            P.op('dve', lambda e, gs=gs: e.tensor_tensor_scan(out=CUM[:, gs], data0=ONES16, data1=MASK[:, gs], initial=0.0, op0=ALU.mult, op1=ALU.add),
                 reads=[Bmask, Bones16], writes=[Bcum])
        P.op('dve', lambda e: e.tensor_tensor(out=CUM, in0=CUM, in1=MASK, op=ALU.mult), reads=[Bmask, Bcum], writes=[Bcum])
        P.op('dve', lambda e: e.tensor_scalar(out=CUM, in0=CUM, scalar1=-1.0, scalar2=None, op0=ALU.add), reads=[Bcum], writes=[Bcum])
        P.op('act', lambda e: e.activation(out=POSM, in_=CUM, func=AF.Copy), reads=[Bcum], writes=[Bposm])
        for c in range(NCH):
            P.op('pe', lambda e, c=c: e.transpose(bank(2)[:, c * 16:(c + 1) * 16], CUM[:, c * 128:(c + 1) * 128], c_identf[0:16, 0:16]), reads=[Bcum, Bcst], writes=[BPS[2]])
        P.op('dve', lambda e: e.tensor_copy(out=POST.rearrange("p c e -> p (c e)"), in_=bank(2)[:, 0:256]), reads=[BPS[2]], writes=[Bpost])
        P.op('dve', lambda e: e.scalar_tensor_tensor(out=GA.rearrange("p c e -> p (c e)"), in0=POST.rearrange("p c e -> p (c e)"), scalar=0.0, in1=AFF.rearrange("p c e -> p (c e)"), op0=ALU.is_ge, op1=ALU.mult),
             reads=[Bpost, Baff], writes=[Bga])
        tap("post", POST.rearrange("p c e -> p (c e)"), Bpost)
        for hf in range(2):
            hsl = slice(hf * 512, (hf + 1) * 512)
            for dc in range(8):
                P.op('pe', lambda e, dc=dc, hf=hf: e.matmul(bank(0)[0:2, :], lhsT=ST2[:, dc, :], rhs=WT[hf][:, dc, :], start=(dc == 0), stop=(dc == 7)), reads=[Bfin, Bwt[hf]], writes=[BPS[0]])
            P.op('dve', lambda e, hsl=hsl: e.tensor_tensor(out=MROW2[:, hsl], in0=bank(0)[0:2, :], in1=BADA2[:, hsl], op=ALU.add), reads=[BPS[0], Bbada2], writes=[Bmrow2])
            P.op('pe', lambda e, hsl=hsl: e.matmul(bank(1), lhsT=c_sel0, rhs=MROW2[:, hsl], start=True, stop=True), reads=[Bmrow2, Bcst], writes=[BPS[1]])
            P.op('dve', lambda e, hsl=hsl: e.tensor_tensor(out=G2[:, hsl], in0=bank(1), in1=GAIN2[:, hsl], op=ALU.mult), reads=[BPS[1], Bgain2], writes=[Bg2])
        P.barrier()

        ckpt(8)
        ring_i = [0]

        def ring_load(src_ap):
            i = ring_i[0] % 5
            ring_i[0] += 1
            P.dma('pool', lambda e: e.dma_start(out=RING[i], in_=src_ap.rearrange("(c p) f -> p c f", p=128)), writes=[Bring[i]])
            return i

        def expert_pieces(ex):
            lst = []
            for j in range(4):
                lst.append(wg_d[ex, :, j * 512:(j + 1) * 512])
                lst.append(wu_d[ex, :, j * 512:(j + 1) * 512])
            for hf in range(2):
                for a in range(2):
                    lst.append(wd_d[ex, a * 1024:(a + 1) * 1024, hf * 512:(hf + 1) * 512])
            return lst

        all_pieces = []
        for ex in range(ned):
            all_pieces += expert_pieces(ex)
        piece_slot = {}
        next_load = [0]

        def ensure_loaded(upto):
            while next_load[0] <= upto and next_load[0] < len(all_pieces):
                piece_slot[next_load[0]] = ring_load(all_pieces[next_load[0]])
                next_load[0] += 1

        SCOL = [c_scv[:, 0:1], c_scv[:, 1:2], c_scv[:, 2:3], c_scv[:, 3:4]]
        for ex in range(ned):
            base = ex * 12
            ensure_loaded(base + 3)
            for c in range(NCH):
                P.op('dve', lambda e, c=c, ex=ex: e.tensor_scalar(out=PT_[:, c, :], in0=c_iota, scalar1=POST[:, c, ex:ex + 1], scalar2=None, op0=ALU.is_equal),
                     reads=[Bpost, Bcst], writes=[Bpt])
            for q in range(4):
                P.op('pe', lambda e, q=q, ex=ex: e.matmul(bank(4 + (q % 2)), lhsT=SELB[:, ex, :], rhs=POSM[:, q * 512:(q + 1) * 512], start=True, stop=True),
                     reads=[Bselb, Bposm], writes=[BPS[4 + (q % 2)]])
                g = q // 2
                for k in range(2):
                    idx = g * 2 + k
                    P.op('dve', lambda e, q=q, idx=idx: e.tensor_scalar(out=PTT[:, idx, (q % 2) * 512:(q % 2) * 512 + 512], in0=bank(4 + (q % 2)), scalar1=SCOL[idx], scalar2=None, op0=ALU.is_equal),
                         reads=[BPS[4 + (q % 2)], Bcst], writes=[Bptt])
            for g in range(NG):
                for dp in range(4):
                    for k in range(2):
                        dc = dp * 2 + k
                        for cc in range(8):
                            c = g * 8 + cc
                            P.op('pe', lambda e, dp=dp, k=k, dc=dc, c=c, cc=cc: e.matmul(bank(dp)[:, k * CAPG:(k + 1) * CAPG], lhsT=HX2[:, c, dc * 128:(dc + 1) * 128], rhs=PT_[:, c, :], start=(cc == 0), stop=(cc == 7)),
                                 reads=[Bhx, Bpt], writes=[BPS[dp]])
                    P.op('act', lambda e, dp=dp, g=g: e.activation(out=XST[:, dp * 2:dp * 2 + 2, g * CAPG:(g + 1) * CAPG], in_=bank(dp)[:, 0:2 * CAPG].rearrange("p (k s) -> p k s", k=2), func=AF.Copy),
                         reads=[BPS[dp]], writes=[Bxst])
            for j in range(4):
                ensure_loaded(base + 2 * j + 4)
                sg_ = piece_slot[base + 2 * j]
                su_ = piece_slot[base + 2 * j + 1]
                for f in range(4):
                    fc = j * 4 + f
                    fs = slice(f * 128, (f + 1) * 128)
                    ba = 4 + (fc % 2) * 2
                    for dc in range(8):
                        P.op('pe', lambda e, dc=dc, fs=fs, ba=ba, sg_=sg_: e.matmul(bank(ba)[:, 0:NS], lhsT=RING[sg_][:, dc, fs], rhs=XST[:, dc, :], start=(dc == 0), stop=(dc == 7)),
                             reads=[Bring[sg_], Bxst], writes=[BPS[ba]])
                    for dc in range(8):
                        P.op('pe', lambda e, dc=dc, fs=fs, ba=ba, su_=su_: e.matmul(bank(ba + 1)[:, 0:NS], lhsT=RING[su_][:, dc, fs], rhs=XST[:, dc, :], start=(dc == 0), stop=(dc == 7)),
                             reads=[Bring[su_], Bxst], writes=[BPS[ba + 1]])
                    P.op('act', lambda e, ba=ba: e.activation(out=SA, in_=bank(ba)[:, 0:NS], func=AF.Silu), reads=[BPS[ba]], writes=[Bsa])
                    P.op('dve', lambda e, ba=ba, fc=fc: e.tensor_tensor(out=HT[:, fc, :], in0=SA, in1=bank(ba + 1)[:, 0:NS], op=ALU.mult), reads=[Bsa, BPS[ba + 1]], writes=[Bht])
            for hf in range(2):
                ensure_loaded(base + 8 + 2 * hf + 4)
                sd = [piece_slot[base + 8 + 2 * hf], piece_slot[base + 8 + 2 * hf + 1]]
                for sc in range(3):
                    bi = (hf * 3 + sc) % 4
                    for fc in range(16):
                        P.op('pe', lambda e, fc=fc, sc=sc, bi=bi, sd=sd: e.matmul(bank(bi), lhsT=HT[:, fc, sc * 128:(sc + 1) * 128], rhs=RING[sd[fc // 8]][:, fc % 8, :], start=(fc == 0), stop=(fc == 15)),
                             reads=[Bht, Bring[sd[fc // 8]]], writes=[BPS[bi]])
                    P.op('act', lambda e, sc=sc, hf=hf, bi=bi: e.activation(out=YG[:, sc, hf * 512:(hf + 1) * 512], in_=bank(bi), func=AF.Copy), reads=[BPS[bi]], writes=[Byg])
            for c in range(NCH):
                g = c // 8
                tl = (c % 8) * 128
                bo = 2 * (c % 2)
                for hf in range(2):
                    for k in range(2):
                        idx = g * 2 + k
                        sc = g + k
                        P.op('pe', lambda e, hf=hf, k=k, idx=idx, sc=sc, tl=tl, bo=bo: e.matmul(bank(4 + bo + hf), lhsT=PTT[:, idx, tl:tl + 128], rhs=YG[:, sc, hf * 512:(hf + 1) * 512], start=(k == 0), stop=(k == 1)),
                             reads=[Bptt, Byg], writes=[BPS[4 + bo + hf]])
                O = psb[2 + (c % 2)][:, :]
                P.op('dve', lambda e, c=c, ex=ex, O=O: e.scalar_tensor_tensor(out=ACC[:, c, :], in0=O, scalar=GA[:, c, ex:ex + 1], in1=ACC[:, c, :], op0=ALU.mult, op1=ALU.add),
                     reads=[BPS[4 + bo], BPS[5 + bo], Bga, Bacc[c]], writes=[Bacc[c]])
        tap("acc0", ACC[:, 0, :], Bacc[0])

        ckpt(9)
        P.barrier()
        NXM = 4
        XM = [A.at(120 + 4 * i, [D], F32) for i in range(NXM)]
        SQ = A.at(136, [D], F32)
        SQJ = A.at(140, [D], F32)
        Bxmm = [Buf(f"xmm{i}") for i in range(NXM)]
        Bsq, Bsqj = Buf("sq"), Buf("sqj")
        Bstf = [Buf("stf0"), Buf("stf1")]
        Bout = Buf("out")
        for c in range(NCH):
            i = c % NXM
            st = STAT[:, 24 + 2 * (c % 2):26 + 2 * (c % 2)]
            bst = Bstf[c % 2]
            P.dma('sp', lambda e, c=c, i=i: e.dma_start(out=XM[i], in_=xm_scr.ap()[c * 128:(c + 1) * 128, :]), reads=[Bxm], writes=[Bxmm[i]])
            P.op('act', lambda e, c=c, st=st: e.activation(out=SQJ, in_=ACC[:, c, :], func=AF.Square, accum_out=st[:, 0:1]), reads=[Bacc[c]], writes=[Bsqj, bst])
            P.op('act', lambda e, st=st: e.activation(out=st[:, 1:2], in_=st[:, 0:1], func=AF.Sqrt, scale=1.0 / D, bias=c_eps), reads=[bst, Bcst], writes=[bst])
            P.op('dve', lambda e, st=st: e.reciprocal(out=st[:, 1:2], in_=st[:, 1:2]), reads=[bst], writes=[bst])
            P.op('dve', lambda e, c=c, st=st: e.scalar_tensor_tensor(out=SQ, in0=ACC[:, c, :], scalar=st[:, 1:2], in1=G2, op0=ALU.mult, op1=ALU.mult), reads=[Bacc[c], bst, Bg2], writes=[Bsq])
            P.op('dve', lambda e, i=i: e.tensor_tensor(out=XM[i], in0=XM[i], in1=SQ, op=ALU.add), reads=[Bsq, Bxmm[i]], writes=[Bxmm[i]])
            out_toks.append(P.dma('sp', lambda e, c=c, i=i: e.dma_start(out=out_d[c * 128:(c + 1) * 128, :], in_=XM[i]), reads=[Bxmm[i]], writes=[Bout], sembuf=Bxmm[i]))
        P.wait_all('sp', [t for t in out_toks if t is not None])
        with nc.Block() as block:
            P.emit(block)
    return nc


def _host_consts(r):
    cst = np.zeros((128, 1600), np.float32)
    p = np.arange(128, dtype=np.float32)
    i = np.arange(128, dtype=np.float32)
    cst[:, 0:192] = np.arange(192, dtype=np.float32)[None, :]
    rel = i[None, :] - p[:, None]
    cst[:, 192:320] = np.maximum(rel, 0)
    cst[:, 320:448] = (rel >= 0)
    cst[:, 448:576] = np.maximum(-rel, 0)
    cst[:, 576:704] = (rel < 0)
    cst[:, 704:832] = (i + 1)[None, :]
    cst[:, 832:960] = (128 - i)[None, :]
    cst[:, 960:1088] = np.eye(128, dtype=np.float32)
    cst[:, 1088] = 127 - p
    cst[:, 1089] = p
    for c in range(2):
        cst[:, 1090 + c] = 255 - (c * 128 + p)
        cst[:, 1092 + c] = c * 128 + p
    for idx, (g, sc) in enumerate(((0, 0), (0, 1), (1, 1), (1, 2))):
        v = 128 * sc + p - g * CAPG
        cst[:, 1094 + idx] = np.where((v >= 0) & (v < CAPG), v, -5.0)
    pe = (np.arange(128) // 2) % 16
    cst[:, 1100:1228] = (pe[:, None] == pe[None, :])
    sel = np.zeros((128, 16), np.float32)
    for e in range(16):
        sel[2 * e, e] = 1
    cst[:, 1228:1244] = sel
    cst[:, 1244:1260] = (128 * (15 - np.arange(16)))[None, :]
    cst[:, 1260:1276] = (128 * np.arange(16))[None, :]
    xe = np.zeros(10, np.float32)
    xm = np.zeros(10, np.float32)
    for s in range(4):
        if s < r:
            xe[s] = 2048 * (r - 1 - s); xm[s] = 1
        if s > r:
            xe[4 + s] = 2048 * (s - r - 1); xm[4 + s] = 1
    xe[8] = 2048 * r; xm[8] = 1
    xe[9] = 2048 * (3 - r); xm[9] = 1
    cst[:, 1276:1286] = xe[None, :]
    cst[:, 1432:1442] = xm[None, :]
    cst[:, 1300:1428] = 1.0
    cst[0, 1428] = 1.0
    cst[1, 1429] = 1.0
    cst[:, 1430] = 1e-6
    cst[0, 1442:1570] = 1.0
    half = 64
    inv = (1.0 / (10000.0 ** (np.arange(half, dtype=np.float32) / half))).astype(np.float32)
    def tabs(pos, scale):
        ang = pos[:, None].astype(np.float32) * inv[None, :]
        return (np.cos(ang) * scale).astype(np.float32), (np.sin(ang) * scale).astype(np.float32)
    pos = (256 + r * 2048 + np.arange(2048)).astype(np.float32)
    cq, sq = tabs(pos, 1.0)
    ck, sk = tabs(pos, 128.0 ** -0.5)
    cc, sc_ = tabs(np.arange(256).astype(np.float32), 128.0 ** -0.5)
    def lay(t, n):
        return t.reshape(n, 128, 64).transpose(1, 0, 2)
    rotq = np.stack([lay(cq, 16), lay(sq, 16)], axis=1)
    rotk = np.stack([lay(ck, 16), lay(sk, 16)], axis=1)
    rotc = np.stack([lay(cc, 2), lay(sc_, 2)], axis=1)
    return cst, np.ascontiguousarray(rotq), np.ascontiguousarray(rotk), np.ascontiguousarray(rotc)


def make_in_maps(x, c, ctx, c_ctx, w_ada, b_ada, pre_mix_g, post_mix_g, pre_ffn_g, post_ffn_g,
                 w_in, conv_w, conv_b, ret_decay_logit, ret_norm_g, w_out,
                 w_router, b_router, w_gate, w_up, w_down):
    f = lambda a: np.ascontiguousarray(np.asarray(a, dtype=np.float32))
    x, c, ctx, c_ctx = f(x), f(c), f(ctx), f(c_ctx)
    rep = lambda v: np.ascontiguousarray(np.broadcast_to(f(v).reshape(1, -1), (128, f(v).size)))
    shared = {
        "w_ada": f(w_ada[0]), "b_ada2": np.ascontiguousarray(np.broadcast_to(f(b_ada[0])[None, :], (2, 6 * D))),
        "pmg": np.ascontiguousarray(f(pre_mix_g[0]).reshape(8, 128).T),
        "postmix_row": rep(post_mix_g[0]), "preffn_row": rep(pre_ffn_g[0]), "postffn_row": rep(post_ffn_g[0]),
        "w_in": f(w_in[0]),
        "convw": np.ascontiguousarray(f(conv_w[0]).reshape(3, 4, 128).transpose(2, 1, 0)),
        "convb": np.ascontiguousarray(f(conv_b[0]).reshape(4, 128).T),
        "decay": rep(f(ret_decay_logit[0]).reshape(-1)),
        "retg_row": rep(ret_norm_g[0]),
        "w_out": f(w_out[0]),
        "w_router": np.ascontiguousarray(f(w_router[0]).reshape(8, 128, 16).transpose(1, 0, 2)),
        "brouter_row": rep(b_router[0]),
        "w_gate": f(w_gate[0]), "w_up": f(w_up[0]), "w_down": f(w_down[0]),
        "identb": np.eye(128, dtype=np.float32).astype(ml_dtypes.bfloat16),
    }
    selb = np.zeros((16, NE, 128), np.float32)
    for e in range(NE):
        selb[e, e, :] = 1
    shared["selb"] = selb.astype(ml_dtypes.bfloat16)
    maps = []
    for j in range(8):
        b, r = j // 4, j % 4
        cst, rotq, rotk, rotc = _host_consts(r)
        cv = np.stack([c[b].reshape(8, 128).T, c_ctx.reshape(8, 128).T], axis=-1)
        m = dict(shared)
        m.update({"x": np.ascontiguousarray(x[b, r * TPC:(r + 1) * TPC]), "ctx": np.ascontiguousarray(ctx[b]),
                  "cvec": np.ascontiguousarray(cv.astype(np.float32)), "cst": cst, "rotq": rotq, "rotk": rotk, "rotc": rotc})
        maps.append(m)
    return maps


_NC_CACHE = {}


def kernel(**inputs):
    if "nc" not in _NC_CACHE:
        _NC_CACHE["nc"] = build()
    nc = _NC_CACHE["nc"]
    maps = make_in_maps(**inputs)
    res = run_bass_kernel_spmd(nc, maps, core_ids=list(range(8)))
    out = np.zeros((2, 8192, D), np.float32)
    for j in range(8):
        b, r = j // 4, j % 4
        out[b, r * TPC:(r + 1) * TPC] = res.results[j]["out"]
    return out
```

```python
import numpy as np
import ml_dtypes
import concourse.bass as bass
import concourse.mybir as mybir
from concourse.bass_utils import run_bass_kernel_spmd
from contextlib import ExitStack

F32 = mybir.dt.float32
BF16 = mybir.dt.bfloat16
ALU = mybir.AluOpType
AF = mybir.ActivationFunctionType
AX = mybir.AxisListType

ENG = ('pe', 'act', 'dve', 'pool', 'sp')
D = 1024
TPC = 2048
NCH = 16
NE = 16
CAPG = 192
NG = 2
NS = NG * CAPG
KCAP = 1024.0
NBIS = 26
DBG = {}


class Buf:
    __slots__ = ('name', 'w', 'r', 'dsem', 'dcnt', 'const', 'psum')

    def __init__(self, name, psum=False):
        self.name = name
        self.psum = psum
        self.w = None
        self.r = {}
        self.dsem = None
        self.dcnt = 0
        self.const = False


class Prog:
    def __init__(self, nc, es):
        self.nc = nc
        self.es = es
        self.sem = {e: es.enter_context(nc.semaphore('s_' + e)) for e in ENG}
        self.cnt = {e: 0 for e in ENG}
        self.stream = {e: [] for e in ENG}
        self.known = {e: {} for e in ENG}
        self.dbufs = []
        self.frozen = False

    def _waits(self, eng, reads, writes):
        need = {}

        def add(tok):
            if tok is None:
                return
            sem, val, teng = tok
            if teng == 'pe' and eng == 'pe':
                return
            key = id(sem)
            if key not in need or need[key][1] < val:
                need[key] = (sem, val)

        for b in reads:
            add(b.w)
            if b.psum:
                for t in b.r.values():
                    if t[2] != eng:
                        add(t)
        for b in writes:
            add(b.w)
            for t in b.r.values():
                add(t)
        out = []
        kn = self.known[eng]
        for key, (sem, val) in need.items():
            if kn.get(key, 0) >= val:
                continue
            kn[key] = val
            out.append((sem, val))
        return out

    def _record(self, tok, reads, writes):
        key = id(tok[0])
        for b in reads:
            if not b.const:
                b.r[key] = tok
        for b in writes:
            b.w = tok
            b.r = {}

    def op(self, eng, fn, reads=(), writes=()):
        if self.frozen:
            return None
        waits = self._waits(eng, reads, writes)
        self.cnt[eng] += 1
        tok = (self.sem[eng], self.cnt[eng], eng)
        self._record(tok, reads, writes)
        self.stream[eng].append((waits, fn, self.sem[eng], 1))
        return tok

    def dma(self, q, fn, reads=(), writes=(), sembuf=None, inc=16):
        if self.frozen:
            return None
        waits = self._waits(q, reads, writes)
        sb = sembuf or (writes[0] if writes else reads[0])
        if sb.dsem is None:
            sb.dsem = self.es.enter_context(self.nc.semaphore('d_' + sb.name))
            self.dbufs.append(sb)
        sb.dcnt += inc
        tok = (sb.dsem, sb.dcnt, 'dma')
        self._record(tok, reads, writes)
        self.stream[q].append((waits, fn, sb.dsem, inc))
        return tok

    def wait_all(self, eng, toks):
        waits = []
        kn = self.known[eng]
        for (sem, val, _) in toks:
            if kn.get(id(sem), 0) >= val:
                continue
            kn[id(sem)] = val
            waits.append((sem, val))
        self.stream[eng].append((waits, None, None, 0))

    def barrier(self):
        if self.frozen:
            return
        toks = [(self.sem[e], self.cnt[e], e) for e in ENG if self.cnt[e] > 0]
        toks += [(b.dsem, b.dcnt, 'dma') for b in self.dbufs]
        for e in ENG:
            self.wait_all(e, toks)

    def emit(self, block):
        streams = self.stream

        def run(e, name):
            for waits, fn, sem, inc in streams[name]:
                for s, v in waits:
                    e.wait_ge(s, v)
                if fn is not None:
                    ins = fn(e)
                    ins.then_inc(sem, inc)

        @block.tensor
        def _(e):
            run(e, 'pe')

        @block.scalar
        def _(e):
            run(e, 'act')

        @block.vector
        def _(e):
            run(e, 'dve')

        @block.gpsimd
        def _(e):
            run(e, 'pool')

        @block.sync
        def _(e):
            run(e, 'sp')


class Arena:
    def __init__(self, big):
        self.big = big

    def at(self, off_kib, free_shape, dt, parts=128):
        n = int(np.prod(free_shape))
        nb = n * (2 if dt == BF16 else 4)
        n32 = (nb + 3) // 4
        off = int(off_kib * 256)
        ap = self.big[0:parts, off:off + n32]
        if dt != F32:
            ap = ap.bitcast(dt)
        if len(free_shape) == 2:
            ap = ap.rearrange("p (a b) -> p a b", a=free_shape[0], b=free_shape[1])
        elif len(free_shape) == 3:
            ap = ap.rearrange("p (a b c) -> p a b c", a=free_shape[0], b=free_shape[1], c=free_shape[2])
        return ap


class _Stop(Exception):
    pass


def build(dbg=(), stop=99):
    nc = bass.Bass("TRN2", target_bir_lowering=False)

    def din(name, shape, dt=F32):
        return nc.dram_tensor(name, list(shape), dt, kind="ExternalInput").ap()

    x_d = din("x", [TPC, D])
    ctx_d = din("ctx", [256, D])
    cvec_d = din("cvec", [128, 8, 2])
    wada_d = din("w_ada", [D, 6 * D])
    bada_d = din("b_ada2", [2, 6 * D])
    pmg_d = din("pmg", [128, 8])
    postmix_d = din("postmix_row", [128, D])
    preffn_d = din("preffn_row", [128, D])
    postffn_d = din("postffn_row", [128, D])
    win_d = din("w_in", [D, 3584])
    convw_d = din("convw", [128, 4, 3])
    convb_d = din("convb", [128, 4])
    dec_d = din("decay", [128, 8])
    retg_d = din("retg_row", [128, 512])
    wout_d = din("w_out", [D, D])
    wr_d = din("w_router", [128, 8, 16])
    br_d = din("brouter_row", [128, 16])
    ned = NE if stop > 8 else 1
    wg_d = din("w_gate", [ned, D, 2 * D])
    wu_d = din("w_up", [ned, D, 2 * D])
    wd_d = din("w_down", [ned, 2 * D, D])
    rotq_d = din("rotq", [128, 2, NCH, 64])
    rotk_d = din("rotk", [128, 2, NCH, 64])
    rotc_d = din("rotc", [128, 2, 2, 64])
    cst_d = din("cst", [128, 1600])
    identb_d = din("identb", [128, 128], BF16)
    selb_d = din("selb", [16, NE, 128], BF16)
    out_d = nc.dram_tensor("out", [TPC, D], F32, kind="ExternalOutput").ap()
    taps = {}
    for name, shape in dbg:
        taps[name] = nc.dram_tensor("dbg_" + name, list(shape), F32, kind="ExternalOutput").ap()

    xm_scr = nc.dram_tensor("xm_scr", [TPC, D], F32)
    hx2_scr = nc.dram_tensor("hx2_scr", [TPC, D], BF16)
    ag1_in = nc.dram_tensor("ag1_in", [128, 1024], F32)
    ag1_out = nc.dram_tensor("ag1_out", [512, 1024], F32)
    ag2_in = nc.dram_tensor("ag2_in", [16, TPC], F32)
    ag2_out = nc.dram_tensor("ag2_out", [64, TPC], F32)

    with ExitStack() as es:
        P = Prog(nc, es)
        big = es.enter_context(nc.sbuf_tensor("big", [128, 204 * 256], F32))
        A = Arena(big)
        psb = [es.enter_context(nc.psum_tensor(f"psb{i}", [128, 1024], F32)) for i in range(4)]
        BPS = [Buf(f"ps{i}", psum=True) for i in range(8)]

        def bank(i):
            return psb[i // 2][:, (i % 2) * 512:(i % 2) * 512 + 512]

        def bank_bf(i):
            return bank(i).bitcast(BF16)

        out_toks = []

        def ckpt(n):
            if stop == n:
                P.frozen = True


        def tap(name, src_ap, buf, q='sp', orr=None, **kw):
            if name in taps:
                dst = taps[name] if orr is None else taps[name].rearrange(orr, **kw)
                out_toks.append(P.dma(q, lambda e: e.dma_start(out=dst, in_=src_ap), reads=[buf], writes=[Buf("tap_" + name)], sembuf=Buf("tsem_" + name)))

        CST = A.at(0, [1600], F32)
        Bcst = Buf("cst"); Bcst.const = True
        c_iota = CST[:, 0:192]
        c_relF = CST[:, 192:320]
        c_mskF = CST[:, 320:448]
        c_relB = CST[:, 448:576]
        c_mskB = CST[:, 576:704]
        c_i1 = CST[:, 704:832]
        c_i128 = CST[:, 832:960]
        c_identf = CST[:, 960:1088]
        c_p127 = CST[:, 1088:1089]
        c_p = CST[:, 1089:1090]
        c_ctxf = CST[:, 1090:1092]
        c_ctxb = CST[:, 1092:1094]
        c_scv = CST[:, 1094:1098]
        c_grp = CST[:, 1100:1228]
        c_sel = CST[:, 1228:1244]
        c_c128f = CST[:, 1244:1260]
        c_c128b = CST[:, 1260:1276]
        c_xexp = CST[:, 1276:1292]
        c_ones = CST[:, 1300:1428]
        c_e01 = CST[:, 1428:1430]
        c_eps = CST[:, 1430:1431]
        c_sel0 = CST[0:2, 1442:1570]
        IDB = A.at(6.25, [128], BF16)
        Bidb = Buf("idb"); Bidb.const = True
        SELB = A.at(6.5, [NE, 128], BF16, parts=16)
        Bselb = Buf("selb"); Bselb.const = True
        SMALL = A.at(10.5, [384], F32)
        Bsm = Buf("small")
        LG = SMALL[:, 0:8]
        A1 = SMALL[:, 8:16]; SH1 = SMALL[:, 16:24]; A1C = SMALL[:, 24:32]; SH1C = SMALL[:, 32:40]
        ZF = SMALL[:, 40:44]; ZB = SMALL[:, 44:48]
        CZF = SMALL[:, 48:56]; CZB = SMALL[:, 56:64]
        CF = SMALL[:, 64:128]; CB = SMALL[:, 128:192]
        G128 = SMALL[:, 192:200]
        XCO = SMALL[:, 200:240]
        PMG = SMALL[:, 240:248]
        CW = SMALL[:, 248:260]
        CBI = SMALL[:, 260:264]
        BRR = SMALL[:, 264:280]
        THR = SMALL[:, 280:281]; MID = SMALL[:, 281:282]; CNT = SMALL[:, 282:283]; TT = SMALL[:, 283:284]
        THREM = SMALL[:, 284:285]
        STAT = SMALL[:, 288:320]
        DECL = SMALL[:, 320:328]
        AFF = A.at(12, [NCH, NE], F32)
        POST = A.at(13, [NCH, NE], F32)
        GA = A.at(14, [NCH, NE], F32)
        Baff, Bpost, Bga = Buf("aff"), Buf("post"), Buf("ga")
        POSM = A.at(15, [TPC], BF16, parts=16)
        Bposm = Buf("posm")
        LF = A.at(19, [4, 128], F32)
        LB = A.at(21, [4, 128], F32)
        Blf, Blb = Buf("lf"), Buf("lb")
        KT = A.at(24, [4, TPC], BF16)
        V = A.at(40, [NCH, 512], BF16)
        UF = A.at(56, [NCH, 512], BF16)
        UB = A.at(72, [NCH, 512], BF16)
        CONVT = A.at(88, [4, TPC], BF16)
        Bkt = [Buf(f"kt{c}") for c in range(NCH)]
        Bv = [Buf(f"v{c}") for c in range(NCH)]
        Buf_ = [Buf(f"uf{c}") for c in range(NCH)]
        Bub = [Buf(f"ub{c}") for c in range(NCH)]
        Bconvt = [Buf(f"convt{g}") for g in range(4)]
        WA = A.at(104, [8, 512], BF16)
        WB_ = A.at(112, [8, 512], BF16)
        WC = A.at(120, [8, 512], BF16)
        WD = A.at(128, [8, 512], BF16)
        WE = A.at(136, [8, 512], BF16)
        Bw = [Buf(f"w{i}") for i in range(5)]
        WSL = [WA, WB_, WC, WD, WE]
        ROT = A.at(144, [2, NCH, 64], F32)
        Brot = Buf("rot")
        ROTC = A.at(152, [2, 2, 64], F32)
        Brotc = Buf("rotc")
        DT = A.at(153, [4, 128], F32)
        XF = A.at(155, [4, 128], F32)
        XB = A.at(157, [4, 128], F32)
        Btab = Buf("tab")
        ROWS = A.at(159, [3, D], F32)
        Brows = Buf("rows")
        HXTG = A.at(159, [8, 512], BF16)
        Bhxtg = Buf("hxtg")
        NGR = A.at(171, [512], F32)
        Bngr = Buf("ngr")
        T0 = 173
        XC = [A.at(T0, [D], F32), A.at(T0 + 4, [D], F32)]
        Bxc = [Buf("xc0"), Buf("xc1")]
        XS = A.at(T0 + 8, [D], BF16)
        Bxs = Buf("xs")
        HXTC = A.at(T0 + 10, [8, 128], BF16)
        Bhxtc = Buf("hxtc")
        TMPA = A.at(T0 + 12, [4, 64], F32)
        TMPB = A.at(T0 + 13, [4, 64], F32)
        Btmp = Buf("tmpab")
        KR = A.at(T0 + 14, [512], BF16)
        Bkr = Buf("kr")
        KFs = A.at(T0 + 15, [512], BF16)
        KBs = A.at(T0 + 16, [512], BF16)
        Bkf, Bkb = Buf("kf"), Buf("kb")
        S1 = A.at(T0 + 17, [512], F32)
        S2 = A.at(T0 + 19, [512], F32)
        S3 = A.at(T0 + 21, [512], F32)
        Bs1, Bs2, Bs3 = Buf("s1"), Buf("s2"), Buf("s3")
        S4 = A.at(T0 + 23, [D], F32)
        Bs4 = Buf("s4")
        S5 = A.at(T0 + 27, [D], F32)
        Bs5 = Buf("s5")

        P.dma('sp', lambda e: e.dma_start(out=CST, in_=cst_d), writes=[Bcst])
        P.dma('sp', lambda e: e.dma_start(out=IDB, in_=identb_d), writes=[Bidb])
        P.dma('sp', lambda e: e.dma_start(out=SELB, in_=selb_d), writes=[Bselb])
        P.dma('sp', lambda e: e.dma_start(out=DECL, in_=dec_d), writes=[Bsm])
        P.dma('sp', lambda e: e.dma_start(out=PMG, in_=pmg_d), writes=[Bsm])
        P.dma('sp', lambda e: e.dma_start(out=CW, in_=convw_d.rearrange("p a b -> p (a b)")), writes=[Bsm])
        P.dma('sp', lambda e: e.dma_start(out=CBI, in_=convb_d), writes=[Bsm])
        P.dma('sp', lambda e: e.dma_start(out=BRR, in_=br_d), writes=[Bsm])
        P.dma('sp', lambda e: e.dma_start(out=ROT, in_=rotk_d), writes=[Brot])
        P.dma('sp', lambda e: e.dma_start(out=ROTC, in_=rotc_d), writes=[Brotc])
        P.dma('sp', lambda e: e.dma_start(out=NGR, in_=retg_d), writes=[Bngr])

        P.op('act', lambda e: e.activation(out=LG, in_=DECL, func=AF.Exp, scale=-1.0), reads=[Bsm], writes=[Bsm])
        P.op('dve', lambda e: e.tensor_scalar(out=LG, in0=LG, scalar1=1.0, scalar2=None, op0=ALU.add), reads=[Bsm], writes=[Bsm])
        P.op('act', lambda e: e.activation(out=LG, in_=LG, func=AF.Ln), reads=[Bsm], writes=[Bsm])
        P.op('dve', lambda e: e.tensor_scalar(out=LG, in0=LG, scalar1=-1.0, scalar2=None, op0=ALU.mult), reads=[Bsm], writes=[Bsm])
        for h in range(4):
            lf = LG[:, h:h + 1]
            lb = LG[:, 4 + h:5 + h]
            P.op('act', lambda e, h=h, lf=lf: e.activation(out=DT[:, h, :], in_=c_relF, func=AF.Exp, scale=lf), reads=[Bsm, Bcst], writes=[Btab])
            P.op('act', lambda e, h=h, lb=lb: e.activation(out=XF[:, h, :], in_=c_relB, func=AF.Exp, scale=lb), reads=[Bsm, Bcst], writes=[Btab])
            P.op('dve', lambda e, h=h: e.tensor_tensor(out=DT[:, h, :], in0=DT[:, h, :], in1=c_mskF, op=ALU.mult), reads=[Btab, Bcst], writes=[Btab])
            P.op('dve', lambda e, h=h: e.tensor_tensor(out=XF[:, h, :], in0=XF[:, h, :], in1=c_mskB, op=ALU.mult), reads=[Btab, Bcst], writes=[Btab])
            P.op('dve', lambda e, h=h: e.tensor_tensor(out=DT[:, h, :], in0=DT[:, h, :], in1=XF[:, h, :], op=ALU.add), reads=[Btab], writes=[Btab])
        for h in range(4):
            lf = LG[:, h:h + 1]
            lb = LG[:, 4 + h:5 + h]
            P.op('act', lambda e, h=h, lf=lf: e.activation(out=XF[:, h, :], in_=c_i1, func=AF.Exp, scale=lf), reads=[Bsm, Bcst, Btab], writes=[Btab])
            P.op('act', lambda e, h=h, lb=lb: e.activation(out=XB[:, h, :], in_=c_i128, func=AF.Exp, scale=lb), reads=[Bsm, Bcst], writes=[Btab])
            P.op('act', lambda e, h=h, lf=lf: e.activation(out=ZF[:, h:h + 1], in_=c_p127, func=AF.Exp, scale=lf), reads=[Bsm, Bcst], writes=[Bsm])
            P.op('act', lambda e, h=h, lb=lb: e.activation(out=ZB[:, h:h + 1], in_=c_p, func=AF.Exp, scale=lb), reads=[Bsm, Bcst], writes=[Bsm])
            for c in range(2):
                P.op('act', lambda e, h=h, c=c, lf=lf: e.activation(out=CZF[:, c * 4 + h:c * 4 + h + 1], in_=c_ctxf[:, c:c + 1], func=AF.Exp, scale=lf), reads=[Bsm, Bcst], writes=[Bsm])
                P.op('act', lambda e, h=h, c=c, lb=lb: e.activation(out=CZB[:, c * 4 + h:c * 4 + h + 1], in_=c_ctxb[:, c:c + 1], func=AF.Exp, scale=lb), reads=[Bsm, Bcst], writes=[Bsm])
            cfv = CF.rearrange("p (c h) -> p c h", h=4)
            cbv = CB.rearrange("p (c h) -> p c h", h=4)
            P.op('act', lambda e, h=h, lf=lf, cfv=cfv: e.activation(out=cfv[:, :, h], in_=c_c128f, func=AF.Exp, scale=lf), reads=[Bsm, Bcst], writes=[Bsm])
            P.op('act', lambda e, h=h, lb=lb, cbv=cbv: e.activation(out=cbv[:, :, h], in_=c_c128b, func=AF.Exp, scale=lb), reads=[Bsm, Bcst], writes=[Bsm])
            P.op('act', lambda e, h=h, lf=lf: e.activation(out=G128[:, h:h + 1], in_=c_ones[:, 0:1], func=AF.Exp, scale=lf), reads=[Bsm, Bcst], writes=[Bsm])
            P.op('act', lambda e, h=h, lb=lb: e.activation(out=G128[:, 4 + h:5 + h], in_=c_ones[:, 0:1], func=AF.Exp, scale=lb), reads=[Bsm, Bcst], writes=[Bsm])
            xco = XCO.rearrange("p (s h) -> p s h", h=4)
            P.op('act', lambda e, h=h, lf=lf, xco=xco: e.activation(out=xco[:, 0:4, h], in_=c_xexp[:, 0:4], func=AF.Exp, scale=lf), reads=[Bsm, Bcst], writes=[Bsm])
            P.op('act', lambda e, h=h, lb=lb, xco=xco: e.activation(out=xco[:, 4:8, h], in_=c_xexp[:, 4:8], func=AF.Exp, scale=lb), reads=[Bsm, Bcst], writes=[Bsm])
            P.op('act', lambda e, h=h, lf=lf, xco=xco: e.activation(out=xco[:, 8:9, h], in_=c_xexp[:, 8:9], func=AF.Exp, scale=lf), reads=[Bsm, Bcst], writes=[Bsm])
            P.op('act', lambda e, h=h, lb=lb, xco=xco: e.activation(out=xco[:, 9:10, h], in_=c_xexp[:, 9:10], func=AF.Exp, scale=lb), reads=[Bsm, Bcst], writes=[Bsm])
        for _ in range(7):
            P.op('dve', lambda e: e.tensor_tensor(out=G128, in0=G128, in1=G128, op=ALU.mult), reads=[Bsm], writes=[Bsm])
        xco = XCO.rearrange("p (s h) -> p s h", h=4)
        c_xmsk = CST[:, 1432:1442]
        P.op('dve', lambda e: e.tensor_tensor(out=xco, in0=xco, in1=c_xmsk.unsqueeze(2).to_broadcast([128, 10, 4]), op=ALU.mult), reads=[Bsm, Bcst], writes=[Bsm])

        ckpt(1)
        wslot = [0]

        def load_piece(src_ap, slot):
            P.dma('pool', lambda e: e.dma_start(out=WSL[slot], in_=src_ap.rearrange("(c p) f -> p c f", p=128)), writes=[Bw[slot]])

        CV = A.at(23, [8, 2], F32)
        ST_ = A.at(23.25, [8, 2], BF16)
        Bcv = Buf('cv')
        MROW = A.at(T0 + 27, [512], F32, parts=2)
        P.dma('sp', lambda e: e.dma_start(out=CV, in_=cvec_d), writes=[Bcv])
        P.op('act', lambda e: e.activation(out=ST_, in_=CV, func=AF.Silu), reads=[Bcv], writes=[Bcv])
        BADA = A.at(T0 + 17, [512], F32, parts=2)

        def mod_block(n, slot):
            load_piece(wada_d[:, n * 512:(n + 1) * 512], slot)
            P.dma('sp', lambda e: e.dma_start(out=BADA, in_=bada_d[:, n * 512:(n + 1) * 512]), writes=[Bs1])
            for dc in range(8):
                P.op('pe', lambda e, dc=dc: e.matmul(bank(0)[0:2, :], lhsT=ST_[:, dc, :], rhs=WSL[slot][:, dc, :], start=(dc == 0), stop=(dc == 7)),
                     reads=[Bcv, Bw[slot]], writes=[BPS[0]])
            P.op('dve', lambda e: e.tensor_tensor(out=MROW, in0=bank(0)[0:2, :], in1=BADA, op=ALU.add), reads=[BPS[0], Bs1], writes=[Bs5])

        def row_bcast(dst_ap, dst_buf, gain_src, plus_one):
            P.op('pe', lambda e: e.matmul(bank(1), lhsT=c_sel0, rhs=MROW, start=True, stop=True),
                 reads=[Bs5, Bcst], writes=[BPS[1]])
            if gain_src is None:
                P.op('act', lambda e: e.activation(out=dst_ap, in_=bank(1), func=AF.Copy), reads=[BPS[1]], writes=[dst_buf])
            elif plus_one:
                P.op('dve', lambda e: e.scalar_tensor_tensor(out=dst_ap, in0=bank(1), scalar=1.0, in1=gain_src, op0=ALU.add, op1=ALU.mult),
                     reads=[BPS[1], Bs2], writes=[dst_buf])
            else:
                P.op('dve', lambda e: e.tensor_tensor(out=dst_ap, in0=bank(1), in1=gain_src, op=ALU.mult), reads=[BPS[1], Bs2], writes=[dst_buf])

        def col_extract(n, dst_lat, dst_ctx, half):
            for cc in range(4):
                for r, dst in ((0, dst_lat), (1, dst_ctx)):
                    P.op('pe', lambda e, cc=cc, r=r: e.matmul(bank(2)[:, (r * 4 + cc):(r * 4 + cc) + 1], lhsT=MROW[0:2, cc * 128:(cc + 1) * 128],
                                                               rhs=c_e01[0:2, r:r + 1], start=True, stop=True),
                         reads=[Bs5, Bcst], writes=[BPS[2]])
            P.op('dve', lambda e: e.tensor_copy(out=dst_lat[:, half * 4:half * 4 + 4], in_=bank(2)[:, 0:4]), reads=[BPS[2]], writes=[Bsm])
            P.op('dve', lambda e: e.tensor_copy(out=dst_ctx[:, half * 4:half * 4 + 4], in_=bank(2)[:, 4:8]), reads=[BPS[2]], writes=[Bsm])

        for n in range(4):
            mod_block(n, n % 5)
            if n < 2:
                col_extract(n, SH1, SH1C, n)
            else:
                col_extract(n, A1, A1C, n - 2)
        for dst in (A1, A1C):
            P.op('dve', lambda e, dst=dst: e.scalar_tensor_tensor(out=dst, in0=dst, scalar=1.0, in1=PMG, op0=ALU.add, op1=ALU.mult), reads=[Bsm], writes=[Bsm])

        ckpt(2)
        P.barrier()
        load_piece(win_d[:, 1536 + 1 * 512:1536 + 2 * 512], 0)
        load_piece(win_d[:, 1536 + 2 * 512:1536 + 3 * 512], 1)
        load_piece(win_d[:, 512:1024], 2)
        load_piece(win_d[:, 1024:1536], 3)
        load_piece(win_d[:, 0:512], 4)

        STN = SMALL[:, 316:318]
        Bstn = Buf("stn")

        def load_x(src_d, row0, xbuf_i):
            xc, bxc = XC[xbuf_i], Bxc[xbuf_i]
            P.dma('sp', lambda e: e.dma_start(out=xc, in_=src_d[row0:row0 + 128, :]), writes=[bxc])

        def norm_compute(a_col, sh_col, xbuf_i, dst_ap, dst_buf):
            xc, bxc = XC[xbuf_i], Bxc[xbuf_i]
            P.op('act', lambda e: e.activation(out=XS, in_=xc, func=AF.Square, accum_out=STN[:, 0:1]), reads=[bxc], writes=[Bxs, Bstn])
            P.op('act', lambda e: e.activation(out=STN[:, 1:2], in_=STN[:, 0:1], func=AF.Sqrt, scale=1.0 / D, bias=c_eps), reads=[Bstn, Bcst], writes=[Bstn])
            P.op('dve', lambda e: e.reciprocal(out=STN[:, 1:2], in_=STN[:, 1:2]), reads=[Bstn], writes=[Bstn])
            P.op('act', lambda e: e.activation(out=XS, in_=xc, func=AF.Copy, scale=STN[:, 1:2]), reads=[bxc, Bstn], writes=[Bxs])
            tb = bank_bf(0).rearrange("p (c t) -> p c t", c=8)
            for dc in range(8):
                P.op('pe', lambda e, dc=dc: e.transpose(tb[:, dc, :], XS[:, dc * 128:(dc + 1) * 128], IDB), reads=[Bxs, Bidb], writes=[BPS[0]])
            for dc in range(8):
                P.op('dve', lambda e, dc=dc: e.tensor_scalar(out=dst_ap[:, dc, :], in0=tb[:, dc, :], scalar1=a_col[:, dc:dc + 1], scalar2=sh_col[:, dc:dc + 1], op0=ALU.mult, op1=ALU.add),
                     reads=[BPS[0], Bsm], writes=[dst_buf])

        def norm_transpose(src_d, row0, a_col, sh_col, xbuf_i, dst_ap, dst_buf, keep_x=False):
            load_x(src_d, row0, xbuf_i)
            norm_compute(a_col, sh_col, xbuf_i, dst_ap, dst_buf)

        def rotary(ps_ap, cos_ap, sin_ap, dst_ap, dst_buf, ps_buf, tab_buf):
            pv = ps_ap.rearrange("p (h t d) -> p h t d", h=4, t=2)
            dv = dst_ap.rearrange("p (h t d) -> p h t d", h=4, t=2)
            cb = cos_ap.unsqueeze(1).to_broadcast([128, 4, 64])
            sb = sin_ap.unsqueeze(1).to_broadcast([128, 4, 64])
            P.op('dve', lambda e: e.tensor_tensor(out=TMPA, in0=pv[:, :, 0, :], in1=cb, op=ALU.mult), reads=[ps_buf, tab_buf], writes=[Btmp])
            P.op('dve', lambda e: e.tensor_tensor(out=TMPB, in0=pv[:, :, 1, :], in1=sb, op=ALU.mult), reads=[ps_buf, tab_buf], writes=[Btmp])
            P.op('dve', lambda e: e.tensor_tensor(out=dv[:, :, 0, :], in0=TMPA, in1=TMPB, op=ALU.subtract), reads=[Btmp], writes=[dst_buf])
            P.op('dve', lambda e: e.tensor_tensor(out=TMPA, in0=pv[:, :, 0, :], in1=sb, op=ALU.mult), reads=[ps_buf, tab_buf], writes=[Btmp])
            P.op('dve', lambda e: e.tensor_tensor(out=TMPB, in0=pv[:, :, 1, :], in1=cb, op=ALU.mult), reads=[ps_buf, tab_buf], writes=[Btmp])
            P.op('dve', lambda e: e.tensor_tensor(out=dv[:, :, 1, :], in0=TMPA, in1=TMPB, op=ALU.add), reads=[Btmp], writes=[dst_buf])

        def kv_chunk(hx_ap, hx_buf, cos_ap, sin_ap, tab_buf, zf_ap, zb_ap, v_dst, v_buf, ck=False, mid=None):
            for dc in range(8):
                P.op('pe', lambda e, dc=dc: e.matmul(bank(1), lhsT=hx_ap[:, dc, :], rhs=WSL[0][:, dc, :], start=(dc == 0), stop=(dc == 7)),
                     reads=[hx_buf, Bw[0]], writes=[BPS[1]])
            for dc in range(8):
                P.op('pe', lambda e, dc=dc: e.matmul(bank(2), lhsT=hx_ap[:, dc, :], rhs=WSL[1][:, dc, :], start=(dc == 0), stop=(dc == 7)),
                     reads=[hx_buf, Bw[1]], writes=[BPS[2]])
            if mid is not None:
                mid()
            P.op('act', lambda e: e.activation(out=v_dst, in_=bank(2), func=AF.Copy), reads=[BPS[2]], writes=[v_buf])
            if ck:
                ckpt(22)
            rotary(bank(1), cos_ap, sin_ap, KR, Bkr, BPS[1], tab_buf)
            if ck:
                ckpt(23)
            for h in range(4):
                hs = slice(h * 128, (h + 1) * 128)
                P.op('dve', lambda e, h=h, hs=hs: e.tensor_scalar(out=KFs[:, hs], in0=KR[:, hs], scalar1=zf_ap[:, h:h + 1], scalar2=None, op0=ALU.mult), reads=[Bkr, Bsm], writes=[Bkf])
                P.op('dve', lambda e, h=h, hs=hs: e.tensor_scalar(out=KBs[:, hs], in0=KR[:, hs], scalar1=zb_ap[:, h:h + 1], scalar2=None, op0=ALU.mult), reads=[Bkr, Bsm], writes=[Bkb])

        VC = S3.bitcast(BF16)[:, 0:512]
        SCF = A.at(T0 + 27, [4, 128], F32)
        SCB = A.at(T0 + 29, [4, 128], F32)
        xco = XCO.rearrange("p (s h) -> p s h", h=4)
        s1v_ = S1.rearrange("p (h d) -> p h d", h=4)
        for c in range(2):
            norm_transpose(ctx_d, c * 128, A1C, SH1C, c % 2, HXTC, Bhxtc)
            if c == 0:
                ckpt(21)
            kv_chunk(HXTC, Bhxtc, ROTC[:, 0, c, :], ROTC[:, 1, c, :], Brotc, CZF[:, c * 4:c * 4 + 4], CZB[:, c * 4:c * 4 + 4], VC, Bs3, ck=(c == 0))
            for h in range(4):
                hs = slice(h * 128, (h + 1) * 128)
                P.op('pe', lambda e, hs=hs: e.matmul(bank(4)[:, hs], lhsT=KFs[:, hs], rhs=VC[:, hs], start=True, stop=True),
                     reads=[Bkf, Bs3], writes=[BPS[4]])
            for h in range(4):
                hs = slice(h * 128, (h + 1) * 128)
                P.op('pe', lambda e, hs=hs: e.matmul(bank(5)[:, hs], lhsT=KBs[:, hs], rhs=VC[:, hs], start=True, stop=True),
                     reads=[Bkb, Bs3], writes=[BPS[5]])
            if c == 0:
                ckpt(25)
            for (dstv, bi, col) in ((SCF, 4, 8), (SCB, 5, 9)):
                for h in range(4):
                    hs = slice(h * 128, (h + 1) * 128)
                    if c == 0:
                        P.op('dve', lambda e, dstv=dstv, bi=bi, col=col, h=h, hs=hs: e.tensor_scalar(out=dstv[:, h, :], in0=bank(bi)[:, hs], scalar1=xco[:, col, h:h + 1], scalar2=None, op0=ALU.mult),
                             reads=[BPS[bi], Bsm], writes=[Bs5])
                    else:
                        P.op('dve', lambda e, dstv=dstv, bi=bi, col=col, h=h, hs=hs: e.scalar_tensor_tensor(out=dstv[:, h, :], in0=bank(bi)[:, hs], scalar=xco[:, col, h:h + 1], in1=dstv[:, h, :], op0=ALU.mult, op1=ALU.add),
                             reads=[BPS[bi], Bsm, Bs5], writes=[Bs5])
        tap("scf", SCF.rearrange("p h d -> p (h d)"), Bs5)

        ckpt(3)
        P.op('dve', lambda e: e.memset(LF, 0.0), writes=[Blf])
        P.op('dve', lambda e: e.memset(LB, 0.0), writes=[Blb])
        cfv = CF.rearrange("p (c h) -> p c h", h=4)
        cbv = CB.rearrange("p (c h) -> p c h", h=4)
        def pre_norm(c):
            norm_compute(A1, SH1, c % 2, HXTG[:, :, (c % 4) * 128:(c % 4) * 128 + 128], Bhxtg)

        load_x(x_d, 0, 0)
        pre_norm(0)
        for c in range(NCH):
            g = c // 4
            if c + 1 < NCH:
                load_x(x_d, (c + 1) * 128, (c + 1) % 2)
            hx = HXTG[:, :, (c % 4) * 128:(c % 4) * 128 + 128]
            kv_chunk(hx, Bhxtg, ROT[:, 0, c, :], ROT[:, 1, c, :], Brot, ZF, ZB, V[:, c, :], Bv[c],
                     mid=(lambda c=c: pre_norm(c + 1)) if (c + 1 < NCH and c % 4 != 3) else None)
            if c == 0:
                ckpt(32)
            tb = bank_bf(3).rearrange("p (h t) -> p h t", h=8)
            for h in range(4):
                P.op('pe', lambda e, h=h: e.transpose(tb[:, h, :], KR[:, h * 128:(h + 1) * 128], IDB), reads=[Bkr, Bidb], writes=[BPS[3]])
            P.op('act', lambda e, c=c: e.activation(out=KT[:, :, c * 128:(c + 1) * 128], in_=tb[:, 0:4, :], func=AF.Copy), reads=[BPS[3]], writes=[Bkt[c]])
            if c == 0:
                ckpt(33)
            for h in range(4):
                hs = slice(h * 128, (h + 1) * 128)
                P.op('pe', lambda e, hs=hs, c=c: e.matmul(bank(4)[:, hs], lhsT=KFs[:, hs], rhs=V[:, c, hs], start=True, stop=True),
                     reads=[Bkf, Bv[c]], writes=[BPS[4]])
            for h in range(4):
                hs = slice(h * 128, (h + 1) * 128)
                P.op('pe', lambda e, hs=hs, c=c: e.matmul(bank(5)[:, hs], lhsT=KBs[:, hs], rhs=V[:, c, hs], start=True, stop=True),
                     reads=[Bkb, Bv[c]], writes=[BPS[5]])
            P.op('act', lambda e, c=c: e.activation(out=UF[:, c, :], in_=bank(4), func=AF.Copy), reads=[BPS[4]], writes=[Buf_[c]])
            P.op('act', lambda e, c=c: e.activation(out=UB[:, c, :], in_=bank(5), func=AF.Copy), reads=[BPS[5]], writes=[Bub[c]])
            if c == 0:
                ckpt(34)
            for h in range(4):
                hs = slice(h * 128, (h + 1) * 128)
                P.op('dve', lambda e, c=c, h=h, hs=hs: e.scalar_tensor_tensor(out=LF[:, h, :], in0=bank(4)[:, hs], scalar=cfv[:, c, h:h + 1], in1=LF[:, h, :], op0=ALU.mult, op1=ALU.add),
                     reads=[BPS[4], Bsm, Blf], writes=[Blf])
                P.op('dve', lambda e, c=c, h=h, hs=hs: e.scalar_tensor_tensor(out=LB[:, h, :], in0=bank(5)[:, hs], scalar=cbv[:, c, h:h + 1], in1=LB[:, h, :], op0=ALU.mult, op1=ALU.add),
                     reads=[BPS[5], Bsm, Blb], writes=[Blb])
            if c == 0:
                ckpt(341)
            if c == 1:
                ckpt(342)
            if c == 2:
                ckpt(35)
            if c % 4 == 3:
                ckpt(36) if c == 3 else None
                for j in range(4):
                    js = slice(j * 128, (j + 1) * 128)
                    for bi, slot in ((6, 2), (7, 3), (3, 4)):
                        for dc in range(8):
                            P.op('pe', lambda e, dc=dc, bi=bi, slot=slot, js=js: e.matmul(bank(bi), lhsT=WSL[slot][:, dc, js], rhs=HXTG[:, dc, :], start=(dc == 0), stop=(dc == 7)),
                                 reads=[Bhxtg, Bw[slot]], writes=[BPS[bi]])
                    P.op('act', lambda e: e.activation(out=S2, in_=bank(6), func=AF.Copy), reads=[BPS[6]], writes=[Bs2])
                    P.op('dve', lambda e: e.tensor_tensor(out=S2, in0=S2, in1=bank(7), op=ALU.mult), reads=[BPS[7], Bs2], writes=[Bs2])
                    P.op('dve', lambda e, j=j: e.tensor_scalar(out=S3, in0=S2, scalar1=CW[:, j * 3 + 1:j * 3 + 2], scalar2=CBI[:, j:j + 1], op0=ALU.mult, op1=ALU.add),
                         reads=[Bs2, Bsm], writes=[Bs3])
                    u3 = S2.rearrange("p (r w) -> p r w", w=64)
                    y3 = S3.rearrange("p (r w) -> p r w", w=64)
                    P.op('dve', lambda e, j=j: e.scalar_tensor_tensor(out=y3[:, :, 1:64], in0=u3[:, :, 0:63], scalar=CW[:, j * 3:j * 3 + 1], in1=y3[:, :, 1:64], op0=ALU.mult, op1=ALU.add),
                         reads=[Bs2, Bs3, Bsm], writes=[Bs3])
                    P.op('dve', lambda e, j=j: e.scalar_tensor_tensor(out=y3[:, :, 0:63], in0=u3[:, :, 1:64], scalar=CW[:, j * 3 + 2:j * 3 + 3], in1=y3[:, :, 0:63], op0=ALU.mult, op1=ALU.add),
                         reads=[Bs2, Bs3, Bsm], writes=[Bs3])
                    P.op('dve', lambda e, j=j, g=g: e.tensor_tensor(out=CONVT[:, j, g * 512:(g + 1) * 512], in0=S3, in1=bank(3), op=ALU.mult),
                         reads=[Bs3, BPS[3]], writes=[Bconvt[g]])
                if c + 1 < NCH:
                    pre_norm(c + 1)

        ckpt(4)
        Bag1i, Bag1o = Buf("ag1i"), Buf("ag1o")
        P.dma('sp', lambda e: e.dma_start(out=ag1_in.ap()[:, 0:512], in_=LF.rearrange("p h d -> p (h d)")), reads=[Blf], writes=[Bag1i])
        P.dma('sp', lambda e: e.dma_start(out=ag1_in.ap()[:, 512:1024], in_=LB.rearrange("p h d -> p (h d)")), reads=[Blb], writes=[Bag1i])
        P.dma('pool', lambda e: e.collective_compute("AllGather", ALU.bypass, replica_groups=[[0, 1, 2, 3], [4, 5, 6, 7]],
                                                     ins=[ag1_in.ap().opt()], outs=[ag1_out.ap().opt()]),
              reads=[Bag1i], writes=[Bag1o], inc=1)
        G1 = ROWS[:, 0, :]; A2 = ROWS[:, 1, :]; SH2 = ROWS[:, 2, :]
        W5 = A.at(T0, [8, 512], BF16)
        BADA_b = A.at(T0 + 10, [512], F32, parts=2)
        MROW_b = A.at(T0 + 12, [512], F32, parts=2)
        GAIN_b = A.at(T0 + 14, [512], F32)
        Bw5s = Buf("w5s")
        mods = []
        for (blk0, dst, gsrc, p1) in ((4, G1, postmix_d, False), (8, A2, preffn_d, True), (6, SH2, None, False)):
            for hf in range(2):
                mods.append((blk0 + hf, dst, gsrc, p1, hf))

        def emit_mod(i):
            n, dst, gsrc, p1, hf = mods[i]
            if i % 2 == 0:
                wsl, bw = WSL[4], [Bw[4]]
            else:
                wsl, bw = W5, [Bxc[0], Bxc[1]]
            P.dma('pool', lambda e: e.dma_start(out=wsl, in_=wada_d[:, n * 512:(n + 1) * 512].rearrange("(c p) f -> p c f", p=128)), writes=bw,
                  sembuf=(Bw[4] if i % 2 == 0 else Bw5s))
            P.dma('sp', lambda e: e.dma_start(out=BADA_b, in_=bada_d[:, n * 512:(n + 1) * 512]), writes=[Bhxtc])
            for dc in range(8):
                P.op('pe', lambda e, dc=dc: e.matmul(bank(0)[0:2, :], lhsT=ST_[:, dc, :], rhs=wsl[:, dc, :], start=(dc == 0), stop=(dc == 7)),
                     reads=[Bcv] + bw[:1], writes=[BPS[0]])
            P.op('dve', lambda e: e.tensor_tensor(out=MROW_b, in0=bank(0)[0:2, :], in1=BADA_b, op=ALU.add), reads=[BPS[0], Bhxtc], writes=[Btmp])
            if gsrc is not None:
                P.dma('sp', lambda e: e.dma_start(out=GAIN_b, in_=gsrc[:, hf * 512:(hf + 1) * 512]), writes=[Bkr, Bkf])
            P.op('pe', lambda e: e.matmul(bank(1), lhsT=c_sel0, rhs=MROW_b, start=True, stop=True), reads=[Btmp, Bcst], writes=[BPS[1]])
            d = dst[:, hf * 512:(hf + 1) * 512]
            if gsrc is None:
                P.op('act', lambda e: e.activation(out=d, in_=bank(1), func=AF.Copy), reads=[BPS[1]], writes=[Brows, Bhxtg])
            elif p1:
                P.op('dve', lambda e: e.scalar_tensor_tensor(out=d, in0=bank(1), scalar=1.0, in1=GAIN_b, op0=ALU.add, op1=ALU.mult),
                     reads=[BPS[1], Bkr, Bkf], writes=[Brows, Bhxtg])
            else:
                P.op('dve', lambda e: e.tensor_tensor(out=d, in0=bank(1), in1=GAIN_b, op=ALU.mult), reads=[BPS[1], Bkr, Bkf], writes=[Brows, Bhxtg])

        emit_mod(0)
        emit_mod(1)
        load_piece(win_d[:, 1536:2048], 0)
        load_piece(win_d[:, 1536 + 3 * 512:3584], 1)
        load_piece(wout_d[:, 0:512], 2)
        load_piece(wout_d[:, 512:1024], 3)
        P.dma('sp', lambda e: e.dma_start(out=ROT, in_=rotq_d), writes=[Brot])
        SINF = S1.rearrange("p (h d) -> p h d", h=4)
        SINB = S2.rearrange("p (h d) -> p h d", h=4)
        P.op('dve', lambda e: e.tensor_copy(out=SINF, in_=SCF), reads=[Bs5], writes=[Bs1])
        P.op('dve', lambda e: e.tensor_copy(out=SINB, in_=SCB), reads=[Bs5], writes=[Bs2])
        for s in range(4):
            P.dma('sp', lambda e, s=s: e.dma_start(out=S4, in_=ag1_out.ap()[s * 128:(s + 1) * 128, :]), reads=[Bag1o], writes=[Bs4])
            s4f = S4[:, 0:512].rearrange("p (h d) -> p h d", h=4)
            s4b = S4[:, 512:1024].rearrange("p (h d) -> p h d", h=4)
            for h in range(4):
                P.op('dve', lambda e, s=s, h=h, s4f=s4f: e.scalar_tensor_tensor(out=SINF[:, h, :], in0=s4f[:, h, :], scalar=xco[:, s, h:h + 1], in1=SINF[:, h, :], op0=ALU.mult, op1=ALU.add),
                     reads=[Bs4, Bsm, Bs1], writes=[Bs1])
                P.op('dve', lambda e, s=s, h=h, s4b=s4b: e.scalar_tensor_tensor(out=SINB[:, h, :], in0=s4b[:, h, :], scalar=xco[:, 4 + s, h:h + 1], in1=SINB[:, h, :], op0=ALU.mult, op1=ALU.add),
                     reads=[Bs4, Bsm, Bs2], writes=[Bs2])
        tap("sinf", S1, Bs1)
        tap("sinb", S2, Bs2)
        def v4(ap):
            return ap.rearrange("p (h d) -> p h d", h=4)
        Bs4a, Bs4b, Bs5a = Buf("s4a"), Buf("s4b"), Buf("s5a")
        FB = [(SINF, Bs1), (v4(S3), Bs3), (v4(S4[:, 0:512]), Bs4a)]
        BB = [(SINB, Bs2), (v4(S4[:, 512:1024]), Bs4b), (v4(S5[:, 0:512]), Bs5a)]
        first_extra = {id(Bs4a): [Bs4], id(Bs4b): [Bs4], id(Bs5a): [Bs5]}

        def scan_step(k, c, bufs, U, BU, goff):
            (cur, bcur), (nxt, bnxt) = bufs[k % 3], bufs[(k + 1) % 3]
            uc = v4(U[:, c, :])
            extra = first_extra.pop(id(bnxt), [])
            for h in range(4):
                P.op('dve', lambda e, h=h: e.scalar_tensor_tensor(out=nxt[:, h, :], in0=cur[:, h, :], scalar=G128[:, goff + h:goff + h + 1], in1=uc[:, h, :], op0=ALU.mult, op1=ALU.add),
                     reads=[bcur, Bsm, BU[c]], writes=[bnxt] + (extra if h == 0 else []))
            P.op('act', lambda e: e.activation(out=uc, in_=cur, func=AF.Copy), reads=[bcur], writes=[BU[c]])

        for k in range(NCH):
            scan_step(k, k, FB, UF, Buf_, 0)
            scan_step(k, NCH - 1 - k, BB, UB, Bub, 4)
            if k in (1, 5, 9, 13):
                emit_mod(2 + (k - 1) // 4)
        tap("rows", ROWS.rearrange("p a b -> p (a b)"), Brows)
        P.barrier()

        ckpt(6)
        Bxm, Bhx2 = Buf("xm_scr"), Buf("hx2_scr")
        WR = A.at(152, [8, 16], F32)
        Bwr = Buf("wr")
        P.dma('sp', lambda e: e.dma_start(out=WR, in_=wr_d), reads=[Brotc], writes=[Bwr])
        QT = A.at(T0 + 15, [4, 128], BF16)
        QFT = A.at(T0 + 16, [4, 128], BF16)
        QBT = A.at(T0 + 12, [4, 128], BF16)
        XSJ = A.at(136, [D], BF16)
        XSH = A.at(138, [D], BF16)
        Bxsj, Bxsh = Buf("xsj"), Buf("xsh")
        load_x(x_d, 0, 0)
        norm_compute(A1, SH1, 0, HXTC, Bhxtc)
        Bst = Buf("stat")

        def qg_proj():
            for dc in range(8):
                P.op('pe', lambda e, dc=dc: e.matmul(bank(4), lhsT=HXTC[:, dc, :], rhs=WSL[0][:, dc, :], start=(dc == 0), stop=(dc == 7)),
                     reads=[Bhxtc, Bw[0]], writes=[BPS[4]])
            for dc in range(8):
                P.op('pe', lambda e, dc=dc: e.matmul(bank(5), lhsT=HXTC[:, dc, :], rhs=WSL[1][:, dc, :], start=(dc == 0), stop=(dc == 7)),
                     reads=[Bhxtc, Bw[1]], writes=[BPS[5]])

        qg_proj()
        for c in range(NCH):
            cs = slice(c * 128, (c + 1) * 128)
            xi = c % 2
            w4 = [Bw[4]] if c == 0 else []
            if c + 1 < NCH:
                load_x(x_d, (c + 1) * 128, (c + 1) % 2)
            rotary(bank(4), ROT[:, 0, c, :], ROT[:, 1, c, :], KR, Bkr, BPS[4], Brot)
            P.op('act', lambda e: e.activation(out=S1, in_=bank(5), func=AF.Silu), reads=[BPS[5]], writes=[Bs1])
            tb = bank_bf(3).rearrange("p (h t) -> p h t", h=8)
            for h in range(4):
                P.op('pe', lambda e, h=h: e.transpose(tb[:, h, :], KR[:, h * 128:(h + 1) * 128], IDB), reads=[Bkr, Bidb], writes=[BPS[3]])
            P.op('act', lambda e: e.activation(out=QT, in_=tb[:, 0:4, :], func=AF.Copy), reads=[BPS[3]], writes=[Bkf])
            P.op('dve', lambda e: e.tensor_tensor(out=QFT, in0=tb[:, 0:4, :], in1=XF, op=ALU.mult), reads=[BPS[3], Btab], writes=[Bkb])
            P.op('dve', lambda e: e.tensor_tensor(out=QBT, in0=tb[:, 0:4, :], in1=XB, op=ALU.mult), reads=[BPS[3], Btab], writes=[Btmp])
            for h in range(4):
                P.op('pe', lambda e, h=h, cs=cs: e.matmul(bank(4)[:, h * 128:(h + 1) * 128], lhsT=KT[:, h, cs], rhs=QT[:, h, :], start=True, stop=True),
                     reads=[Bkt[c], Bkf], writes=[BPS[4]])
            STb = S2.bitcast(BF16)[:, 0:512]
            P.op('dve', lambda e: e.tensor_tensor(out=STb.rearrange("p (h i) -> p h i", h=4), in0=bank(4).rearrange("p (h i) -> p h i", h=4), in1=DT, op=ALU.mult),
                 reads=[BPS[4], Btab], writes=[Bs2])
            for h in range(4):
                hs = slice(h * 128, (h + 1) * 128)
                P.op('pe', lambda e, hs=hs, c=c: e.matmul(bank(5)[:, hs], lhsT=STb[:, hs], rhs=V[:, c, hs], start=True, stop=False), reads=[Bs2, Bv[c]], writes=[BPS[5]])
                P.op('pe', lambda e, hs=hs, h=h, c=c: e.matmul(bank(5)[:, hs], lhsT=QFT[:, h, :], rhs=UF[:, c, hs], start=False, stop=False), reads=[Bkb, Buf_[c]], writes=[BPS[5]])
                P.op('pe', lambda e, hs=hs, h=h, c=c: e.matmul(bank(5)[:, hs], lhsT=QBT[:, h, :], rhs=UB[:, c, hs], start=False, stop=True), reads=[Btmp, Bub[c]], writes=[BPS[5]])
            if c == 0:
                P.op('act', lambda e: e.activation(out=S3, in_=bank(5), func=AF.Copy), reads=[BPS[5]], writes=[Bs3])
                tap("y0", S3, Bs3)
            yv = bank(5).rearrange("p (h d) -> p h d", h=4)
            P.op('act', lambda e: e.activation(out=S3, in_=bank(5), func=AF.Square), reads=[BPS[5]], writes=[Bs3])
            P.op('dve', lambda e: e.tensor_reduce(out=STAT[:, 4:8], in_=yv, axis=AX.X, op=ALU.add), reads=[BPS[5]], writes=[Bst])
            P.op('dve', lambda e: e.tensor_reduce(out=STAT[:, 8:12], in_=S3.rearrange("p (h d) -> p h d", h=4), axis=AX.X, op=ALU.add), reads=[Bs3], writes=[Bst])
            P.op('dve', lambda e: e.tensor_scalar(out=STAT[:, 4:8], in0=STAT[:, 4:8], scalar1=1.0 / 128, scalar2=None, op0=ALU.mult), reads=[Bst], writes=[Bst])
            P.op('dve', lambda e: e.tensor_tensor(out=STAT[:, 12:16], in0=STAT[:, 4:8], in1=STAT[:, 4:8], op=ALU.mult), reads=[Bst], writes=[Bst])
            P.op('dve', lambda e: e.scalar_tensor_tensor(out=STAT[:, 8:12], in0=STAT[:, 8:12], scalar=1.0 / 128, in1=STAT[:, 12:16], op0=ALU.mult, op1=ALU.subtract), reads=[Bst], writes=[Bst])
            P.op('act', lambda e: e.activation(out=STAT[:, 8:12], in_=STAT[:, 8:12], func=AF.Sqrt, bias=c_eps), reads=[Bst, Bcst], writes=[Bst])
            P.op('dve', lambda e: e.reciprocal(out=STAT[:, 8:12], in_=STAT[:, 8:12]), reads=[Bst], writes=[Bst])
            for h in range(4):
                hs = slice(h * 128, (h + 1) * 128)
                P.op('dve', lambda e, h=h, hs=hs: e.tensor_scalar(out=S3[:, hs], in0=bank(5)[:, hs], scalar1=STAT[:, 4 + h:5 + h], scalar2=STAT[:, 8 + h:9 + h], op0=ALU.subtract, op1=ALU.mult),
                     reads=[BPS[5], Bst], writes=[Bs3])
            P.op('dve', lambda e: e.tensor_tensor(out=S3, in0=S3, in1=NGR, op=ALU.mult), reads=[Bs3, Bngr], writes=[Bs3])
            P.op('dve', lambda e: e.tensor_tensor(out=KR, in0=S3, in1=S1, op=ALU.mult), reads=[Bs3, Bs1], writes=[Bkr])
            for h in range(4):
                P.op('pe', lambda e, h=h: e.transpose(tb[:, 4 + h, :], KR[:, h * 128:(h + 1) * 128], IDB), reads=[Bkr, Bidb], writes=[BPS[3]])
            ROTt = S2.bitcast(BF16)[:, 512:1024].rearrange("p (h t) -> p h t", h=4)
            P.op('act', lambda e: e.activation(out=ROTt, in_=tb[:, 4:8, :], func=AF.Copy), reads=[BPS[3]], writes=[Bs2])
            for hf in range(2):
                for kc in range(8):
                    if kc < 4:
                        lhs = CONVT[:, kc, cs]
                        rd = [Bconvt[c // 4]]
                    else:
                        lhs = ROTt[:, kc - 4, :]
                        rd = [Bs2]
                    P.op('pe', lambda e, hf=hf, kc=kc, lhs=lhs: e.matmul(bank(6 + hf), lhsT=lhs, rhs=WSL[2 + hf][:, kc, :], start=(kc == 0), stop=(kc == 7)),
                         reads=rd + [Bw[2 + hf]], writes=[BPS[6 + hf]])
            if c + 1 < NCH:
                norm_compute(A1, SH1, (c + 1) % 2, HXTC, Bhxtc)
                qg_proj()
            M = psb[3][:, :]
            P.op('act', lambda e: e.activation(out=XSJ, in_=M, func=AF.Square, accum_out=STAT[:, 16:17]), reads=[BPS[6], BPS[7]], writes=[Bxsj, Bst] + w4)
            P.op('act', lambda e: e.activation(out=STAT[:, 17:18], in_=STAT[:, 16:17], func=AF.Sqrt, scale=1.0 / D, bias=c_eps), reads=[Bst, Bcst], writes=[Bst])
            P.op('dve', lambda e: e.reciprocal(out=STAT[:, 17:18], in_=STAT[:, 17:18]), reads=[Bst], writes=[Bst])
            P.op('dve', lambda e: e.scalar_tensor_tensor(out=S4, in0=M, scalar=STAT[:, 17:18], in1=G1, op0=ALU.mult, op1=ALU.mult), reads=[BPS[6], BPS[7], Bst, Brows], writes=[Bs4])
            P.op('dve', lambda e, xi=xi: e.tensor_tensor(out=S4, in0=S4, in1=XC[xi], op=ALU.add), reads=[Bs4, Bxc[xi]], writes=[Bs4])
            P.dma('sp', lambda e, c=c: e.dma_start(out=xm_scr.ap()[c * 128:(c + 1) * 128, :], in_=S4), reads=[Bs4], writes=[Bxm])
            if c == 0:
                tap("xm0", S4, Bs4)
            P.op('act', lambda e: e.activation(out=XSJ, in_=S4, func=AF.Square, accum_out=STAT[:, 18:19]), reads=[Bs4], writes=[Bxsj, Bst])
            P.op('act', lambda e: e.activation(out=STAT[:, 19:20], in_=STAT[:, 18:19], func=AF.Sqrt, scale=1.0 / D, bias=c_eps), reads=[Bst, Bcst], writes=[Bst])
            P.op('dve', lambda e: e.reciprocal(out=STAT[:, 19:20], in_=STAT[:, 19:20]), reads=[Bst], writes=[Bst])
            P.op('dve', lambda e: e.scalar_tensor_tensor(out=S5, in0=S4, scalar=STAT[:, 19:20], in1=A2, op0=ALU.mult, op1=ALU.mult), reads=[Bs4, Bst, Brows], writes=[Bs5])
            P.op('dve', lambda e: e.tensor_tensor(out=S5, in0=S5, in1=SH2, op=ALU.add), reads=[Bs5, Brows], writes=[Bs5])
            P.op('act', lambda e: e.activation(out=XSH, in_=S5, func=AF.Copy), reads=[Bs5], writes=[Bxsh] + w4)
            P.dma('sp', lambda e, c=c: e.dma_start(out=hx2_scr.ap()[c * 128:(c + 1) * 128, :], in_=XSH), reads=[Bxsh], writes=[Bhx2])
            tf = psb[0][:, :].rearrange("p (c t) -> p c t", c=8)
            for dc in range(8):
                P.op('pe', lambda e, dc=dc: e.transpose(tf[:, dc, :], S5[:, dc * 128:(dc + 1) * 128], c_identf), reads=[Bs5, Bcst], writes=[BPS[0], BPS[1]])
            HX2T = S4.rearrange("p (c t) -> p c t", c=8)
            P.op('act', lambda e: e.activation(out=HX2T, in_=tf, func=AF.Copy), reads=[BPS[0], BPS[1]], writes=[Bs4])
            for dc in range(8):
                P.op('pe', lambda e, dc=dc: e.matmul(bank(2)[:, 0:16], lhsT=HX2T[:, dc, :], rhs=WR[:, dc, :], start=(dc == 0), stop=(dc == 7)), reads=[Bs4, Bwr], writes=[BPS[2]])
            P.op('dve', lambda e: e.tensor_tensor(out=STAT[:, 0:16], in0=bank(2)[:, 0:16], in1=BRR, op=ALU.add), reads=[BPS[2], Bsm], writes=[Bst])
            P.op('dve', lambda e: e.tensor_reduce(out=STAT[:, 20:21], in_=STAT[:, 0:16], axis=AX.X, op=ALU.max), reads=[Bst], writes=[Bst])
            P.op('dve', lambda e: e.tensor_scalar(out=STAT[:, 20:21], in0=STAT[:, 20:21], scalar1=-1.0, scalar2=None, op0=ALU.mult), reads=[Bst], writes=[Bst])
            P.op('act', lambda e: e.activation(out=STAT[:, 0:16], in_=STAT[:, 0:16], func=AF.Exp, bias=STAT[:, 20:21], accum_out=STAT[:, 21:22]), reads=[Bst], writes=[Bst])
            P.op('dve', lambda e: e.reciprocal(out=STAT[:, 21:22], in_=STAT[:, 21:22]), reads=[Bst], writes=[Bst])
            P.op('dve', lambda e, c=c: e.tensor_scalar(out=AFF[:, c, :], in0=STAT[:, 0:16], scalar1=STAT[:, 21:22], scalar2=None, op0=ALU.mult), reads=[Bst], writes=[Baff])
        tap("aff", AFF.rearrange("p c e -> p (c e)"), Baff)

        ckpt(7)
        P.barrier()
        ACC = A.at(24, [NCH, D], F32)
        HX2 = A.at(88, [NCH, D], BF16)
        Bacc = [Buf(f"acc{c}") for c in range(NCH)]
        Bhx = Buf("hx2")
        RING = [A.at(120 + 8 * i, [8, 512], BF16) for i in range(5)]
        Bring = [Buf(f"ring{i}") for i in range(5)]
        XST = A.at(160, [8, NS], BF16)
        HT = A.at(166, [16, NS], BF16)
        YG = A.at(178, [3, D], BF16)
        PT_ = A.at(184, [NCH, CAPG], BF16)
        PTT = A.at(190, [4, D], BF16)
        Bxst, Bht, Byg, Bpt, Bptt = Buf("xst"), Buf("ht"), Buf("yg"), Buf("pt"), Buf("ptt")
        SA = A.at(198, [NS], F32)
        Bsa = Buf("sa")
        AFFEM = A.at(160, [TPC], F32, parts=16)
        MASK = A.at(168, [TPC], F32, parts=16)
        CUM = A.at(176, [TPC], F32, parts=16)
        AFFALL = A.at(184, [1024], F32)
        Baffem, Bmask, Bcum, Baffall = Buf("affem"), Buf("mask"), Buf("cum"), Buf("affall")
        CMPS = A.at(188, [1024], F32)
        Bcmp = Buf('cmp')
        ONES16 = A.at(192, [1024], F32, parts=16)
        Bones16 = Buf('ones16')
        P.op('dve', lambda e: e.memset(ONES16, 1.0), writes=[Bones16])
        P.dma('sp', lambda e: e.dma_start(out=HX2, in_=hx2_scr.ap().rearrange("(c p) d -> p c d", p=128)), reads=[Bhx2], writes=[Bhx])
        G2 = A.at(199.5, [D], F32)
        Bg2 = Buf("g2")
        MROW2 = A.at(128, [D], F32, parts=2)
        CV2 = A.at(132, [8, 2], F32)
        ST2 = A.at(132.5, [8, 2], BF16)
        WT = [A.at(136, [8, 512], BF16), A.at(152, [8, 512], BF16)]
        BADA2 = A.at(144, [D], F32, parts=2)
        GAIN2 = A.at(148, [D], F32)
        Bfin, Bbada2, Bgain2, Bmrow2 = Buf("fin"), Buf("bada2"), Buf("gain2"), Buf("mrow2")
        Bwt = [Buf("wt0"), Buf("wt1")]
        P.dma('sp', lambda e: e.dma_start(out=CV2, in_=cvec_d), writes=[Bfin])
        P.op('act', lambda e: e.activation(out=ST2, in_=CV2, func=AF.Silu), reads=[Bfin], writes=[Bfin])
        for hf in range(2):
            P.dma('pool', lambda e, hf=hf: e.dma_start(out=WT[hf], in_=wada_d[:, (10 + hf) * 512:(11 + hf) * 512].rearrange("(c p) f -> p c f", p=128)), writes=[Bwt[hf]])
        P.dma('sp', lambda e: e.dma_start(out=BADA2, in_=bada_d[:, 10 * 512:12 * 512]), writes=[Bbada2])
        P.dma('sp', lambda e: e.dma_start(out=GAIN2, in_=postffn_d), writes=[Bgain2])
        for c in range(NCH):
            P.op('dve', lambda e, c=c: e.memset(ACC[:, c, :], 0.0), writes=[Bacc[c]])
        for c in range(NCH):
            P.op('pe', lambda e, c=c: e.transpose(bank(c // 4)[0:16, (c % 4) * 128:(c % 4) * 128 + 128], AFF[:, c, :], c_identf), reads=[Baff, Bcst], writes=[BPS[c // 4]])
        for q in range(4):
            P.op('act', lambda e, q=q: e.activation(out=AFFEM[:, q * 512:(q + 1) * 512], in_=bank(q)[0:16, :], func=AF.Copy), reads=[BPS[q]], writes=[Baffem])
        Bag2i, Bag2o = Buf("ag2i"), Buf("ag2o")
        P.dma('sp', lambda e: e.dma_start(out=ag2_in.ap(), in_=AFFEM), reads=[Baffem], writes=[Bag2i])
        P.dma('pool', lambda e: e.collective_compute("AllGather", ALU.bypass, replica_groups=[[0, 1, 2, 3], [4, 5, 6, 7]],
                                                     ins=[ag2_in.ap().opt()], outs=[ag2_out.ap().opt()]),
              reads=[Bag2i], writes=[Bag2o], inc=1)
        P.dma('sp', lambda e: e.dma_start(out=AFFALL, in_=ag2_out.ap().rearrange("r (h t) -> (r h) t", h=2)), reads=[Bag2o], writes=[Baffall])
        P.op('dve', lambda e: e.memset(MID, 0.5), writes=[Bsm])
        for k in range(1, NBIS + 1):
            hk = 2.0 ** -(k)
            hn = 2.0 ** -(k + 1)
            P.op('dve', lambda e: e.tensor_scalar(out=CMPS, in0=AFFALL, scalar1=MID, scalar2=None, op0=ALU.is_ge, op1=ALU.add, accum_out=CNT),
                 reads=[Baffall, Bsm], writes=[Bcmp, Bsm])
            P.op('pe', lambda e: e.matmul(bank(0)[:, 0:1], lhsT=c_grp, rhs=CNT, start=True, stop=True), reads=[Bsm, Bcst], writes=[BPS[0]])
            P.op('dve', lambda e, hk=hk: e.tensor_scalar(out=TT, in0=bank(0)[:, 0:1], scalar1=KCAP, scalar2=hk, op0=ALU.is_ge, op1=ALU.mult), reads=[BPS[0]], writes=[Bsm])
            P.op('dve', lambda e, hn=hn: e.scalar_tensor_tensor(out=MID, in0=MID, scalar=-hn, in1=TT, op0=ALU.add, op1=ALU.add), reads=[Bsm], writes=[Bsm])
        P.op('dve', lambda e: e.tensor_scalar(out=THR, in0=MID, scalar1=-(2.0 ** -(NBIS + 1)), scalar2=None, op0=ALU.add), reads=[Bsm], writes=[Bsm])
        P.op('pe', lambda e: e.matmul(bank(1)[0:16, 0:1], lhsT=c_sel, rhs=THR, start=True, stop=True), reads=[Bsm, Bcst], writes=[BPS[1]])
        P.op('dve', lambda e: e.tensor_copy(out=THREM[0:16, :], in_=bank(1)[0:16, 0:1]), reads=[BPS[1]], writes=[Bsm])
        tap("thr", THREM[0:16, :], Bsm)
        P.op('dve', lambda e: e.tensor_scalar(out=MASK, in0=AFFEM, scalar1=THREM[0:16, :], scalar2=None, op0=ALU.is_ge), reads=[Baffem, Bsm], writes=[Bmask])
        for g in range(NG):
            gs = slice(g * 1024, (g + 1) * 1024)
            P.op('dve', lambda e, gs=gs: e.tensor_tensor_scan(out=CUM[:, gs], data0=ONES16, data1=MASK[:, gs], initial=0.0, op0=ALU.mult, op1=ALU.add),
                 reads=[Bmask, Bones16], writes=[Bcum])
        P.op('dve', lambda e: e.tensor_tensor(out=CUM, in0=CUM, in1=MASK, op=ALU.mult), reads=[Bmask, Bcum], writes=[Bcum])
        P.op('dve', lambda e: e.tensor_scalar(out=CUM, in0=CUM, scalar1=-1.0, scalar2=None, op0=ALU.add), reads=[Bcum], writes=[Bcum])
        P.op('act', lambda e: e.activation(out=POSM, in_=CUM, func=AF.Copy), reads=[Bcum], writes=[Bposm])
        for c in range(NCH):
            P.op('pe', lambda e, c=c: e.transpose(bank(2)[:, c * 16:(c + 1) * 16], CUM[:, c * 128:(c + 1) * 128], c_identf[0:16, 0:16]), reads=[Bcum, Bcst], writes=[BPS[2]])
        P.op('dve', lambda e: e.tensor_copy(out=POST.rearrange("p c e -> p (c e)"), in_=bank(2)[:, 0:256]), reads=[BPS[2]], writes=[Bpost])
        P.op('dve', lambda e: e.scalar_tensor_tensor(out=GA.rearrange("p c e -> p (c e)"), in0=POST.rearrange("p c e -> p (c e)"), scalar=0.0, in1=AFF.rearrange("p c e -> p (c e)"), op0=ALU.is_ge, op1=ALU.mult),
             reads=[Bpost, Baff], writes=[Bga])
        tap("post", POST.rearrange("p c e -> p (c e)"), Bpost)
        for hf in range(2):
            hsl = slice(hf * 512, (hf + 1) * 512)
            for dc in range(8):
                P.op('pe', lambda e, dc=dc, hf=hf: e.matmul(bank(0)[0:2, :], lhsT=ST2[:, dc, :], rhs=WT[hf][:, dc, :], start=(dc == 0), stop=(dc == 7)), reads=[Bfin, Bwt[hf]], writes=[BPS[0]])
            P.op('dve', lambda e, hsl=hsl: e.tensor_tensor(out=MROW2[:, hsl], in0=bank(0)[0:2, :], in1=BADA2[:, hsl], op=ALU.add), reads=[BPS[0], Bbada2], writes=[Bmrow2])
            P.op('pe', lambda e, hsl=hsl: e.matmul(bank(1), lhsT=c_sel0, rhs=MROW2[:, hsl], start=True, stop=True), reads=[Bmrow2, Bcst], writes=[BPS[1]])
            P.op('dve', lambda e, hsl=hsl: e.tensor_tensor(out=G2[:, hsl], in0=bank(1), in1=GAIN2[:, hsl], op=ALU.mult), reads=[BPS[1], Bgain2], writes=[Bg2])
        P.barrier()

        ckpt(8)
        ring_i = [0]

        def ring_load(src_ap):
            i = ring_i[0] % 5
            ring_i[0] += 1
            P.dma('pool', lambda e: e.dma_start(out=RING[i], in_=src_ap.rearrange("(c p) f -> p c f", p=128)), writes=[Bring[i]])
            return i

        def expert_pieces(ex):
            lst = []
            for j in range(4):
                lst.append(wg_d[ex, :, j * 512:(j + 1) * 512])
                lst.append(wu_d[ex, :, j * 512:(j + 1) * 512])
            for hf in range(2):
                for a in range(2):
                    lst.append(wd_d[ex, a * 1024:(a + 1) * 1024, hf * 512:(hf + 1) * 512])
            return lst

        all_pieces = []
        for ex in range(ned):
            all_pieces += expert_pieces(ex)
        piece_slot = {}
        next_load = [0]

        def ensure_loaded(upto):
            while next_load[0] <= upto and next_load[0] < len(all_pieces):
                piece_slot[next_load[0]] = ring_load(all_pieces[next_load[0]])
                next_load[0] += 1

        SCOL = [c_scv[:, 0:1], c_scv[:, 1:2], c_scv[:, 2:3], c_scv[:, 3:4]]
        def gen_pt(ex):
            for c in range(NCH):
                P.op('dve', lambda e, c=c, ex=ex: e.tensor_scalar(out=PT_[:, c, :], in0=c_iota, scalar1=POST[:, c, ex:ex + 1], scalar2=None, op0=ALU.is_equal),
                     reads=[Bpost, Bcst], writes=[Bpt])

        gen_pt(0)
        for ex in range(ned):
            base = ex * 12
            ensure_loaded(base + 3)
            for g in range(NG):
                for dp in range(4):
                    for k in range(2):
                        dc = dp * 2 + k
                        for cc in range(8):
                            c = g * 8 + cc
                            P.op('pe', lambda e, dp=dp, k=k, dc=dc, c=c, cc=cc: e.matmul(bank(dp)[:, k * CAPG:(k + 1) * CAPG], lhsT=HX2[:, c, dc * 128:(dc + 1) * 128], rhs=PT_[:, c, :], start=(cc == 0), stop=(cc == 7)),
                                 reads=[Bhx, Bpt], writes=[BPS[dp]])
                    P.op('act', lambda e, dp=dp, g=g: e.activation(out=XST[:, dp * 2:dp * 2 + 2, g * CAPG:(g + 1) * CAPG], in_=bank(dp)[:, 0:2 * CAPG].rearrange("p (k s) -> p k s", k=2), func=AF.Copy),
                         reads=[BPS[dp]], writes=[Bxst])
            for q in range(4):
                P.op('pe', lambda e, q=q, ex=ex: e.matmul(bank(4 + (q % 2)), lhsT=SELB[:, ex, :], rhs=POSM[:, q * 512:(q + 1) * 512], start=True, stop=True),
                     reads=[Bselb, Bposm], writes=[BPS[4 + (q % 2)]])
                g = q // 2
                for k in range(2):
                    idx = g * 2 + k
                    P.op('dve', lambda e, q=q, idx=idx: e.tensor_scalar(out=PTT[:, idx, (q % 2) * 512:(q % 2) * 512 + 512], in0=bank(4 + (q % 2)), scalar1=SCOL[idx], scalar2=None, op0=ALU.is_equal),
                         reads=[BPS[4 + (q % 2)], Bcst], writes=[Bptt])
            for j in range(4):
                ensure_loaded(base + 2 * j + 4)
                sg_ = piece_slot[base + 2 * j]
                su_ = piece_slot[base + 2 * j + 1]
                for f in range(4):
                    fc = j * 4 + f
                    fs = slice(f * 128, (f + 1) * 128)
                    ba = 4 + (fc % 2) * 2
                    for dc in range(8):
                        P.op('pe', lambda e, dc=dc, fs=fs, ba=ba, sg_=sg_: e.matmul(bank(ba)[:, 0:NS], lhsT=RING[sg_][:, dc, fs], rhs=XST[:, dc, :], start=(dc == 0), stop=(dc == 7)),
                             reads=[Bring[sg_], Bxst], writes=[BPS[ba]])
                    for dc in range(8):
                        P.op('pe', lambda e, dc=dc, fs=fs, ba=ba, su_=su_: e.matmul(bank(ba + 1)[:, 0:NS], lhsT=RING[su_][:, dc, fs], rhs=XST[:, dc, :], start=(dc == 0), stop=(dc == 7)),
                             reads=[Bring[su_], Bxst], writes=[BPS[ba + 1]])
                    P.op('act', lambda e, ba=ba: e.activation(out=SA, in_=bank(ba)[:, 0:NS], func=AF.Silu), reads=[BPS[ba]], writes=[Bsa])
                    P.op('dve', lambda e, ba=ba, fc=fc: e.tensor_tensor(out=HT[:, fc, :], in0=SA, in1=bank(ba + 1)[:, 0:NS], op=ALU.mult), reads=[Bsa, BPS[ba + 1]], writes=[Bht])
            if ex + 1 < ned:
                gen_pt(ex + 1)
            for hf in range(2):
                ensure_loaded(base + 8 + 2 * hf + 4)
                sd = [piece_slot[base + 8 + 2 * hf], piece_slot[base + 8 + 2 * hf + 1]]
                for sc in range(3):
                    bi = (hf * 3 + sc) % 4
                    for fc in range(16):
                        P.op('pe', lambda e, fc=fc, sc=sc, bi=bi, sd=sd: e.matmul(bank(bi), lhsT=HT[:, fc, sc * 128:(sc + 1) * 128], rhs=RING[sd[fc // 8]][:, fc % 8, :], start=(fc == 0), stop=(fc == 15)),
                             reads=[Bht, Bring[sd[fc // 8]]], writes=[BPS[bi]])
                    P.op('act', lambda e, sc=sc, hf=hf, bi=bi: e.activation(out=YG[:, sc, hf * 512:(hf + 1) * 512], in_=bank(bi), func=AF.Copy), reads=[BPS[bi]], writes=[Byg])
            for c in range(NCH):
                g = c // 8
                tl = (c % 8) * 128
                bo = 2 * (c % 2)
                for hf in range(2):
                    for k in range(2):
                        idx = g * 2 + k
                        sc = g + k
                        P.op('pe', lambda e, hf=hf, k=k, idx=idx, sc=sc, tl=tl, bo=bo: e.matmul(bank(4 + bo + hf), lhsT=PTT[:, idx, tl:tl + 128], rhs=YG[:, sc, hf * 512:(hf + 1) * 512], start=(k == 0), stop=(k == 1)),
                             reads=[Bptt, Byg], writes=[BPS[4 + bo + hf]])
                O = psb[2 + (c % 2)][:, :]
                P.op('dve', lambda e, c=c, ex=ex, O=O: e.scalar_tensor_tensor(out=ACC[:, c, :], in0=O, scalar=GA[:, c, ex:ex + 1], in1=ACC[:, c, :], op0=ALU.mult, op1=ALU.add),
                     reads=[BPS[4 + bo], BPS[5 + bo], Bga, Bacc[c]], writes=[Bacc[c]])
        tap("acc0", ACC[:, 0, :], Bacc[0])

        ckpt(9)
        P.barrier()
        NXM = 4
        XM = [A.at(120 + 4 * i, [D], F32) for i in range(NXM)]
        SQ = A.at(136, [D], F32)
        SQJ = A.at(140, [D], F32)
        Bxmm = [Buf(f"xmm{i}") for i in range(NXM)]
        Bsq, Bsqj = Buf("sq"), Buf("sqj")
        Bstf = [Buf("stf0"), Buf("stf1")]
        Bout = Buf("out")
        for c in range(NCH):
            i = c % NXM
            st = STAT[:, 24 + 2 * (c % 2):26 + 2 * (c % 2)]
            bst = Bstf[c % 2]
            P.dma('sp', lambda e, c=c, i=i: e.dma_start(out=XM[i], in_=xm_scr.ap()[c * 128:(c + 1) * 128, :]), reads=[Bxm], writes=[Bxmm[i]])
            P.op('act', lambda e, c=c, st=st: e.activation(out=SQJ, in_=ACC[:, c, :], func=AF.Square, accum_out=st[:, 0:1]), reads=[Bacc[c]], writes=[Bsqj, bst])
            P.op('act', lambda e, st=st: e.activation(out=st[:, 1:2], in_=st[:, 0:1], func=AF.Sqrt, scale=1.0 / D, bias=c_eps), reads=[bst, Bcst], writes=[bst])
            P.op('dve', lambda e, st=st: e.reciprocal(out=st[:, 1:2], in_=st[:, 1:2]), reads=[bst], writes=[bst])
            P.op('dve', lambda e, c=c, st=st: e.scalar_tensor_tensor(out=SQ, in0=ACC[:, c, :], scalar=st[:, 1:2], in1=G2, op0=ALU.mult, op1=ALU.mult), reads=[Bacc[c], bst, Bg2], writes=[Bsq])
            P.op('dve', lambda e, i=i: e.tensor_tensor(out=XM[i], in0=XM[i], in1=SQ, op=ALU.add), reads=[Bsq, Bxmm[i]], writes=[Bxmm[i]])
            out_toks.append(P.dma('sp', lambda e, c=c, i=i: e.dma_start(out=out_d[c * 128:(c + 1) * 128, :], in_=XM[i]), reads=[Bxmm[i]], writes=[Bout], sembuf=Bxmm[i]))
        P.wait_all('sp', [t for t in out_toks if t is not None])
        with nc.Block() as block:
            P.emit(block)
    return nc


def _host_consts(r):
    cst = np.zeros((128, 1600), np.float32)
    p = np.arange(128, dtype=np.float32)
    i = np.arange(128, dtype=np.float32)
    cst[:, 0:192] = np.arange(192, dtype=np.float32)[None, :]
    rel = i[None, :] - p[:, None]
    cst[:, 192:320] = np.maximum(rel, 0)
    cst[:, 320:448] = (rel >= 0)
    cst[:, 448:576] = np.maximum(-rel, 0)
    cst[:, 576:704] = (rel < 0)
    cst[:, 704:832] = (i + 1)[None, :]
    cst[:, 832:960] = (128 - i)[None, :]
    cst[:, 960:1088] = np.eye(128, dtype=np.float32)
    cst[:, 1088] = 127 - p
    cst[:, 1089] = p
    for c in range(2):
        cst[:, 1090 + c] = 255 - (c * 128 + p)
        cst[:, 1092 + c] = c * 128 + p
    for idx, (g, sc) in enumerate(((0, 0), (0, 1), (1, 1), (1, 2))):
        v = 128 * sc + p - g * CAPG
        cst[:, 1094 + idx] = np.where((v >= 0) & (v < CAPG), v, -5.0)
    pe = (np.arange(128) // 2) % 16
    cst[:, 1100:1228] = (pe[:, None] == pe[None, :])
    sel = np.zeros((128, 16), np.float32)
    for e in range(16):
        sel[2 * e, e] = 1
    cst[:, 1228:1244] = sel
    cst[:, 1244:1260] = (128 * (15 - np.arange(16)))[None, :]
    cst[:, 1260:1276] = (128 * np.arange(16))[None, :]
    xe = np.zeros(10, np.float32)
    xm = np.zeros(10, np.float32)
    for s in range(4):
        if s < r:
            xe[s] = 2048 * (r - 1 - s); xm[s] = 1
        if s > r:
            xe[4 + s] = 2048 * (s - r - 1); xm[4 + s] = 1
    xe[8] = 2048 * r; xm[8] = 1
    xe[9] = 2048 * (3 - r); xm[9] = 1
    cst[:, 1276:1286] = xe[None, :]
    cst[:, 1432:1442] = xm[None, :]
    cst[:, 1300:1428] = 1.0
    cst[0, 1428] = 1.0
    cst[1, 1429] = 1.0
    cst[:, 1430] = 1e-6
    cst[0, 1442:1570] = 1.0
    half = 64
    inv = (1.0 / (10000.0 ** (np.arange(half, dtype=np.float32) / half))).astype(np.float32)
    def tabs(pos, scale):
        ang = pos[:, None].astype(np.float32) * inv[None, :]
        return (np.cos(ang) * scale).astype(np.float32), (np.sin(ang) * scale).astype(np.float32)
    pos = (256 + r * 2048 + np.arange(2048)).astype(np.float32)
    cq, sq = tabs(pos, 1.0)
    ck, sk = tabs(pos, 128.0 ** -0.5)
    cc, sc_ = tabs(np.arange(256).astype(np.float32), 128.0 ** -0.5)
    def lay(t, n):
        return t.reshape(n, 128, 64).transpose(1, 0, 2)
    rotq = np.stack([lay(cq, 16), lay(sq, 16)], axis=1)
    rotk = np.stack([lay(ck, 16), lay(sk, 16)], axis=1)
    rotc = np.stack([lay(cc, 2), lay(sc_, 2)], axis=1)
    return cst, np.ascontiguousarray(rotq), np.ascontiguousarray(rotk), np.ascontiguousarray(rotc)


def make_in_maps(x, c, ctx, c_ctx, w_ada, b_ada, pre_mix_g, post_mix_g, pre_ffn_g, post_ffn_g,
                 w_in, conv_w, conv_b, ret_decay_logit, ret_norm_g, w_out,
                 w_router, b_router, w_gate, w_up, w_down):
    f = lambda a: np.ascontiguousarray(np.asarray(a, dtype=np.float32))
    x, c, ctx, c_ctx = f(x), f(c), f(ctx), f(c_ctx)
    rep = lambda v: np.ascontiguousarray(np.broadcast_to(f(v).reshape(1, -1), (128, f(v).size)))
    shared = {
        "w_ada": f(w_ada[0]), "b_ada2": np.ascontiguousarray(np.broadcast_to(f(b_ada[0])[None, :], (2, 6 * D))),
        "pmg": np.ascontiguousarray(f(pre_mix_g[0]).reshape(8, 128).T),
        "postmix_row": rep(post_mix_g[0]), "preffn_row": rep(pre_ffn_g[0]), "postffn_row": rep(post_ffn_g[0]),
        "w_in": f(w_in[0]),
        "convw": np.ascontiguousarray(f(conv_w[0]).reshape(3, 4, 128).transpose(2, 1, 0)),
        "convb": np.ascontiguousarray(f(conv_b[0]).reshape(4, 128).T),
        "decay": rep(f(ret_decay_logit[0]).reshape(-1)),
        "retg_row": rep(ret_norm_g[0]),
        "w_out": f(w_out[0]),
        "w_router": np.ascontiguousarray(f(w_router[0]).reshape(8, 128, 16).transpose(1, 0, 2)),
        "brouter_row": rep(b_router[0]),
        "w_gate": f(w_gate[0]), "w_up": f(w_up[0]), "w_down": f(w_down[0]),
        "identb": np.eye(128, dtype=np.float32).astype(ml_dtypes.bfloat16),
    }
    selb = np.zeros((16, NE, 128), np.float32)
    for e in range(NE):
        selb[e, e, :] = 1
    shared["selb"] = selb.astype(ml_dtypes.bfloat16)
    maps = []
    for j in range(8):
        b, r = j // 4, j % 4
        cst, rotq, rotk, rotc = _host_consts(r)
        cv = np.stack([c[b].reshape(8, 128).T, c_ctx.reshape(8, 128).T], axis=-1)
        m = dict(shared)
        m.update({"x": np.ascontiguousarray(x[b, r * TPC:(r + 1) * TPC]), "ctx": np.ascontiguousarray(ctx[b]),
                  "cvec": np.ascontiguousarray(cv.astype(np.float32)), "cst": cst, "rotq": rotq, "rotk": rotk, "rotc": rotc})
        maps.append(m)
    return maps


_NC_CACHE = {}


def kernel(**inputs):
    if "nc" not in _NC_CACHE:
        _NC_CACHE["nc"] = build()
    nc = _NC_CACHE["nc"]
    maps = make_in_maps(**inputs)
    res = run_bass_kernel_spmd(nc, maps, core_ids=list(range(8)))
    out = np.zeros((2, 8192, D), np.float32)
    for j in range(8):
        b, r = j // 4, j % 4
        out[b, r * TPC:(r + 1) * TPC] = res.results[j]["out"]
    return out
```
